# Optimizing a Trainium2 kernel written in Bass

```python
import math
import numpy as np
import jax
import jax.numpy as jnp
from jax import lax

D_MODEL = 1024
BATCH = 8
SEQ = 4096
DEPTH = 2

N_MIXERS = 2
HEAD_DIM = 64
MIX_WIDTH = D_MODEL
MEM_HEADS = 4
MEM_WIDTH = MEM_HEADS * HEAD_DIM
TOK_WIDTH = MIX_WIDTH - MEM_WIDTH
MEM_LEN = 256
S5_GROUP_CH = 16
S5_GROUPS = TOK_WIDTH // S5_GROUP_CH
S5_STATE = 64
DT_MIN = 0.001
DT_MAX = 0.1
MOBA_HEADS = TOK_WIDTH // HEAD_DIM
MOBA_BLOCK = 256
MOBA_TOPK = 3
Q_CHUNK = 32
FFN_HIDDEN = -(-(8 * D_MODEL) // (3 * 256)) * 256
N_S5_LAYERS = (DEPTH + 1) // 2
N_MOBA_LAYERS = DEPTH // 2
RMS_EPS = 1e-6
NEG_INF = -1e30

kernel_name = "hybrid_s5_moba_memory_trunk"


def _rmsnorm(x, g):
    xf = x.astype(jnp.float32)
    y = xf * lax.rsqrt(jnp.mean(xf * xf, axis=-1, keepdims=True) + RMS_EPS) * g.astype(jnp.float32)
    return y.astype(x.dtype)


def _alibi_slopes(n):
    return jnp.asarray(2.0 ** (-8.0 * (np.arange(n) + 1) / n), dtype=jnp.float32)


def _swiglu(h, w_gate, w_up, w_down):
    return (jax.nn.silu(h @ w_gate) * (h @ w_up)) @ w_down


def _memory_attention(q, mem_k, mem_v):
    bsz, s, _ = q.shape
    qh = q.astype(jnp.float32).reshape(bsz, s, MEM_HEADS, HEAD_DIM) * (HEAD_DIM ** -0.5)
    scores = jnp.einsum('bshd,bmhd->bhsm', qh, mem_k.astype(jnp.float32))
    p = jax.nn.softmax(scores, axis=-1)
    out = jnp.einsum('bhsm,bmhd->bshd', p, mem_v.astype(jnp.float32))
    return out.reshape(bsz, s, MEM_WIDTH).astype(q.dtype)


def _s5_mixer(u, lam_re, lam_im, log_dt, b_re, b_im, c_re, c_im, d, w_glu, b_glu):
    bsz, s, _ = u.shape
    f32 = jnp.float32
    uf = u.astype(f32)
    ug = uf.reshape(bsz, s, S5_GROUPS, S5_GROUP_CH)
    lr = lam_re.astype(f32)
    li = lam_im.astype(f32)
    dt = jnp.exp(log_dt.astype(f32))[:, None]
    mag = jnp.exp(lr * dt)
    ar = mag * jnp.cos(li * dt)
    ai = mag * jnp.sin(li * dt)
    den = lr * lr + li * li
    nr = ar - 1.0
    fr = (nr * lr + ai * li) / den
    fi = (ai * lr - nr * li) / den
    br = b_re.astype(f32)
    bi = b_im.astype(f32)
    bbr = fr[..., None] * br - fi[..., None] * bi
    bbi = fr[..., None] * bi + fi[..., None] * br
    bu_r = jnp.einsum('bsgc,gpc->bsgp', ug, bbr)
    bu_i = jnp.einsum('bsgc,gpc->bsgp', ug, bbi)
    a_r = jnp.broadcast_to(ar[None, None], (1, s, S5_GROUPS, S5_STATE))
    a_i = jnp.broadcast_to(ai[None, None], (1, s, S5_GROUPS, S5_STATE))

    def combine(e1, e2):
        a1r, a1i, b1r, b1i = e1
        a2r, a2i, b2r, b2i = e2
        return (a2r * a1r - a2i * a1i,
                a2r * a1i + a2i * a1r,
                a2r * b1r - a2i * b1i + b2r,
                a2r * b1i + a2i * b1r + b2i)

    _, _, xr, xi = lax.associative_scan(combine, (a_r, a_i, bu_r, bu_i), axis=1)
    y = (jnp.einsum('bsgp,gcp->bsgc', xr, c_re.astype(f32))
         - jnp.einsum('bsgp,gcp->bsgc', xi, c_im.astype(f32)))
    y = y.reshape(bsz, s, TOK_WIDTH) + d.astype(f32) * uf
    y = jax.nn.gelu(y, approximate=False)
    y = y * jax.nn.sigmoid(y @ w_glu.astype(f32) + b_glu.astype(f32))
    return y.astype(u.dtype)


def _moba_attention(q, k, v, slopes):
    bsz, nh, s, dh = q.shape
    nb = -(-s // MOBA_BLOCK)
    pad = nb * MOBA_BLOCK - s
    k_blk = jnp.pad(k, ((0, 0), (0, 0), (0, pad), (0, 0))).reshape(bsz, nh, nb, MOBA_BLOCK, dh)
    v_blk = jnp.pad(v, ((0, 0), (0, 0), (0, pad), (0, 0))).reshape(bsz, nh, nb, MOBA_BLOCK, dh)
    k_mean = jnp.mean(k_blk, axis=3)
    top = min(MOBA_TOPK, nb)
    n_chunks = s // Q_CHUNK
    q_chunks = (q * (dh ** -0.5)).reshape(bsz, nh, n_chunks, Q_CHUNK, dh).transpose(2, 0, 1, 3, 4)
    offs = jnp.arange(MOBA_BLOCK, dtype=jnp.int32)
    blk_ids = jnp.arange(nb, dtype=jnp.int32)
    bidx = jnp.arange(bsz)[:, None, None, None]
    hidx = jnp.arange(nh)[None, :, None, None]
    slope5 = slopes[None, :, None, None, None]

    def one_chunk(args):
        ci, qc = args
        start = ci * Q_CHUNK
        t = start + jnp.arange(Q_CHUNK, dtype=jnp.int32)
        qb = start // MOBA_BLOCK
        gate = jnp.einsum('bhqd,bhnd->bhqn', qc, k_mean)
        gate = jnp.where(blk_ids < qb, gate, NEG_INF)
        _, sel = lax.top_k(gate, top)
        valid = jnp.arange(top) < qb
        k_sel = k_blk[bidx, hidx, sel]
        v_sel = v_blk[bidx, hidx, sel]
        s_sel = jnp.einsum('bhqd,bhqnld->bhqnl', qc, k_sel)
        pos_sel = sel[..., None] * MOBA_BLOCK + offs
        dist_sel = (t[None, None, :, None, None] - pos_sel).astype(jnp.float32)
        s_sel = jnp.where(valid[None, None, None, :, None], s_sel - slope5 * dist_sel, NEG_INF)
        k_own = lax.dynamic_index_in_dim(k_blk, qb, axis=2, keepdims=False)
        v_own = lax.dynamic_index_in_dim(v_blk, qb, axis=2, keepdims=False)
        s_own = jnp.einsum('bhqd,bhld->bhql', qc, k_own)
        dist_own = t[:, None] - (qb * MOBA_BLOCK + offs)[None, :]
        s_own = jnp.where(dist_own[None, None] >= 0,
                          s_own - slopes[:, None, None] * dist_own.astype(jnp.float32)[None, None],
                          NEG_INF)
        n_sel = top * MOBA_BLOCK
        s_all = jnp.concatenate([s_sel.reshape(bsz, nh, Q_CHUNK, n_sel), s_own], axis=-1)
        p = jax.nn.softmax(s_all, axis=-1)
        p_sel = p[..., :n_sel].reshape(bsz, nh, Q_CHUNK, top, MOBA_BLOCK)
        p_own = p[..., n_sel:]
        return (jnp.einsum('bhqnl,bhqnld->bhqd', p_sel, v_sel)
                + jnp.einsum('bhql,bhld->bhqd', p_own, v_own))

    out = lax.map(one_chunk, (jnp.arange(n_chunks, dtype=jnp.int32), q_chunks))
    return out.transpose(1, 2, 0, 3, 4).reshape(bsz, nh, s, dh)


def _moba_mixer(qkv, slopes):
    bsz, s, _ = qkv.shape
    t = qkv.astype(jnp.float32).reshape(bsz, s, 3, MOBA_HEADS, HEAD_DIM).transpose(2, 0, 3, 1, 4)
    out = _moba_attention(t[0], t[1], t[2], slopes)
    return out.transpose(0, 2, 1, 3).reshape(bsz, s, TOK_WIDTH).astype(qkv.dtype)


def setup_inputs(seed: int = 0) -> dict:
    key = jax.random.key(seed)
    ks = jax.random.split(key, 24)
    f32 = jnp.float32

    def nrm(k, shape, scale):
        return jax.random.normal(k, shape, f32) * scale

    n_idx = jnp.arange(S5_STATE, dtype=f32)
    return {
        "x": nrm(ks[0], (BATCH, SEQ, D_MODEL), 1.0),
        "mem": nrm(ks[1], (BATCH, MEM_LEN, D_MODEL), 1.0),
        "mem_norm_g": 1.0 + nrm(ks[2], (D_MODEL,), 0.01),
        "w_mem_kv": nrm(ks[3], (D_MODEL, 2 * MEM_WIDTH), D_MODEL ** -0.5),
        "mix_norm_g": 1.0 + nrm(ks[4], (DEPTH, D_MODEL), 0.01),
        "s5_w_in": nrm(ks[5], (N_S5_LAYERS, D_MODEL, TOK_WIDTH + MEM_WIDTH), D_MODEL ** -0.5),
        "s5_lambda_re": -0.5 + nrm(ks[6], (N_S5_LAYERS, S5_GROUPS, S5_STATE), 0.01),
        "s5_lambda_im": math.pi * n_idx + nrm(ks[7], (N_S5_LAYERS, S5_GROUPS, S5_STATE), 0.01),
        "s5_log_dt": jax.random.uniform(ks[8], (N_S5_LAYERS, S5_GROUPS), f32,
                                        minval=math.log(DT_MIN), maxval=math.log(DT_MAX)),
        "s5_b_re": nrm(ks[9], (N_S5_LAYERS, S5_GROUPS, S5_STATE, S5_GROUP_CH), (2 * S5_GROUP_CH) ** -0.5),
        "s5_b_im": nrm(ks[10], (N_S5_LAYERS, S5_GROUPS, S5_STATE, S5_GROUP_CH), (2 * S5_GROUP_CH) ** -0.5),
        "s5_c_re": nrm(ks[11], (N_S5_LAYERS, S5_GROUPS, S5_GROUP_CH, S5_STATE), (2 * S5_STATE) ** -0.5),
        "s5_c_im": nrm(ks[12], (N_S5_LAYERS, S5_GROUPS, S5_GROUP_CH, S5_STATE), (2 * S5_STATE) ** -0.5),
        "s5_d": nrm(ks[13], (N_S5_LAYERS, TOK_WIDTH), 1.0),
        "s5_w_glu": nrm(ks[14], (N_S5_LAYERS, TOK_WIDTH, TOK_WIDTH), TOK_WIDTH ** -0.5),
        "s5_b_glu": nrm(ks[15], (N_S5_LAYERS, TOK_WIDTH), 0.01),
        "moba_w_in": nrm(ks[16], (N_MOBA_LAYERS, D_MODEL, 3 * TOK_WIDTH + MEM_WIDTH), D_MODEL ** -0.5),
        "w_out": nrm(ks[17], (DEPTH, MIX_WIDTH, D_MODEL), MIX_WIDTH ** -0.5),
        "ffn_norm_g": 1.0 + nrm(ks[18], (DEPTH, D_MODEL), 0.01),
        "w_gate": nrm(ks[19], (DEPTH, D_MODEL, FFN_HIDDEN), D_MODEL ** -0.5),
        "w_up": nrm(ks[20], (DEPTH, D_MODEL, FFN_HIDDEN), D_MODEL ** -0.5),
        "w_down": nrm(ks[21], (DEPTH, FFN_HIDDEN, D_MODEL), FFN_HIDDEN ** -0.5),
        "final_norm_g": 1.0 + nrm(ks[22], (D_MODEL,), 0.01),
    }


def reference(x, mem, mem_norm_g, w_mem_kv, mix_norm_g, s5_w_in, s5_lambda_re, s5_lambda_im,
              s5_log_dt, s5_b_re, s5_b_im, s5_c_re, s5_c_im, s5_d, s5_w_glu, s5_b_glu,
              moba_w_in, w_out, ffn_norm_g, w_gate, w_up, w_down, final_norm_g):
    bsz = x.shape[0]
    mem_kv = _rmsnorm(mem, mem_norm_g) @ w_mem_kv
    mem_k = mem_kv[..., :MEM_WIDTH].reshape(bsz, -1, MEM_HEADS, HEAD_DIM)
    mem_v = mem_kv[..., MEM_WIDTH:].reshape(bsz, -1, MEM_HEADS, HEAD_DIM)
    slopes = _alibi_slopes(MOBA_HEADS)

    for i in range(DEPTH):
        j = i // N_MIXERS
        h = _rmsnorm(x, mix_norm_g[i])
        if i % N_MIXERS == 0:
            proj = h @ s5_w_in[j]
            tok = _s5_mixer(proj[..., :TOK_WIDTH], s5_lambda_re[j], s5_lambda_im[j], s5_log_dt[j],
                            s5_b_re[j], s5_b_im[j], s5_c_re[j], s5_c_im[j], s5_d[j],
                            s5_w_glu[j], s5_b_glu[j])
        else:
            proj = h @ moba_w_in[j]
            tok = _moba_mixer(proj[..., :3 * TOK_WIDTH], slopes)
        mem_out = _memory_attention(proj[..., -MEM_WIDTH:], mem_k, mem_v)
        x = x + jnp.concatenate([tok, mem_out], axis=-1) @ w_out[i]
        h = _rmsnorm(x, ffn_norm_g[i])
        x = x + _swiglu(h, w_gate[i], w_up[i], w_down[i])
    return _rmsnorm(x, final_norm_g)
```

```python
import math
import numpy as np
import concourse.bass as bass
import concourse.mybir as mybir
from concourse.bass_utils import run_bass_kernel_spmd

F32 = mybir.dt.float32
BF16 = mybir.dt.bfloat16
I32 = mybir.dt.int32
U8 = mybir.dt.uint8
AF = mybir.ActivationFunctionType
ALU = mybir.AluOpType
AX = mybir.AxisListType

T = 4096
D = 1024
NEG = -30000.0
MAGIC = 12582912.0


class Dep:
    __slots__ = ("w", "r")

    def __init__(self):
        self.w = None
        self.r = []


class Prog:
    ENGS = ("pe", "act", "dve", "pool", "sp")

    def __init__(self, nc, n_dma_sems=40):
        self.nc = nc
        self.streams = {e: [] for e in self.ENGS}
        self.sem = {e: nc.alloc_semaphore("s_" + e) for e in self.ENGS if e != "sp"}
        self.cnt = {e: 0 for e in self.ENGS}
        self.known = {e: {} for e in self.ENGS}
        self.dsems = [nc.alloc_semaphore("d%d" % i) for i in range(n_dma_sems)]
        self.dcnt = [0] * n_dma_sems
        self.dnext = 0
        self.tg = 0

    def _waits(self, eng, reads, writes):
        w = {}

        def add(t):
            if t is None:
                return
            s, v = t
            k = id(s)
            if k not in w or w[k][1] < v:
                w[k] = (s, v)

        for d in reads:
            add(d.w)
        for d in writes:
            add(d.w)
            for t in d.r:
                add(t)
        out = []
        kn = self.known[eng]
        own = id(self.sem["pe"]) if eng == "pe" else None
        for k, (s, v) in w.items():
            if k == own or kn.get(k, 0) >= v:
                continue
            kn[k] = v
            out.append((s, v))
        return out

    def _commit(self, tok, reads, writes):
        for d in reads:
            d.r.append(tok)
            if len(d.r) > 64:
                d.r = _compress(d.r)
        for d in writes:
            d.w = tok
            d.r = []

    def op(self, eng, fn, reads=(), writes=()):
        waits = self._waits(eng, reads, writes)
        self.cnt[eng] += 1
        tok = (self.sem[eng], self.cnt[eng])
        self.streams[eng].append((fn, waits, tok, 1))
        self._commit(tok, reads, writes)
        return tok

    def group(self, eng, fns, reads=(), writes=()):
        n = len(fns)
        if n == 1:
            return self.op(eng, fns[0], reads, writes)
        waits = self._waits(eng, reads, writes)
        self.streams[eng].append((fns[0], waits, None, 0))
        for fn in fns[1:-1]:
            self.streams[eng].append((fn, [], None, 0))
        self.cnt[eng] += 1
        tok = (self.sem[eng], self.cnt[eng])
        self.streams[eng].append((fns[-1], [], tok, 1))
        self._commit(tok, reads, writes)
        return tok

    def dma(self, q, fn, reads=(), writes=()):
        i = self.dnext
        self.dnext = (self.dnext + 1) % len(self.dsems)
        s = self.dsems[i]
        waits = self._waits(q, reads, writes)
        if self.dcnt[i] > 0:
            kn = self.known[q]
            if kn.get(id(s), 0) < self.dcnt[i]:
                kn[id(s)] = self.dcnt[i]
                waits.append((s, self.dcnt[i]))
        self.dcnt[i] += 16
        tok = (s, self.dcnt[i])
        self.streams[q].append((fn, waits, tok, 16))
        self._commit(tok, reads, writes)
        return tok

    def barrier(self):
        toks = [(self.sem[e], self.cnt[e]) for e in self.sem if self.cnt[e] > 0]
        toks += [(s, c) for s, c in zip(self.dsems, self.dcnt) if c > 0]
        for e in self.ENGS:
            kn = self.known[e]
            ws = []
            for s, v in toks:
                if kn.get(id(s), 0) < v:
                    kn[id(s)] = v
                    ws.append((s, v))
            if ws:
                self.streams[e].append((None, ws, None, 0))

    def finish(self):
        self.barrier()
        nc = self.nc
        streams = self.streams

        def replay(name, e):
            for fn, waits, tok, inc in streams[name]:
                for s, v in waits:
                    e.wait_ge(s, v)
                if fn is None:
                    continue
                ins = fn(e)
                if tok is not None:
                    ins.then_inc(tok[0], inc)

        with nc.Block() as block:
            @block.tensor
            def _(e):
                replay("pe", e)

            @block.scalar
            def _(e):
                replay("act", e)

            @block.vector
            def _(e):
                replay("dve", e)

            @block.gpsimd
            def _(e):
                replay("pool", e)

            @block.sync
            def _(e):
                replay("sp", e)


def _compress(toks):
    best = {}
    for s, v in toks:
        k = id(s)
        if k not in best or best[k][1] < v:
            best[k] = (s, v)
    return list(best.values())


_ES = {F32: 4, BF16: 2, I32: 4}


class Arena:
    def __init__(self, nc, nbytes):
        self.base = nc.alloc_sbuf_tensor("arena", [128, nbytes], U8).ap()
        self.n = nbytes
        self.limit = nbytes
        self.off = 0

    def alloc(self, shape, dt):
        es = _ES[dt]
        free = 1
        for s in shape[1:]:
            free *= s
        nb = free * es
        off = (self.off + 63) // 64 * 64
        assert off + nb <= self.limit, ("arena overflow", off, nb, self.limit)
        self.off = off + nb
        ap = self.base[0:shape[0], off:off + nb].bitcast(dt)
        if len(shape) == 3:
            ap = ap.rearrange("p (a b) -> p a b", a=shape[1])
        elif len(shape) == 4:
            ap = ap.rearrange("p (a b c) -> p a b c", a=shape[1], b=shape[2])
        elif len(shape) == 5:
            ap = ap.rearrange("p (a b c d) -> p a b c d", a=shape[1], b=shape[2], c=shape[3])
        return ap

    def alloc_top(self, shape, dt):
        es = _ES[dt]
        free = 1
        for s in shape[1:]:
            free *= s
        nb = (free * es + 63) // 64 * 64
        self.limit -= nb
        assert self.limit >= self.off, ("arena top overflow", self.limit, self.off)
        off = self.limit
        ap = self.base[0:shape[0], off:off + free * es].bitcast(dt)
        if len(shape) == 3:
            ap = ap.rearrange("p (a b) -> p a b", a=shape[1])
        return ap

    def release_top(self):
        self.limit = self.n

    def mark(self):
        return self.off

    def release(self, m):
        self.off = m


class Builder:
    def __init__(self, dbg=(), stop=None):
        self.dbg = set(dbg)
        self.stop = stop
        nc = self.nc = bass.Bass("TRN2", target_bir_lowering=False)
        self.P = Prog(nc)
        self.A = Arena(nc, 207 * 1024)
        self.banks = [nc.alloc_psum_tensor("psb%d" % i, [128, 512], F32).ap() for i in range(8)]
        self.bdep = [Dep() for _ in range(8)]
        self.bnext = 0
        self.bank_set = list(range(8))
        self.etog = 0
        self.inp = {}
        self.outs = []

    def din(self, name, shape):
        ap = self.nc.dram_tensor(name, list(shape), F32, kind="ExternalInput").ap()
        self.inp[name] = ap
        return ap

    def scratch(self, name, shape, dt):
        if name in self.dbg:
            self.outs.append(name)
            return self.nc.dram_tensor(name, list(shape), dt, kind="ExternalOutput").ap()
        return self.nc.dram_tensor(name, list(shape), dt).ap()

    def bank(self):
        bs = self.bank_set
        i = bs[self.bnext % len(bs)]
        self.bnext += 1
        return self.banks[i], self.bdep[i]

    def evac(self, out, in_, reads, writes, scale=None, eng=None):
        P = self.P
        if eng is None:
            eng = "act" if self.etog == 0 else "dve"
            self.etog ^= 1
        if eng == "act":
            sc = 1.0 if scale is None else scale
            return P.op("act", lambda e: e.activation(out=out, in_=in_, func=AF.Copy, scale=sc), reads, writes)
        if scale is None:
            return P.op(eng, lambda e: e.tensor_copy(out=out, in_=in_), reads, writes)
        return P.op(eng, lambda e: e.tensor_scalar(out=out, in0=in_, scalar1=scale, scalar2=None, op0=ALU.mult), reads, writes)

    def load_w(self, dst, dd, src2d, kt, ncols, c0=0, r0=0):
        P = self.P
        for k in range(kt):
            for c in range(0, ncols, 2048):
                cw = min(2048, ncols - c)
                P.dma("pool", lambda e, k=k, c=c, cw=cw: e.dma_start(
                    out=dst[:, k, c:c + cw], in_=src2d[r0 + k * 128:r0 + (k + 1) * 128, c0 + c:c0 + c + cw]),
                    writes=[dd])

    def rmsnorm(self, xT, dx, gcol, out, dout, n, tmp):
        P = self.P
        sq, dsq, rt, drt, rs, drs = tmp
        P.op("act", lambda e: e.activation(out=sq[:, :, 0:n], in_=xT, func=AF.Square), [dx], [dsq])
        ps, dps = self.bank()
        fns = [lambda e, f=f: e.matmul(ps[:, 0:n], lhsT=self.ones_bf, rhs=sq[:, f, 0:n], start=(f == 0), stop=(f == 7))
               for f in range(8)]
        P.group("pe", fns, [dsq, self.dconst], [dps])
        P.op("act", lambda e: e.activation(out=rt[:, 0:n], in_=ps[:, 0:n], func=AF.Sqrt, bias=self.eps_col, scale=1.0 / D),
             [dps, self.dconst], [drt])
        P.op("dve", lambda e: e.reciprocal(out=rs[:, 0:n], in_=rt[:, 0:n]), [drt], [drs])
        for f in range(8):
            P.op("dve", lambda e, f=f: e.scalar_tensor_tensor(
                out=out[:, f, :], in0=xT[:, f, :], scalar=self.gains[:, gcol + f:gcol + f + 1], in1=rs[:, 0:n],
                op0=ALU.mult, op1=ALU.mult), [dx, drs, self.dconst], [dout])

    def rms_tmp(self, n):
        A = self.A
        return (A.alloc([128, 8, n], BF16), Dep(), A.alloc([128, n], F32), Dep(), A.alloc([128, n], F32), Dep())

    def build(self):
        nc, P, A = self.nc, self.P, self.A
        din = self.din
        x = din("x", [T, D])
        mem = din("mem", [256, D])
        mem_norm_g = din("mem_norm_g", [D])
        w_mem_kv = din("w_mem_kv", [D, 512])
        mix_norm_g = din("mix_norm_g", [2, D])
        s5_w_in = din("s5_w_in", [D, D])
        lam_re = din("s5_lambda_re", [48, 64])
        lam_im = din("s5_lambda_im", [48, 64])
        log_dt = din("s5_log_dt", [48])
        b_re = din("s5_b_re", [48, 64, 16])
        b_im = din("s5_b_im", [48, 64, 16])
        c_re = din("s5_c_re", [48, 16, 64])
        c_im = din("s5_c_im", [48, 16, 64])
        s5_d = din("s5_d", [768])
        w_glu = din("s5_w_glu", [768, 768])
        b_glu = din("s5_b_glu", [768])
        moba_w_in = din("moba_w_in", [D, 2560])
        w_out = din("w_out", [2, D, D])
        ffn_norm_g = din("ffn_norm_g", [2, D])
        w_gate = din("w_gate", [2, D, 2816])
        w_up = din("w_up", [2, D, 2816])
        w_down = din("w_down", [2, 2816, D])
        final_norm_g = din("final_norm_g", [D])
        out = nc.dram_tensor("out", [T, D], F32, kind="ExternalOutput").ap()
        self.outs.append("out")

        XT0 = self.scratch("XT0", [8, 128, T], F32)
        XT1 = self.scratch("XT1", [8, 128, T], F32)
        XT2 = self.scratch("XT2", [8, 128, T], F32)
        XT3 = self.scratch("XT3", [8, 128, T], F32)
        ATD = [self.scratch("ATD%d" % l, [8, 128, T], BF16) for l in range(2)]
        HTD = [self.scratch("HTD%d" % l, [8, 128, T], BF16) for l in range(2)]
        YTD = self.scratch("YTD", [6, 128, T], BF16)
        QMD = self.scratch("QMD", [2, 128, T], BF16)
        ALD = self.scratch("ALD", [12, 3, T], BF16)
        NALD = self.scratch("NALD", [12, 3, T], BF16)

        self.DEND = self.scratch("DEND", [4, 512], F32)
        self.dconst = dconst = Dep()
        ident = A.alloc([128, 128], F32)
        ident_bf = A.alloc([128, 128], BF16)
        self.ones_bf = ones_bf = A.alloc([128, 128], BF16)
        self.ones_f = ones_f = A.alloc([128, 64], F32)
        self.eps_col = eps_col = A.alloc([128, 1], F32)
        self.gains = gains = A.alloc([128, 64], F32)
        memKT = A.alloc([128, 2, 256], BF16)
        memVA = A.alloc([128, 2, 4, 65], BF16)
        dmemK, dmemV = Dep(), Dep()
        self.ident, self.ident_bf = ident, ident_bf

        P.op("pool", lambda e: e.memset(ident, 0.0), [], [dconst])
        P.op("pool", lambda e: e.affine_select(out=ident, in_=ident, pattern=[[-1, 128]], compare_op=ALU.not_equal,
                                                fill=1.0, base=0, channel_multiplier=1), [dconst], [dconst])
        P.op("pool", lambda e: e.tensor_copy(out=ident_bf, in_=ident), [dconst], [dconst])
        P.op("pool", lambda e: e.memset(ones_bf, 1.0), [], [dconst])
        P.op("pool", lambda e: e.memset(ones_f, 1.0), [], [dconst])
        P.op("pool", lambda e: e.memset(eps_col, 1e-6), [], [dconst])

        m0 = A.mark()
        grow = A.alloc([64, 128], F32)
        dgrow = Dep()
        P.op("pool", lambda e: e.memset(grow, 0.0), [], [dgrow])
        srcs = [mix_norm_g[0], mix_norm_g[1], ffn_norm_g[0], ffn_norm_g[1], final_norm_g, mem_norm_g]
        for i, s in enumerate(srcs):
            P.dma("sp", lambda e, i=i, s=s: e.dma_start(out=grow[8 * i:8 * i + 8, :], in_=s.rearrange("(f p) -> f p", p=128)),
                  [], [dgrow])
        P.dma("sp", lambda e: e.dma_start(out=grow[48:54, :], in_=s5_d.rearrange("(f p) -> f p", p=128)), [], [dgrow])
        P.dma("sp", lambda e: e.dma_start(out=grow[54:60, :], in_=b_glu.rearrange("(f p) -> f p", p=128)), [], [dgrow])
        ps, dps = self.bank()
        P.op("pe", lambda e: e.transpose(ps[:, 0:64], grow, ident[0:64, 0:64]), [dgrow, dconst], [dps])
        P.op("dve", lambda e: e.tensor_copy(out=gains, in_=ps[:, 0:64]), [dps], [dconst])

        memt = A.alloc([128, 2, D], F32)
        dmemt = Dep()
        P.dma("sp", lambda e: e.dma_start(out=memt, in_=mem.rearrange("(m p) f -> p m f", p=128)), [], [dmemt])
        wkv = A.alloc([128, 8, 512], BF16)
        dwkv = Dep()
        self.load_w(wkv, dwkv, w_mem_kv, 8, 512)
        memT = A.alloc([128, 8, 256], F32)
        dmemT = Dep()
        for fp in range(4):
            ps, dps = self.bank()
            fns = []
            for fl in range(2):
                for m in range(2):
                    f = 2 * fp + fl
                    fns.append(lambda e, f=f, m=m, fl=fl, ps=ps: e.transpose(
                        ps[:, fl * 256 + m * 128: fl * 256 + (m + 1) * 128], memt[:, m, f * 128:(f + 1) * 128], ident))
            P.group("pe", fns, [dmemt, dconst], [dps])
            self.evac(memT[:, 2 * fp:2 * fp + 2, :].rearrange("p a b -> p (a b)"), ps, [dps], [dmemT])
        tmpn = self.rms_tmp(256)
        hmT = A.alloc([128, 8, 256], BF16)
        dhm = Dep()
        self.rmsnorm(memT, dmemT, 40, hmT, dhm, 256, tmpn)
        for hp in range(2):
            ps, dps = self.bank()
            fns = [lambda e, k=k, hp=hp, ps=ps: e.matmul(ps[:, 0:256], lhsT=wkv[:, k, hp * 128:(hp + 1) * 128], rhs=hmT[:, k, :],
                                                          start=(k == 0), stop=(k == 7)) for k in range(8)]
            P.group("pe", fns, [dwkv, dhm], [dps])
            self.evac(memKT[:, hp, :], ps[:, 0:256], [dps], [dmemK])
        P.op("pool", lambda e: e.memset(memVA[:, :, :, 64:65], 1.0), [], [dmemV])
        for m in range(2):
            ps, dps = self.bank()
            fns = [lambda e, k=k, m=m, ps=ps: e.matmul(ps[:, 0:256], lhsT=hmT[:, k, m * 128:(m + 1) * 128], rhs=wkv[:, k, 256:512],
                                                        start=(k == 0), stop=(k == 7)) for k in range(8)]
            P.group("pe", fns, [dwkv, dhm], [dps])
            self.evac(memVA[:, m, :, 0:64], ps[:, 0:256].rearrange("p (h d) -> p h d", h=4), [dps], [dmemV])
        P.barrier()
        A.release(m0)
        if self.stop == "M":
            return self._finish_dbg({"memKT": (memKT, dmemK, [128, 2, 256], BF16), "memVA": (memVA, dmemV, [128, 2, 4, 65], BF16),
                                     "gains": (gains, dconst, [128, 64], F32)})

        g_mix0, g_mix1, g_ffn0, g_ffn1, g_fin, g_mem = 0, 8, 16, 24, 32, 40
        mU = A.mark()
        UT = A.alloc([128, 6, T], BF16)
        dUT = [[Dep() for _ in range(8)] for _ in range(6)]
        mA = A.mark()
        QM = A.alloc([128, 2, T], BF16)
        dQM = [Dep() for _ in range(8)]
        dQMD = Dep()
        win = A.alloc([128, 8, D], BF16)
        dwin = Dep()
        self.load_w(win, dwin, s5_w_in, 8, D)
        xrow = [A.alloc([128, 4, D], F32) for _ in range(2)]
        dxrow = [Dep(), Dep()]
        xT = [A.alloc([128, 8, 512], F32) for _ in range(2)]
        dxT = [Dep(), Dep()]
        hT = [A.alloc([128, 8, 512], BF16) for _ in range(2)]
        dhT = [Dep(), Dep()]
        tmpn = self.rms_tmp(512)
        xv = x.rearrange("(c j p) f -> c p j f", p=128, j=4)
        UTv = [UT[:, ot, :].rearrange("p (s k) -> p s k", s=8) for ot in range(6)]
        for sl in range(8):
            b2 = sl % 2
            P.dma("sp", lambda e, sl=sl, b2=b2: e.dma_start(out=xrow[b2], in_=xv[sl]), [], [dxrow[b2]])
            for f in range(8):
                ps, dps = self.bank()
                fns = [lambda e, j=j, f=f, ps=ps, b2=b2: e.transpose(ps[:, j * 128:(j + 1) * 128], xrow[b2][:, j, f * 128:(f + 1) * 128], ident)
                       for j in range(4)]
                P.group("pe", fns, [dxrow[b2], dconst], [dps])
                self.evac(xT[b2][:, f, :], ps, [dps], [dxT[b2]])
            P.dma("sp", lambda e, sl=sl, b2=b2: e.dma_start(
                out=XT0[:, :, sl * 512:(sl + 1) * 512].rearrange("f p n -> p f n"), in_=xT[b2]), [dxT[b2]], [Dep()])
            self.rmsnorm(xT[b2], dxT[b2], g_mix0, hT[b2], dhT[b2], 512, tmpn)
            for ot in range(8):
                ps, dps = self.bank()
                fns = [lambda e, k=k, ot=ot, ps=ps, b2=b2: e.matmul(ps, lhsT=win[:, k, ot * 128:(ot + 1) * 128], rhs=hT[b2][:, k, :],
                                                                   start=(k == 0), stop=(k == 7)) for k in range(8)]
                P.group("pe", fns, [dwin, dhT[b2]], [dps])
                if ot < 6:
                    self.evac(UTv[ot][:, :, 64 * sl:64 * sl + 64], ps.rearrange("p (k s) -> p s k", s=8), [dps], dUT[ot])
                else:
                    self.evac(QM[:, ot - 6, sl * 512:(sl + 1) * 512], ps, [dps], [dQM[sl]], scale=0.125)
            P.dma("sp", lambda e, sl=sl: e.dma_start(out=QMD[:, :, sl * 512:(sl + 1) * 512].rearrange("f p n -> p f n"),
                                                     in_=QM[:, :, sl * 512:(sl + 1) * 512]), [dQM[sl]], [dQMD])
        P.barrier()
        A.release(mA)
        if self.stop == "A":
            dall = Dep()
            return self._finish_dbg({"UT": (UT, dall, [128, 6, T], BF16)})

        WS_sb = A.alloc([128, 6, 8, 2, 128], BF16)
        WY_sb = A.alloc([128, 48, 8, 32], BF16)
        KB_sb = A.alloc([128, 6, 8, 128], BF16)
        V12 = A.alloc([128, 48, 9, 2], F32)
        II = A.alloc([128, 64], BF16)
        dS5 = Dep()
        mS = A.mark()
        self._s5_setup(lam_re, lam_im, log_dt, b_re, b_im, c_re, c_im, WS_sb, WY_sb, KB_sb, V12, II, dS5)
        P.barrier()
        A.release(mS)
        if self.stop == "S":
            return self._finish_dbg({"WS": (WS_sb, dS5, [128, 6, 8, 2, 128], BF16), "WY": (WY_sb, dS5, [128, 48, 8, 32], BF16),
                                     "KB": (KB_sb, dS5, [128, 6, 8, 128], BF16), "V12": (V12, dS5, [128, 48, 9, 2], F32)})

        Xab = A.alloc([128, 8, 2, 512], BF16)
        dX = [[Dep(), Dep()] for _ in range(8)]
        XP = A.alloc([128, 2, 8, 512], BF16)
        dXP = [[Dep() for _ in range(8)] for _ in range(2)]
        DG = A.alloc([128, 8, 2, 128], BF16)
        dDG = [[Dep(), Dep()] for _ in range(8)]
        P.op("pool", lambda e: e.memset(XP[:, :, :, 0:1], 0.0), [], [d for r in dXP for d in r])
        def y_group(o, so):
            ob = o % 2
            ps, dps = self.bank()
            fns = []
            for si in range(so + 1):
                fns.append(lambda e, si=si: e.matmul(
                    ps, lhsT=KB_sb[:, o, so - si, :], rhs=UT[:, o, si * 512:(si + 1) * 512], start=(si == 0), stop=False))
            for g8 in range(8):
                j = g8 // 2
                fns.append(lambda e, g8=g8, j=j: e.matmul(
                    ps[32 * j:32 * j + 32, :], lhsT=WY_sb[:, 8 * o + g8, so, :], rhs=XP[:, ob, g8, :],
                    start=False, stop=(g8 == 7), tile_position=(0, 32 * j)))
            P.group("pe", fns, [dS5] + dUT[o][:so + 1] + dXP[ob], [dps])
            P.op("act", lambda e: e.activation(out=UT[:, o, so * 512:(so + 1) * 512], in_=ps, func=AF.Gelu), [dps], [dUT[o][so]])

        for o in range(6):
            ob = o % 2
            ylist = [(o - 1, so) for so in range(7, -1, -1)] if o > 0 else []
            for g8 in range(8):
                j, par = g8 // 2, g8 % 2
                ps, dps = self.bank()
                fns = [lambda e, sl=sl, j=j, par=par, o=o, ps=ps: e.matmul(
                    ps, lhsT=WS_sb[32 * j:32 * j + 32, o, sl, par, :], rhs=UT[32 * j:32 * j + 32, o, sl * 512:(sl + 1) * 512],
                    start=(sl == 0), stop=(sl == 7), tile_position=(32 * j, 0)) for sl in range(8)]
                P.group("pe", fns, [dS5] + dUT[o], [dps])
                self.evac(Xab[:, g8, 0, :], ps, [dps], [dX[g8][0]])
            cur = 0
            for st in range(9):
                dsh = 1 << st
                for g8 in range(8):
                    g = 8 * o + g8
                    db = st % 2
                    P.op("dve", lambda e, g=g, g8=g8, st=st, db=db: e.tensor_scalar(
                        out=DG[:, g8, db, 0:64], in0=II, scalar1=V12[:, g, st, 0:1], scalar2=None, op0=ALU.mult),
                        [dS5], [dDG[g8][db]])
                    P.op("dve", lambda e, g=g, g8=g8, st=st, db=db: e.tensor_scalar(
                        out=DG[:, g8, db, 64:128], in0=II, scalar1=V12[:, g, st, 1:2], scalar2=None, op0=ALU.mult),
                        [dS5], [dDG[g8][db]])
                    ps, dps = self.bank()
                    fns = [lambda e, g8=g8, cur=cur, ps=ps: e.matmul(ps, lhsT=ident_bf, rhs=Xab[:, g8, cur, :], start=True, stop=False),
                           lambda e, g8=g8, cur=cur, ps=ps, dsh=dsh, db=db: e.matmul(
                               ps[:, dsh:512], lhsT=DG[:, g8, db, :], rhs=Xab[:, g8, cur, 0:512 - dsh], start=False, stop=True)]
                    P.group("pe", fns, [dX[g8][cur], dDG[g8][db], dconst], [dps])
                    ev = "dve" if g8 in (3, 7) else "act"
                    if st < 8:
                        self.evac(Xab[:, g8, 1 - cur, :], ps, [dps], [dX[g8][1 - cur]], eng=ev)
                    else:
                        self.evac(XP[:, ob, g8, 1:512], ps[:, 0:511], [dps], [dXP[ob][g8]], eng=ev)
                if ylist:
                    y_group(*ylist.pop(0))
                cur = 1 - cur
            while ylist:
                y_group(*ylist.pop(0))
        for so in range(7, -1, -1):
            y_group(5, so)
        P.barrier()
        A.release(mA)
        if self.stop == "B":
            return self._finish_dbg({"UT": (UT, Dep(), [128, 6, T], BF16)})

        wglu = A.alloc([128, 6, 768], BF16)
        dwglu = Dep()
        self.load_w(wglu, dwglu, w_glu, 6, 768)
        wts = self.ffn_prefetch(w_gate[0], w_up[0])
        self.attn_init(depth=4, obanks=(0, 1, 3), dbbank=2)
        QMc = [A.alloc([128, 2, 512], BF16) for _ in range(2)]
        dQMc = [Dep(), Dep()]
        SG = [A.alloc([128, 512], BF16) for _ in range(2)]
        dSG = [Dep(), Dep()]
        TOKc = [A.alloc([128, 6, 512], BF16) for _ in range(2)]
        dTOK = [Dep(), Dep()]
        dATD = [Dep(), Dep()]
        for sl in range(8):
            b2 = sl % 2
            cs = slice(sl * 512, (sl + 1) * 512)
            P.dma("sp", lambda e, cs=cs, b2=b2: e.dma_start(out=QMc[b2], in_=QMD[:, :, cs].rearrange("f p n -> p f n")),
                  [dQMD], [dQMc[b2]])
            self.bank_set = [4, 5, 6, 7]
            ks = slice(64 * sl, 64 * sl + 64)
            dUall = [d for r in dUT for d in r]
            for ot in range(6):
                ps, dps = self.bank()
                fns = [lambda e, k=k, ot=ot, ps=ps, ks=ks: e.matmul(ps, lhsT=wglu[:, k, ot * 128:(ot + 1) * 128], rhs=UTv[k][:, :, ks],
                                                                   start=(k == 0), stop=(k == 5)) for k in range(6)]
                P.group("pe", fns, [dwglu] + dUall, [dps])
                sb = ot % 2
                P.op("act", lambda e, ot=ot, ps=ps, sb=sb: e.activation(out=SG[sb], in_=ps, func=AF.Sigmoid,
                                                                     bias=gains[:, 54 + ot:55 + ot]), [dps, dconst], [dSG[sb]])
                P.op("dve", lambda e, ot=ot, ks=ks, sb=sb, b2=b2: e.tensor_tensor(
                    out=TOKc[b2][:, ot, :].rearrange("p (k s) -> p s k", s=8), in0=UTv[ot][:, :, ks],
                    in1=SG[sb].rearrange("p (s k) -> p s k", s=8), op=ALU.mult), [dSG[sb]] + dUall, [dTOK[b2]])
            P.dma("sp", lambda e, cs=cs, b2=b2: e.dma_start(out=ATD[0][0:6, :, cs].rearrange("f p n -> p f n"), in_=TOKc[b2]),
                  [dTOK[b2]], [dATD[0]])
            for h in range(4):
                hb, hp = 64 * (h % 2), h // 2
                self.mem_unit(QMc[b2][hb:hb + 64, hp, :], dQMc[b2],
                              [memKT[hb:hb + 64, hp, j * 128:(j + 1) * 128] for j in range(2)],
                              [memVA[:, j, h, 0:64] for j in range(2)], [dmemK, dmemV],
                              ATD[0][6 + hp, hb:hb + 64, cs], dATD[0])
        self.bank_set = list(range(8))
        P.barrier()
        A.release(mU)
        if self.stop == "C":
            return self._finish_dbg({})

        H2D = self.scratch("H2D", [8, 128, T], BF16)
        self.gf_phase(wts, w_out[0], w_down[0], ATD[0], XT0, XT2, g_ffn0, None, None, nxt=(g_mix1, H2D))
        if self.stop == "D0":
            return self._finish_dbg({})
        if self.stop == "D0":
            return self._finish_dbg({})

        mL = A.mark()
        POS = A.alloc([128, 32], F32)
        VAL = A.alloc([128, 12, 32], F32)
        R1 = A.alloc([128, 12, 32], F32)
        PC = A.alloc([128, 3, 12, 32], BF16)
        NPC = A.alloc([128, 3, 12, 32], BF16)
        dal = Dep()
        dALD = Dep()
        P.op("pool", lambda e: e.iota(POS, pattern=[[1, 32]], base=0, channel_multiplier=32, allow_small_or_imprecise_dtypes=True), [], [dal])
        for h in range(12):
            sl_h = float(np.float32(2.0 ** (-8.0 * (h + 1) / 12)))
            P.op("dve", lambda e, h=h, sl_h=sl_h: e.tensor_scalar(out=VAL[:, h, :], in0=POS, scalar1=sl_h, scalar2=None, op0=ALU.mult),
                 [dal], [dal])
        P.op("dve", lambda e: e.tensor_copy(out=PC[:, 0, :, :], in_=VAL), [dal], [dal])
        P.op("dve", lambda e: e.tensor_tensor(out=R1, in0=VAL, in1=PC[:, 0, :, :], op=ALU.subtract), [dal], [dal])
        P.op("dve", lambda e: e.tensor_copy(out=PC[:, 1, :, :], in_=R1), [dal], [dal])
        P.op("dve", lambda e: e.tensor_tensor(out=VAL, in0=R1, in1=PC[:, 1, :, :], op=ALU.subtract), [dal], [dal])
        P.op("dve", lambda e: e.tensor_copy(out=PC[:, 2, :, :], in_=VAL), [dal], [dal])
        P.op("dve", lambda e: e.tensor_scalar(out=NPC, in0=PC, scalar1=-1.0, scalar2=None, op0=ALU.mult), [dal], [dal])
        for c3 in range(3):
            P.dma("sp", lambda e, c3=c3: e.dma_start(out=ALD[:, c3, :].rearrange("h (p j) -> p h j", j=32), in_=PC[:, c3, :, :]), [dal], [dALD])
            P.dma("sp", lambda e, c3=c3: e.dma_start(out=NALD[:, c3, :].rearrange("h (p j) -> p h j", j=32), in_=NPC[:, c3, :, :]), [dal], [dALD])
        P.barrier()
        A.release(mL)

        H2T = A.alloc([128, 8, T], BF16)
        dH2 = [Dep() for _ in range(8)]
        for f in range(8):
            P.dma("sp", lambda e, f=f: e.dma_start(out=H2T[:, f, :], in_=H2D[f]), [], dH2)
        if self.stop == "E":
            return self._finish_dbg({"H2T": (H2T, Dep(), [128, 8, T], BF16)})

        CM = A.alloc([128, 4, 512], BF16)
        dCM = Dep()
        mAI = A.mark()
        self.attn_init(depth=3, obanks=(0, 1, 2), dbbank=2)
        P.op("pool", lambda e: e.memset(CM.rearrange("p a b -> p (a b)"), 0.0), [], [dCM])
        for jj in range(4):
            P.op("pool", lambda e, jj=jj: e.affine_select(out=CM[:, jj, :], in_=CM[:, jj, :], pattern=[[1, 512]], compare_op=ALU.is_ge,
                                                          fill=NEG, base=-128 * jj, channel_multiplier=-1), [dCM], [dCM])
        dATD1 = Dep()
        mF = A.mark()
        for hg in range(3):
            QA = A.alloc([128, 4, T], BF16)
            KA = A.alloc([128, 4, T], BF16)
            VA = A.alloc([128, 32, 4, 65], BF16)
            wq = A.alloc([128, 8, 256], BF16)
            wk = A.alloc([128, 8, 256], BF16)
            wv = A.alloc([128, 8, 256], BF16)
            KS = A.alloc([64, 4, 16], F32)
            KSb = A.alloc([64, 4, 16], BF16)
            GM = A.alloc([128, 4, 16], F32)
            M8 = A.alloc([128, 4, 8], F32)
            SEL = A.alloc([128, 4, 16], F32)
            BTok = A.alloc([128, 4, 4, 16], BF16)
            BTS = [A.alloc([64, 512], BF16) for _ in range(2)]
            dwq, dwk, dwv, dKS, dKSb, dGM, dM8, dSEL, dBTok = [Dep() for _ in range(9)]
            dBTS = [Dep(), Dep()]
            dQ = [Dep() for _ in range(8)]
            dK = [Dep() for _ in range(8)]
            dV = [Dep() for _ in range(8)]
            dBias = [Dep() for _ in range(8)]
            dQaug, dKaug, dVone = Dep(), Dep(), Dep()
            self.load_w(wq, dwq, moba_w_in, 8, 256, c0=256 * hg)
            self.load_w(wk, dwk, moba_w_in, 8, 256, c0=768 + 256 * hg)
            self.load_w(wv, dwv, moba_w_in, 8, 256, c0=1536 + 256 * hg)
            P.op("pool", lambda e, QA=QA: e.memset(QA[64:86, :, :].rearrange("p a b -> p (a b)"), 1.0), [], [dQaug])
            P.op("pool", lambda e, KA=KA: e.memset(KA[64:86, :, :].rearrange("p a b -> p (a b)"), 1.0), [], [dKaug])
            P.op("pool", lambda e, KA=KA: e.affine_select(out=KA[64:80, :, :], in_=KA[64:80, :, :], pattern=[[0, 4], [1, T]],
                                                          compare_op=ALU.is_ge, fill=0.0, base=0, channel_multiplier=-256), [dKaug], [dKaug])
            P.op("pool", lambda e, KA=KA: e.affine_select(out=KA[64:80, :, :], in_=KA[64:80, :, :], pattern=[[0, 4], [-1, T]],
                                                          compare_op=ALU.is_ge, fill=0.0, base=255, channel_multiplier=256), [dKaug], [dKaug])
            for hh in range(4):
                hd = 4 * hg + hh
                P.dma("sp", lambda e, hh=hh, hd=hd, QA=QA: e.dma_start(out=QA[80:83, hh, :], in_=NALD[hd]), [dALD, dQaug], [dQaug])
                P.dma("sp", lambda e, hh=hh, hd=hd, KA=KA: e.dma_start(out=KA[83:86, hh, :], in_=ALD[hd]), [dALD, dKaug], [dKaug])
            P.op("pool", lambda e, VA=VA: e.memset(VA[:, :, :, 64:65], 1.0), [], [dVone])
            self.bank_set = list(range(8))
            for c in range(8):
                cs = slice(c * 512, (c + 1) * 512)
                for hh in range(4):
                    ps, dps = self.bank()
                    fns = [lambda e, k=k, hh=hh, ps=ps, cs=cs, wq=wq: e.matmul(ps[0:64, :], lhsT=wq[:, k, 64 * hh:64 * hh + 64], rhs=H2T[:, k, cs],
                                                                            start=(k == 0), stop=(k == 7)) for k in range(8)]
                    P.group("pe", fns, [dwq, dH2[c]], [dps])
                    P.op("act", lambda e, hh=hh, ps=ps, cs=cs, QA=QA: e.activation(out=QA[0:64, hh, cs], in_=ps[0:64, :], func=AF.Copy, scale=0.125),
                         [dps], [dQ[c]])
                    ps, dps = self.bank()
                    fns = [lambda e, k=k, hh=hh, ps=ps, cs=cs, wk=wk: e.matmul(ps[0:64, :], lhsT=wk[:, k, 64 * hh:64 * hh + 64], rhs=H2T[:, k, cs],
                                                                            start=(k == 0), stop=(k == 7)) for k in range(8)]
                    P.group("pe", fns, [dwk, dH2[c]], [dps])
                    P.op("dve", lambda e, hh=hh, ps=ps, cs=cs, KA=KA: e.tensor_copy(out=KA[0:64, hh, cs], in_=ps[0:64, :]), [dps], [dK[c]])
                    P.op("dve", lambda e, hh=hh, ps=ps, c=c, KS=KS: e.tensor_reduce(
                        out=KS[:, hh, 2 * c:2 * c + 2], in_=ps[0:64, :].rearrange("p (b n) -> p b n", b=2), axis=AX.X, op=ALU.add), [dps], [dKS])
                for s4 in range(4):
                    ps, dps = self.bank()
                    t0 = c * 512 + s4 * 128
                    fns = [lambda e, k=k, ps=ps, t0=t0, wv=wv: e.matmul(ps[:, 0:256], lhsT=H2T[:, k, t0:t0 + 128], rhs=wv[:, k, :],
                                                                     start=(k == 0), stop=(k == 7)) for k in range(8)]
                    P.group("pe", fns, [dwv, dH2[c]], [dps])
                    self.evac(VA[:, 4 * c + s4, :, 0:64], ps[:, 0:256].rearrange("p (h d) -> p h d", h=4), [dps], [dV[c]])
            P.op("dve", lambda e, KS=KS, KSb=KSb: e.tensor_copy(out=KSb, in_=KS), [dKS], [dKSb])
            for c in range(8):
                cs = slice(c * 512, (c + 1) * 512)
                for i in range(4):
                    tt_ = 4 * c + i
                    qb = tt_ // 2
                    bt = BTok[:, i, :, :]
                    if qb == 0:
                        P.op("pool", lambda e, bt=bt: e.memset(bt[:, :, 0:1], 0.0), [], [dBTok])
                        P.op("pool", lambda e, bt=bt: e.memset(bt[:, :, 1:16], NEG), [], [dBTok])
                        continue
                    ps, dps = self.bank()
                    fns = [lambda e, hh=hh, ps=ps, tt_=tt_, QA=QA, KSb=KSb: e.matmul(
                        ps[:, 16 * hh:16 * hh + 16], lhsT=QA[0:64, hh, tt_ * 128:(tt_ + 1) * 128], rhs=KSb[:, hh, :], start=True, stop=True)
                        for hh in range(4)]
                    P.group("pe", fns, [dQ[c], dKSb], [dps])
                    if qb < 3:
                        P.op("dve", lambda e, GM=GM: e.memset(GM.rearrange("p a b -> p (a b)"), -1e30), [], [dGM])
                    P.op("dve", lambda e, ps=ps, qb=qb, GM=GM: e.tensor_copy(out=GM[:, :, 0:qb], in_=ps[:, 0:64].rearrange("p (h n) -> p h n", h=4)[:, :, 0:qb]),
                         [dps], [dGM])
                    if qb >= 3:
                        P.op("dve", lambda e, qb=qb, GM=GM: e.memset(GM[:, :, qb:qb + 1], 1e30), [dGM], [dGM])
                    for hh in range(4):
                        P.op("dve", lambda e, hh=hh, GM=GM, M8=M8: e.max(out=M8[:, hh, :], in_=GM[:, hh, :]), [dGM], [dM8])
                    ti = 3 if qb >= 3 else 2
                    P.op("dve", lambda e, GM=GM, M8=M8, SEL=SEL, ti=ti: e.tensor_tensor(
                        out=SEL, in0=GM, in1=M8[:, :, ti:ti + 1].to_broadcast([128, 4, 16]), op=ALU.is_ge), [dGM, dM8], [dSEL])
                    P.op("dve", lambda e, bt=bt, SEL=SEL: e.tensor_scalar(out=bt, in0=SEL, scalar1=-NEG, scalar2=NEG, op0=ALU.mult, op1=ALU.add),
                         [dSEL], [dBTok])
                    if qb < 3:
                        P.op("dve", lambda e, bt=bt, qb=qb: e.memset(bt[:, :, qb:qb + 1], 0.0), [dBTok], [dBTok])
                        P.op("dve", lambda e, bt=bt, qb=qb: e.memset(bt[:, :, qb + 1:16], NEG), [dBTok], [dBTok])
                ps, dps = self.bank()
                psb = ps.bitcast(BF16)
                fns = [lambda e, i=i, psb=psb, BTok=BTok: e.transpose(psb[0:64, 128 * i:128 * i + 128],
                                                                    BTok[:, i, :, :].rearrange("p h n -> p (h n)"), ident_bf) for i in range(4)]
                P.group("pe", fns, [dBTok, dconst], [dps])
                b2 = c % 2
                P.op("act", lambda e, psb=psb, b2=b2, BTS=BTS: e.activation(out=BTS[b2], in_=psb[0:64, 0:512], func=AF.Copy), [dps], [dBTS[b2]])
                for hh in range(4):
                    P.dma("sp", lambda e, hh=hh, b2=b2, cs=cs, QA=QA, BTS=BTS: e.dma_start(out=QA[64:80, hh, cs], in_=BTS[b2][16 * hh:16 * hh + 16, :]),
                          [dBTS[b2], dQaug], [dBias[c]])
            if self.stop == "F2" and hg == 0:
                return self._finish_dbg({"QA": (QA, Dep(), [128, 4, T], BF16), "KA": (KA, Dep(), [128, 4, T], BF16)})
            self.bank_set = [3, 4, 5, 6, 7]
            for c in range(8):
                cs = slice(c * 512, (c + 1) * 512)
                nk = 4 * c + 4
                for hh in range(4):
                    hd = 4 * hg + hh
                    self.attn_unit(QA[0:86, hh, cs], [dQ[c], dBias[c], dQaug],
                                   [KA[0:86, hh, j * 128:(j + 1) * 128] for j in range(nk)],
                                   [VA[:, j, hh, :] for j in range(nk)], dK[:c + 1] + dV[:c + 1] + [dKaug, dVone],
                                   [None] * (nk - 4) + [CM[:, jj, :] for jj in range(4)],
                                   ATD[1][hd // 2, 64 * (hd % 2):64 * (hd % 2) + 64, cs], dATD1, cm_dep=dCM)
            self.bank_set = list(range(8))
            P.barrier()
            A.release(mF)
        A.release(mAI)
        self.attn_init(depth=4, obanks=(0, 1, 3), dbbank=2)
        wqm = A.alloc([128, 8, 256], BF16)
        dwqm = Dep()
        self.load_w(wqm, dwqm, moba_w_in, 8, 256, c0=2304)
        wts = self.ffn_prefetch(w_gate[1], w_up[1])
        QMn = A.alloc([128, 2, T], BF16)
        dQn = [Dep() for _ in range(8)]
        for c in range(8):
            cs = slice(c * 512, (c + 1) * 512)
            self.bank_set = [4, 5, 6, 7]
            for hp in range(2):
                ps, dps = self.bank()
                fns = [lambda e, k=k, hp=hp, ps=ps, cs=cs: e.matmul(ps, lhsT=wqm[:, k, 128 * hp:128 * hp + 128], rhs=H2T[:, k, cs],
                                                                   start=(k == 0), stop=(k == 7)) for k in range(8)]
                P.group("pe", fns, [dwqm, dH2[c]], [dps])
                self.evac(QMn[:, hp, cs], ps, [dps], [dQn[c]], scale=0.125)
            for h in range(4):
                hb, hp = 64 * (h % 2), h // 2
                self.mem_unit(QMn[hb:hb + 64, hp, cs], dQn[c],
                              [memKT[hb:hb + 64, hp, j * 128:(j + 1) * 128] for j in range(2)],
                              [memVA[:, j, h, 0:64] for j in range(2)], [dmemK, dmemV],
                              ATD[1][6 + hp, hb:hb + 64, cs], dATD1)
        self.bank_set = list(range(8))
        P.barrier()
        A.release(mL)
        if self.stop == "F":
            return self._finish_dbg({})
        self.gf_phase(wts, w_out[1], w_down[1], ATD[1], XT2, None, g_ffn1, out, g_fin)
        return self._finish_dbg({})

    def attn_init(self, depth=2, obanks=(0, 1), dbbank=2):
        A = self.A
        self.E = [A.alloc([128, 512], BF16) for _ in range(4)]
        self.dE = [Dep() for _ in range(4)]
        self.enext = 0
        self.adepth = depth
        self.obanks = list(obanks)
        self.dbbank = dbbank
        self.DEN = [A.alloc([128, 512], F32) for _ in range(depth)]
        self.dDEN = [Dep() for _ in range(depth)]
        self.RB = [A.alloc([64, 512], F32) for _ in range(depth)]
        self.dRB = [Dep() for _ in range(depth)]
        self.ON = [A.alloc([64, 512], BF16) for _ in range(depth)]
        self.dON = [Dep() for _ in range(depth)]
        self.unext = 0

    def mem_unit(self, q_ap, dq, k_tiles, v_tiles, dkv, dest, ddest):
        P = self.P
        u = self.unext
        self.unext += 1
        E, dE = self.E, self.dE
        di = u % self.adepth
        LN, dLN, RB, dRB, ON, dON = self.DEN[di], self.dDEN[di], self.RB[di], self.dRB[di], self.ON[di], self.dON[di]
        ones_bf, dconst = self.ones_bf, self.dconst
        O, dO = self.banks[u % 2], self.bdep[u % 2]
        DB, dDB = self.banks[2 + u % 2], self.bdep[2 + u % 2]
        dql = list(dq) if isinstance(dq, (list, tuple)) else [dq]
        eis = []
        for j in range(2):
            ps, dps = self.bank()
            P.op("pe", lambda e, j=j, ps=ps: e.matmul(ps, lhsT=k_tiles[j], rhs=q_ap, start=True, stop=True), dql + dkv, [dps])
            ei = self.enext % 4
            self.enext += 1
            P.op("act", lambda e, ei=ei, ps=ps: e.activation(out=E[ei], in_=ps, func=AF.Exp), [dps], [dE[ei]])
            eis.append(ei)
        fns = [lambda e, j=j: e.matmul(O[0:64, :], lhsT=v_tiles[j], rhs=E[eis[j]], start=(j == 0), stop=(j == 1)) for j in range(2)]
        P.group("pe", fns, [dE[eis[0]], dE[eis[1]]] + dkv, [dO])
        fns = [lambda e, j=j: e.matmul(DB[0:64, :], lhsT=ones_bf[:, 0:64], rhs=E[eis[j]], start=(j == 0), stop=(j == 1)) for j in range(2)]
        P.group("pe", fns, [dE[eis[0]], dE[eis[1]], dconst], [dDB])
        P.op("act", lambda e: e.activation(out=LN[0:64, :], in_=DB[0:64, :], func=AF.Ln), [dDB], [dLN])
        P.op("act", lambda e: e.activation(out=RB, in_=LN[0:64, :], func=AF.Exp, scale=-1.0), [dLN], [dRB])
        P.op("dve", lambda e: e.tensor_tensor(out=ON, in0=O[0:64, :], in1=RB, op=ALU.mult), [dO, dRB], [dON])
        P.dma("sp", lambda e: e.dma_start(out=dest, in_=ON), [dON], [ddest])

    def attn_unit(self, q_ap, dq, k_tiles, v_tiles, dkv, cms, dest, ddest, cm_dep=None):
        P = self.P
        u = self.unext
        self.unext += 1
        E, dE = self.E, self.dE
        di = u % self.adepth
        DEN, dDEN, RB, dRB = self.DEN[di], self.dDEN[di], self.RB[di], self.dRB[di]
        ON, dON = self.ON, self.dON
        ones_f, ident_bf = self.ones_f, self.ident_bf
        ob = self.obanks[u % len(self.obanks)]
        O, dO = self.banks[ob], self.bdep[ob]
        DB, dDB = self.banks[self.dbbank], self.bdep[self.dbbank]
        n = len(k_tiles)
        K = q_ap.shape[0]
        Sj = [None] * n

        dql = list(dq) if isinstance(dq, (list, tuple)) else [dq]

        def qk(j):
            ps, dps = self.bank()
            c0 = 0
            if cms[j] is None:
                P.op("pe", lambda e: e.matmul(ps, lhsT=k_tiles[j], rhs=q_ap, start=True, stop=True), dql + dkv, [dps])
            else:
                c0 = 128 * (j - (n - 4))
                fns = [lambda e: e.matmul(ps[:, c0:512], lhsT=k_tiles[j], rhs=q_ap[:, c0:512], start=True, stop=False),
                       lambda e: e.matmul(ps[:, c0:512], lhsT=ident_bf, rhs=cms[j][:, c0:512], start=False, stop=True)]
                P.group("pe", fns, dql + [cm_dep, self.dconst] + dkv, [dps])
            ei = self.enext % 4
            self.enext += 1
            P.op("act", lambda e: e.activation(out=E[ei][:, c0:512], in_=ps[:, c0:512], func=AF.Exp), [dps], [dE[ei]])
            Sj[j] = (ei, c0)

        def pv(j):
            ei, c0 = Sj[j]
            P.op("pe", lambda e: e.matmul(O[0:65, c0:512], lhsT=v_tiles[j], rhs=E[ei][:, c0:512], start=(j == 0), stop=(j == n - 1)),
                 [dE[ei]] + dkv, [dO])

        LOOK = 2
        for j in range(min(LOOK, n)):
            qk(j)
        for j in range(n):
            if j + LOOK < n:
                qk(j + LOOK)
            pv(j)
        P.op("act", lambda e: e.activation(out=DEN[64:65, :], in_=O[64:65, :], func=AF.Copy), [dO], [dDEN])
        dend = self.DEND[di:di + 1, :]
        P.dma("sp", lambda e: e.dma_start(out=dend, in_=DEN[64:65, :]), [dDEN], [dDEN])
        P.dma("sp", lambda e: e.dma_start(out=DEN[0:64, :], in_=dend.partition_broadcast(64)), [dDEN], [dDEN])
        P.op("dve", lambda e: e.reciprocal(out=RB, in_=DEN[0:64, :]), [dDEN], [dRB])
        oi = di
        P.op("dve", lambda e: e.tensor_tensor(out=ON[oi], in0=O[0:64, :], in1=RB, op=ALU.mult), [dO, dRB], [dON[oi]])
        P.dma("sp", lambda e: e.dma_start(out=dest, in_=ON[oi]), [dON[oi]], [ddest])

    def ffn_prefetch(self, wg_d, wu_d):
        P, A = self.P, self.A
        wg = A.alloc_top([128, 8, 2816], BF16)
        wu = A.alloc_top([128, 8, 2816], BF16)
        dwg, dwu = Dep(), Dep()
        self._ffn_w = (None, wg, wu, dwg, dwu, wg_d, wu_d)
        self._issue_ffn_loads()
        return self._ffn_w

    def gf_phase(self, wts, wo_d, wd_d, ATDl, XTin, XTout, gcol, out_final, gfin, nxt=None):
        P, A = self.P, self.A
        _, wg, wu, dwg, dwu, _, _ = wts
        m = A.mark()
        NC = 256
        NCH = T // NC
        wout = A.alloc([128, 8, D], BF16)
        dwo = Dep()
        self.load_w(wout, dwo, wo_d, 8, D)
        wd = A.alloc([128, 22, D], BF16)
        dwd = Dep()
        self.load_w(wd, dwd, wd_d, 22, D)
        ATc = A.alloc([128, 8, NC], BF16)
        dAT = Dep()
        xc = [A.alloc([128, 8, NC], F32) for _ in range(2)]
        dxc = [Dep(), Dep()]
        hc = [A.alloc([128, 8, NC], BF16) for _ in range(2)]
        dhc = [Dep(), Dep()]
        Ac = A.alloc([128, 22, NC], BF16)
        dAc = [Dep() for _ in range(22)]
        sg = [A.alloc([128, NC], F32) for _ in range(2)]
        dsg = [Dep(), Dep()]
        tmpn = self.rms_tmp(NC)
        if nxt is not None:
            hn = A.alloc([128, 8, NC], BF16)
            dhn = Dep()
        if out_final is not None:
            OT = A.alloc([128, 2, D], F32)
            dOT = Dep()
            dfin = [Dep(), Dep()]
            outv = out_final.rearrange("(c t p) f -> c p t f", p=128, t=2)

        def op_part(c):
            b2 = c % 2
            cs = slice(c * NC, (c + 1) * NC)
            P.dma("sp", lambda e: e.dma_start(out=ATc, in_=ATDl[:, :, cs].rearrange("f p n -> p f n")), [], [dAT])
            P.dma("sp", lambda e: e.dma_start(out=xc[b2], in_=XTin[:, :, cs].rearrange("f p n -> p f n")), [], [dxc[b2]])
            for ft in range(8):
                ps, dps = self.bank()
                fns = [lambda e, k=k, ft=ft, ps=ps: e.matmul(ps[:, 0:NC], lhsT=wout[:, k, ft * 128:(ft + 1) * 128], rhs=ATc[:, k, :],
                                                            start=(k == 0), stop=(k == 7)) for k in range(8)]
                P.group("pe", fns, [dwo, dAT], [dps])
                P.op("dve", lambda e, ft=ft, ps=ps: e.tensor_tensor(out=xc[b2][:, ft, :], in0=ps[:, 0:NC], in1=xc[b2][:, ft, :], op=ALU.add),
                     [dps], [dxc[b2]])
            self.rmsnorm(xc[b2], dxc[b2], gcol, hc[b2], dhc[b2], NC, tmpn)

        def ffn_gu(c, mid=None):
            b2 = c % 2
            for ht in range(22):
                if ht == 6 and mid is not None:
                    mid()
                psg, dpsg = self.bank()
                fns = [lambda e, k=k, ht=ht, ps=psg: e.matmul(ps[:, 0:NC], lhsT=wg[:, k, ht * 128:(ht + 1) * 128], rhs=hc[b2][:, k, :],
                                                             start=(k == 0), stop=(k == 7)) for k in range(8)]
                P.group("pe", fns, [dwg, dhc[b2]], [dpsg])
                psu, dpsu = self.bank()
                fns = [lambda e, k=k, ht=ht, ps=psu: e.matmul(ps[:, 0:NC], lhsT=wu[:, k, ht * 128:(ht + 1) * 128], rhs=hc[b2][:, k, :],
                                                             start=(k == 0), stop=(k == 7)) for k in range(8)]
                P.group("pe", fns, [dwu, dhc[b2]], [dpsu])
                sb = ht % 2
                P.op("act", lambda e, ps=psg, sb=sb: e.activation(out=sg[sb], in_=ps[:, 0:NC], func=AF.Silu), [dpsg], [dsg[sb]])
                P.op("dve", lambda e, ps=psu, sb=sb, ht=ht: e.tensor_tensor(out=Ac[:, ht, :], in0=ps[:, 0:NC], in1=sg[sb], op=ALU.mult),
                     [dpsu, dsg[sb]], [dAc[ht]])

        def ffn_down(c):
            b2 = c % 2
            cs = slice(c * NC, (c + 1) * NC)
            for ft in range(8):
                ps, dps = self.bank()
                fns = [lambda e, ht=ht, ft=ft, ps=ps: e.matmul(ps[:, 0:NC], lhsT=wd[:, ht, ft * 128:(ft + 1) * 128], rhs=Ac[:, ht, :],
                                                              start=(ht == 0), stop=(ht == 21)) for ht in range(22)]
                P.group("pe", fns, [dwd] + dAc, [dps])
                P.op("dve", lambda e, ft=ft, ps=ps: e.tensor_tensor(out=xc[b2][:, ft, :], in0=ps[:, 0:NC], in1=xc[b2][:, ft, :], op=ALU.add),
                     [dps], [dxc[b2]])
            if out_final is None:
                P.dma("sp", lambda e: e.dma_start(out=XTout[:, :, cs].rearrange("f p n -> p f n"), in_=xc[b2]), [dxc[b2]], [Dep()])
                if nxt is not None:
                    self.rmsnorm(xc[b2], dxc[b2], nxt[0], hn, dhn, NC, tmpn)
                    P.dma("sp", lambda e: e.dma_start(out=nxt[1][:, :, cs].rearrange("f p n -> p f n"), in_=hn), [dhn], [dhn])
            else:
                self.rmsnorm(xc[b2], dxc[b2], gfin, xc[b2], dxc[b2], NC, tmpn)

        def final_part(c):
            b2 = c % 2
            for t2 in range(2):
                for fq in range(2):
                    ps, dps = self.bank()
                    fns = [lambda e, i=i, t2=t2, fq=fq, ps=ps: e.transpose(
                        ps[:, 128 * i:128 * i + 128], xc[b2][:, 4 * fq + i, t2 * 128:(t2 + 1) * 128], self.ident) for i in range(4)]
                    P.group("pe", fns, [dxc[b2], self.dconst], [dps])
                    self.evac(OT[:, t2, fq * 512:(fq + 1) * 512], ps, [dps], [dOT])
            P.dma("sp", lambda e: e.dma_start(out=outv[c], in_=OT), [dOT, dxc[b2]], [dOT, dxc[b2]])

        op_part(0)
        for c in range(NCH):
            if out_final is not None and c > 0:
                ffn_gu(c, mid=lambda c=c: final_part(c - 1))
            else:
                ffn_gu(c)
            if c + 1 < NCH:
                op_part(c + 1)
            ffn_down(c)
        if out_final is not None:
            final_part(NCH - 1)
        P.barrier()
        A.release(m)
        A.release_top()

    def _issue_ffn_loads(self):
        P = self.P
        m, wg, wu, dwg, dwu, wg_d, wu_d = self._ffn_w
        for k in range(8):
            for (dst, dd, src) in ((wg, dwg, wg_d), (wu, dwu, wu_d)):
                for c in (0, 2048):
                    cw = min(2048, 2816 - c)
                    P.dma("pool", lambda e, dst=dst, src=src, k=k, c=c, cw=cw: e.dma_start(
                        out=dst[:, k, c:c + cw], in_=src[k * 128:(k + 1) * 128, c:c + cw]), [], [dd])

    def outproj_phase(self, wo, ATDl, XTin, XTout, HTDl, gcol):
        P, A = self.P, self.A
        m = A.mark()
        wout = A.alloc([128, 8, D], BF16)
        dwo = Dep()
        self.load_w(wout, dwo, wo, 8, D)
        self._issue_ffn_loads()
        ATc = [A.alloc([128, 8, 512], BF16) for _ in range(2)]
        dAT = [Dep(), Dep()]
        xc = [A.alloc([128, 8, 512], F32) for _ in range(2)]
        dxc = [Dep(), Dep()]
        hc = [A.alloc([128, 8, 512], BF16) for _ in range(2)]
        dhc = [Dep(), Dep()]
        tmpn = self.rms_tmp(512)
        for c in range(8):
            b2 = c % 2
            cs = slice(c * 512, (c + 1) * 512)
            P.dma("sp", lambda e, cs=cs, b2=b2: e.dma_start(out=ATc[b2], in_=ATDl[:, :, cs].rearrange("f p n -> p f n")), [], [dAT[b2]])
            P.dma("sp", lambda e, cs=cs, b2=b2: e.dma_start(out=xc[b2], in_=XTin[:, :, cs].rearrange("f p n -> p f n")), [], [dxc[b2]])
            for ft in range(8):
                ps, dps = self.bank()
                fns = [lambda e, k=k, ft=ft, ps=ps, b2=b2: e.matmul(ps, lhsT=wout[:, k, ft * 128:(ft + 1) * 128], rhs=ATc[b2][:, k, :],
                                                                   start=(k == 0), stop=(k == 7)) for k in range(8)]
                P.group("pe", fns, [dwo, dAT[b2]], [dps])
                P.op("dve", lambda e, ft=ft, ps=ps, b2=b2: e.tensor_tensor(out=xc[b2][:, ft, :], in0=ps, in1=xc[b2][:, ft, :], op=ALU.add),
                     [dps], [dxc[b2]])
            P.dma("sp", lambda e, cs=cs, b2=b2: e.dma_start(out=XTout[:, :, cs].rearrange("f p n -> p f n"), in_=xc[b2]), [dxc[b2]], [Dep()])
            self.rmsnorm(xc[b2], dxc[b2], gcol, hc[b2], dhc[b2], 512, tmpn)
            P.dma("sp", lambda e, cs=cs, b2=b2: e.dma_start(out=HTDl[:, :, cs].rearrange("f p n -> p f n"), in_=hc[b2]), [dhc[b2]], [Dep()])
        P.barrier()
        A.release(m)

    def ffn_phase(self, wts, wd_d, HTDl, XTin, XTout, out_final, gfin):
        P, A = self.P, self.A
        m, wg, wu, dwg, dwu, _, _ = wts
        NC = 256
        wd = A.alloc([128, 22, D], BF16)
        dwd = Dep()
        self.load_w(wd, dwd, wd_d, 22, D)
        hc = [A.alloc([128, 8, NC], BF16) for _ in range(2)]
        dhc = [Dep(), Dep()]
        xc = [A.alloc([128, 8, NC], F32) for _ in range(2)]
        dxc = [Dep(), Dep()]
        Ac = A.alloc([128, 22, NC], BF16)
        dAc = [Dep() for _ in range(22)]
        sg = [A.alloc([128, NC], F32) for _ in range(2)]
        dsg = [Dep(), Dep()]
        if out_final is not None:
            yT = A.alloc([128, 8, NC], F32)
            dyT = Dep()
            OT = A.alloc([128, 2, D], F32)
            dOT = Dep()
            tmpn = self.rms_tmp(NC)
            outv = out_final.rearrange("(c t p) f -> c p t f", p=128, t=2)
        for c in range(T // NC):
            b2 = c % 2
            cs = slice(c * NC, (c + 1) * NC)
            P.dma("sp", lambda e, cs=cs, b2=b2: e.dma_start(out=hc[b2], in_=HTDl[:, :, cs].rearrange("f p n -> p f n")), [], [dhc[b2]])
            P.dma("sp", lambda e, cs=cs, b2=b2: e.dma_start(out=xc[b2], in_=XTin[:, :, cs].rearrange("f p n -> p f n")), [], [dxc[b2]])
            for ht in range(22):
                psg, dpsg = self.bank()
                fns = [lambda e, k=k, ht=ht, ps=psg, b2=b2: e.matmul(ps[:, 0:NC], lhsT=wg[:, k, ht * 128:(ht + 1) * 128], rhs=hc[b2][:, k, :],
                                                                    start=(k == 0), stop=(k == 7)) for k in range(8)]
                P.group("pe", fns, [dwg, dhc[b2]], [dpsg])
                psu, dpsu = self.bank()
                fns = [lambda e, k=k, ht=ht, ps=psu, b2=b2: e.matmul(ps[:, 0:NC], lhsT=wu[:, k, ht * 128:(ht + 1) * 128], rhs=hc[b2][:, k, :],
                                                                    start=(k == 0), stop=(k == 7)) for k in range(8)]
                P.group("pe", fns, [dwu, dhc[b2]], [dpsu])
                sb = ht % 2
                P.op("act", lambda e, ps=psg, sb=sb: e.activation(out=sg[sb], in_=ps[:, 0:NC], func=AF.Silu), [dpsg], [dsg[sb]])
                P.op("dve", lambda e, ps=psu, sb=sb, ht=ht: e.tensor_tensor(out=Ac[:, ht, :], in0=ps[:, 0:NC], in1=sg[sb], op=ALU.mult),
                     [dpsu, dsg[sb]], [dAc[ht]])
            for ft in range(8):
                ps, dps = self.bank()
                fns = [lambda e, ht=ht, ft=ft, ps=ps: e.matmul(ps[:, 0:NC], lhsT=wd[:, ht, ft * 128:(ft + 1) * 128], rhs=Ac[:, ht, :],
                                                              start=(ht == 0), stop=(ht == 21)) for ht in range(22)]
                P.group("pe", fns, [dwd] + dAc, [dps])
                P.op("dve", lambda e, ft=ft, ps=ps, b2=b2: e.tensor_tensor(out=xc[b2][:, ft, :], in0=ps[:, 0:NC], in1=xc[b2][:, ft, :], op=ALU.add),
                     [dps], [dxc[b2]])
            if out_final is None:
                P.dma("sp", lambda e, cs=cs, b2=b2: e.dma_start(out=XTout[:, :, cs].rearrange("f p n -> p f n"), in_=xc[b2]), [dxc[b2]], [Dep()])
            else:
                self.rmsnorm(xc[b2], dxc[b2], gfin, yT, dyT, NC, tmpn)
                for t2 in range(2):
                    for fq in range(2):
                        ps, dps = self.bank()
                        fns = [lambda e, i=i, t2=t2, fq=fq, ps=ps: e.transpose(
                            ps[:, 128 * i:128 * i + 128], yT[:, 4 * fq + i, t2 * 128:(t2 + 1) * 128], self.ident) for i in range(4)]
                        P.group("pe", fns, [dyT, self.dconst], [dps])
                        self.evac(OT[:, t2, fq * 512:(fq + 1) * 512], ps, [dps], [dOT])
                P.dma("sp", lambda e, c=c: e.dma_start(out=outv[c], in_=OT), [dOT], [Dep()])
        P.barrier()
        A.release(m)

    def _s5_setup(self, lam_re, lam_im, log_dt, b_re, b_im, c_re, c_im, WS_sb, WY_sb, KB_sb, V12, II, dS5):
        nc, P, A = self.nc, self.P, self.A
        ident, ident_bf, dconst = self.ident, self.ident_bf, self.dconst

        def tl(shape, dt=F32):
            return (A.alloc(shape, dt), Dep())

        def tt(o, a, b, op, eng="dve"):
            P.op(eng, lambda e: e.tensor_tensor(out=o[0], in0=a[0], in1=b[0], op=op), [a[1], b[1]], [o[1]])

        def ts(o, a, s1, op0, s2=None, op1=None, extra=()):
            if op1 is None:
                P.op("dve", lambda e: e.tensor_scalar(out=o[0], in0=a[0], scalar1=s1, scalar2=None, op0=op0), [a[1]] + list(extra), [o[1]])
            else:
                P.op("dve", lambda e: e.tensor_scalar(out=o[0], in0=a[0], scalar1=s1, scalar2=s2, op0=op0, op1=op1),
                     [a[1]] + list(extra), [o[1]])

        def stt(o, a, sc, b, op0, op1, extra=()):
            P.op("dve", lambda e: e.scalar_tensor_tensor(out=o[0], in0=a[0], scalar=sc, in1=b[0], op0=op0, op1=op1),
                 [a[1], b[1]] + list(extra), [o[1]])

        def act(o, a, func, scale=1.0):
            P.op("act", lambda e: e.activation(out=o[0], in_=a[0], func=func, scale=scale), [a[1]], [o[1]])

        def sub(t, ap):
            return (ap, t[1])

        LT = tl([48, 3, 128])
        LD = tl([48, 1])
        for h in range(2):
            P.dma("sp", lambda e, h=h: e.dma_start(out=LT[0][:, 0, 64 * h:64 * h + 64], in_=lam_re), [], [LT[1]])
            P.dma("sp", lambda e, h=h: e.dma_start(out=LT[0][:, 1, 64 * h:64 * h + 64], in_=lam_im), [], [LT[1]])
        P.dma("sp", lambda e: e.dma_start(out=LD[0], in_=log_dt.rearrange("(g o) -> g o", o=1)), [], [LD[1]])
        P.op("dve", lambda e: e.tensor_copy(out=LT[0][:, 2, :], in_=LD[0].to_broadcast([48, 128])), [LD[1]], [LT[1]])
        BR, BI, CR, CI = tl([128, 48, 16]), tl([128, 48, 16]), tl([128, 48, 16]), tl([128, 48, 16])
        for (dst, src) in ((BR, b_re), (BI, b_im)):
            for h in range(2):
                for gq in range(4):
                    P.dma("sp", lambda e, dst=dst, src=src, h=h, gq=gq: e.dma_start(
                        out=dst[0][64 * h:64 * h + 64, 12 * gq:12 * gq + 12, :],
                        in_=src[12 * gq:12 * gq + 12].rearrange("g p c -> p g c")), [], [dst[1]])
        CT = [tl([128, 6, 2, 64]), tl([128, 6, 2, 64])]
        for i, src in enumerate((c_re, c_im)):
            for h in range(2):
                P.dma("sp", lambda e, i=i, src=src, h=h: e.dma_start(
                    out=CT[i][0][:, :, h, :], in_=src.rearrange("(o g) c p -> (g c) o p", g=8)), [], [CT[i][1]])
        lr, li, ldt = tl([128, 48]), tl([128, 48]), tl([128, 48])
        ps, dps = self.bank()
        fns = [lambda e, i=i, ps=ps: e.transpose(ps[:, 64 * i:64 * i + 48], LT[0][:, i, :], ident[0:48, 0:48]) for i in range(3)]
        P.group("pe", fns, [LT[1], dconst], [dps])
        for i, t in enumerate((lr, li, ldt)):
            P.op("dve", lambda e, i=i, t=t, ps=ps: e.tensor_copy(out=t[0], in_=ps[:, 64 * i:64 * i + 48]), [dps], [t[1]])
        for i, dst in enumerate((CR, CI)):
            for half in range(2):
                ps, dps = self.bank()
                os_ = list(range(4)) if half == 0 else [4, 5]
                fns = [lambda e, o=o, i=i, ps=ps: e.transpose(ps[:, 128 * (o % 4):128 * (o % 4) + 128],
                                                              CT[i][0][:, o, :, :].rearrange("p a b -> p (a b)"), ident) for o in os_]
                P.group("pe", fns, [CT[i][1], dconst], [dps])
                n = len(os_) * 128
                o0 = os_[0]
                P.op("dve", lambda e, dst=dst, ps=ps, n=n, o0=o0: e.tensor_copy(
                    out=dst[0][:, 8 * o0:8 * o0 + n // 16, :].rearrange("p g c -> p (g c)"), in_=ps[:, 0:n]), [dps], [dst[1]])
        mtop, mbot, nmbot, nmtop = tl([128, 1]), tl([128, 1]), tl([128, 1]), tl([128, 1])
        for t, (v0, v1) in ((mtop, (1.0, 0.0)), (mbot, (0.0, 1.0)), (nmbot, (0.0, -1.0)), (nmtop, (-1.0, 0.0))):
            P.op("pool", lambda e, t=t, v0=v0: e.memset(t[0][0:64, :], v0), [], [t[1]])
            P.op("pool", lambda e, t=t, v1=v1: e.memset(t[0][64:128, :], v1), [], [t[1]])
        G = tl([8, 128])
        G2 = tl([4, 128])
        P.op("pool", lambda e: e.memset(G[0], 1.0), [], [G[1]])
        P.op("pool", lambda e: e.affine_select(out=G[0], in_=G[0], pattern=[[1, 128]], compare_op=ALU.is_ge, fill=0.0,
                                                base=0, channel_multiplier=-16), [G[1]], [G[1]])
        P.op("pool", lambda e: e.affine_select(out=G[0], in_=G[0], pattern=[[-1, 128]], compare_op=ALU.is_ge, fill=0.0,
                                                base=15, channel_multiplier=16), [G[1]], [G[1]])
        P.op("pool", lambda e: e.memset(G2[0], 1.0), [], [G2[1]])
        P.op("pool", lambda e: e.affine_select(out=G2[0], in_=G2[0], pattern=[[1, 128]], compare_op=ALU.is_ge, fill=0.0,
                                                base=0, channel_multiplier=-32), [G2[1]], [G2[1]])
        P.op("pool", lambda e: e.affine_select(out=G2[0], in_=G2[0], pattern=[[-1, 128]], compare_op=ALU.is_ge, fill=0.0,
                                                base=15, channel_multiplier=32), [G2[1]], [G2[1]])
        onec = tl([4, 1])
        P.op("pool", lambda e: e.memset(onec[0], 1.0), [], [onec[1]])
        BD, me, mo = tl([128, 128]), tl([128, 1]), tl([128, 1])
        ps, dps = self.bank()
        P.op("pe", lambda e, ps=ps: e.matmul(ps[:, 0:128], lhsT=G[0], rhs=G[0], start=True, stop=True), [G[1]], [dps])
        P.op("dve", lambda e, ps=ps: e.tensor_copy(out=BD[0], in_=ps[:, 0:128]), [dps], [BD[1]])
        ps, dps = self.bank()
        P.op("pe", lambda e, ps=ps: e.matmul(ps[:, 0:1], lhsT=G2[0], rhs=onec[0], start=True, stop=True), [G2[1], onec[1]], [dps])
        P.op("dve", lambda e, ps=ps: e.tensor_copy(out=me[0], in_=ps[:, 0:1]), [dps], [me[1]])
        ts(mo, me, -1.0, ALU.mult, 1.0, ALU.add)
        P.op("dve", lambda e: e.tensor_tensor(out=II, in0=ident_bf[:, 0:64], in1=ident_bf[:, 64:128], op=ALU.add), [dconst], [dS5])
        dt, th, lrd, mag = tl([128, 48]), tl([128, 48]), tl([128, 48]), tl([128, 48])
        act(dt, ldt, AF.Exp)
        tt(th, li, dt, ALU.mult)
        tt(lrd, lr, dt, ALU.mult)
        act(mag, lrd, AF.Exp)
        sn, cs = tl([128, 48]), tl([128, 48])
        t1, t2, t3 = tl([128, 48]), tl([128, 48]), tl([128, 48])
        for (dst, shift) in ((sn, 0.0), (cs, math.pi / 2)):
            ts(t1, th, shift, ALU.add)
            ts(t2, t1, 1.0 / (2 * math.pi), ALU.mult)
            ts(t3, t2, MAGIC, ALU.add)
            ts(t2, t3, MAGIC, ALU.subtract)
            stt(t3, t2, -2.0 * math.pi, t1, ALU.mult, ALU.add)
            ts(t1, t3, 3.1415925, ALU.min, -3.1415925, ALU.max)
            act(dst, t1, AF.Sin)
        ar, ai = tl([128, 48]), tl([128, 48])
        tt(ar, mag, cs, ALU.mult)
        tt(ai, mag, sn, ALU.mult)
        den, rden, nr, fr, fi = tl([128, 48]), tl([128, 48]), tl([128, 48]), tl([128, 48]), tl([128, 48])
        tt(t1, lr, lr, ALU.mult)
        tt(t2, li, li, ALU.mult)
        tt(den, t1, t2, ALU.add)
        P.op("dve", lambda e: e.reciprocal(out=rden[0], in_=den[0]), [den[1]], [rden[1]])
        ts(nr, ar, -1.0, ALU.add)
        tt(t1, nr, lr, ALU.mult)
        tt(t2, ai, li, ALU.mult)
        tt(t3, t1, t2, ALU.add)
        tt(fr, t3, rden, ALU.mult)
        tt(t1, ai, lr, ALU.mult)
        tt(t2, nr, li, ALU.mult)
        tt(t3, t1, t2, ALU.subtract)
        tt(fi, t3, rden, ALU.mult)

        def bc(t):
            return (t[0].unsqueeze(2).to_broadcast([128, 48, 16]), t[1])

        bbr, bbi, u1, u2 = tl([128, 48, 16]), tl([128, 48, 16]), tl([128, 48, 16]), tl([128, 48, 16])
        tt(u1, BR, bc(fr), ALU.mult)
        tt(u2, BI, bc(fi), ALU.mult)
        tt(bbr, u1, u2, ALU.subtract)
        tt(u1, BI, bc(fr), ALU.mult)
        tt(u2, BR, bc(fi), ALU.mult)
        tt(bbi, u1, u2, ALU.add)
        PWr, PWi = tl([128, 48, 9]), tl([128, 48, 9])
        P.op("pool", lambda e: e.memset(PWr[0][:, :, 0:1], 1.0), [], [PWr[1]])
        P.op("pool", lambda e: e.memset(PWi[0][:, :, 0:1], 0.0), [], [PWi[1]])
        P.op("dve", lambda e: e.tensor_copy(out=PWr[0][:, :, 1], in_=ar[0]), [ar[1]], [PWr[1]])
        P.op("dve", lambda e: e.tensor_copy(out=PWi[0][:, :, 1], in_=ai[0]), [ai[1]], [PWi[1]])
        for n in range(1, 8):
            pr, pi = sub(PWr, PWr[0][:, :, n]), sub(PWi, PWi[0][:, :, n])
            tt(t1, pr, ar, ALU.mult)
            tt(t2, pi, ai, ALU.mult)
            tt(sub(PWr, PWr[0][:, :, n + 1]), t1, t2, ALU.subtract)
            tt(t1, pr, ai, ALU.mult)
            tt(t2, pi, ar, ALU.mult)
            tt(sub(PWi, PWi[0][:, :, n + 1]), t1, t2, ALU.add)
        SQr, SQi = tl([128, 48, 9]), tl([128, 48, 9])
        P.op("dve", lambda e: e.tensor_copy(out=SQr[0][:, :, 0], in_=PWr[0][:, :, 8]), [PWr[1]], [SQr[1]])
        P.op("dve", lambda e: e.tensor_copy(out=SQi[0][:, :, 0], in_=PWi[0][:, :, 8]), [PWi[1]], [SQi[1]])
        for j in range(8):
            r, i_ = sub(SQr, SQr[0][:, :, j]), sub(SQi, SQi[0][:, :, j])
            tt(t1, r, r, ALU.mult)
            tt(t2, i_, i_, ALU.mult)
            tt(sub(SQr, SQr[0][:, :, j + 1]), t1, t2, ALU.subtract)
            tt(t1, r, i_, ALU.mult)
            ts(sub(SQi, SQi[0][:, :, j + 1]), t1, 2.0, ALU.mult)
        w1 = tl([128, 48, 9])
        V12t = (V12, dS5)
        ts(w1, SQi, nmbot[0], ALU.mult, extra=[nmbot[1]])
        stt(sub(V12t, V12[:, :, :, 0]), SQr, mtop[0], w1, ALU.mult, ALU.add, extra=[mtop[1]])
        ts(w1, SQr, mbot[0], ALU.mult, extra=[mbot[1]])
        stt(sub(V12t, V12[:, :, :, 1]), SQi, mtop[0], w1, ALU.mult, ALU.add, extra=[mtop[1]])
        P1, P2, Q1, Q2 = tl([128, 48, 9]), tl([128, 48, 9]), tl([128, 48, 9]), tl([128, 48, 9])
        ts(w1, PWi, nmbot[0], ALU.mult, extra=[nmbot[1]])
        stt(P1, PWr, mtop[0], w1, ALU.mult, ALU.add, extra=[mtop[1]])
        ts(w1, PWr, nmbot[0], ALU.mult, extra=[nmbot[1]])
        stt(P2, PWi, nmtop[0], w1, ALU.mult, ALU.add, extra=[nmtop[1]])
        ts(w1, PWi, mbot[0], ALU.mult, extra=[mbot[1]])
        stt(Q1, PWr, mtop[0], w1, ALU.mult, ALU.add, extra=[mtop[1]])
        ts(w1, PWr, mbot[0], ALU.mult, extra=[mbot[1]])
        stt(Q2, PWi, nmtop[0], w1, ALU.mult, ALU.add, extra=[nmtop[1]])
        CA = tl([128, 9, 48, 16], BF16)
        WSb = tl([128, 8, 48, 16], BF16)
        for n in range(9):
            tt(u1, CR, (P1[0][:, :, n:n + 1].to_broadcast([128, 48, 16]), P1[1]), ALU.mult)
            tt(u2, CI, (P2[0][:, :, n:n + 1].to_broadcast([128, 48, 16]), P2[1]), ALU.mult)
            tt(sub(CA, CA[0][:, n, :, :]), u1, u2, ALU.add)
        for sl in range(8):
            n = 7 - sl
            tt(u1, bbr, (Q1[0][:, :, n:n + 1].to_broadcast([128, 48, 16]), Q1[1]), ALU.mult)
            tt(u2, bbi, (Q2[0][:, :, n:n + 1].to_broadcast([128, 48, 16]), Q2[1]), ALU.mult)
            tt(sub(WSb, WSb[0][:, sl, :, :]), u1, u2, ALU.add)
        BBs = tl([128, 48, 16], BF16)
        ts(u1, bbi, mbot[0], ALU.mult, extra=[mbot[1]])
        stt(BBs, bbr, mtop[0], u1, ALU.mult, ALU.add, extra=[mtop[1]])
        tmpk = tl([128, 128])
        for o in range(6):
            for half in range(2):
                ps, dps = self.bank()
                fns = [lambda e, o=o, lag=lag, ps=ps: e.matmul(
                    ps[:, 128 * (lag % 4):128 * (lag % 4) + 128], lhsT=BBs[0][:, 8 * o:8 * o + 8, :].rearrange("p g c -> p (g c)"),
                    rhs=CA[0][:, lag, 8 * o:8 * o + 8, :].rearrange("p g c -> p (g c)"), start=True, stop=True)
                    for lag in range(4 * half, 4 * half + 4)]
                P.group("pe", fns, [BBs[1], CA[1]], [dps])
                for lag in range(4 * half, 4 * half + 4):
                    src = ps[:, 128 * (lag % 4):128 * (lag % 4) + 128]
                    if lag == 0:
                        P.op("dve", lambda e, src=src: e.tensor_tensor(out=tmpk[0], in0=src, in1=BD[0], op=ALU.mult),
                             [dps, BD[1]], [tmpk[1]])
                        P.op("dve", lambda e, o=o: e.scalar_tensor_tensor(
                            out=KB_sb[:, o, 0, :], in0=ident, scalar=self.gains[:, 48 + o:49 + o], in1=tmpk[0],
                            op0=ALU.mult, op1=ALU.add), [tmpk[1], dconst], [dS5])
                    else:
                        P.op("dve", lambda e, src=src, o=o, lag=lag: e.tensor_tensor(out=KB_sb[:, o, lag, :], in0=src, in1=BD[0], op=ALU.mult),
                             [dps, BD[1]], [dS5])
        for o in range(6):
            ps, dps = self.bank()
            psb = ps.bitcast(BF16)
            fns = [lambda e, o=o, sl=sl, psb=psb: e.transpose(
                psb[:, 128 * sl:128 * sl + 128], WSb[0][:, sl, 8 * o:8 * o + 8, :].rearrange("p g c -> p (g c)"), ident_bf)
                for sl in range(8)]
            P.group("pe", fns, [WSb[1], dconst], [dps])
            for par, m in ((0, me), (1, mo)):
                P.op("dve", lambda e, o=o, par=par, m=m, psb=psb: e.tensor_scalar(
                    out=WS_sb[:, o, :, par, :], in0=psb.rearrange("p (s n) -> p s n", s=8), scalar1=m[0], scalar2=None, op0=ALU.mult),
                    [dps, m[1]], [dS5])
        P.op("pool", lambda e: e.memset(WY_sb.rearrange("p g s c -> p (g s c)"), 0.0), [], [dS5])
        CAv = CA[0].rearrange("p n (g two) c -> p n g two c", two=2)
        WYv = WY_sb.rearrange("p (g two) s c -> p g two s c", two=2)
        for sl in range(8):
            for par in range(2):
                P.op("dve", lambda e, sl=sl, par=par: e.tensor_copy(
                    out=WYv[:, :, par, sl, 16 * par:16 * par + 16], in_=CAv[:, sl + 1, :, par, :]), [CA[1]], [dS5])

    def _finish_dbg(self, sb):
        P, nc = self.P, self.nc
        for name, (ap, dep, shape, dt) in sb.items():
            o = nc.dram_tensor("dbg_" + name, list(shape), dt, kind="ExternalOutput").ap()
            self.outs.append("dbg_" + name)
            P.dma("sp", lambda e, o=o, ap=ap: e.dma_start(out=o, in_=ap), [dep], [Dep()])
        P.finish()
        return nc


_INPUT_NAMES = ["x", "mem", "mem_norm_g", "w_mem_kv", "mix_norm_g", "s5_w_in", "s5_lambda_re", "s5_lambda_im", "s5_log_dt",
                "s5_b_re", "s5_b_im", "s5_c_re", "s5_c_im", "s5_d", "s5_w_glu", "s5_b_glu", "moba_w_in", "w_out",
                "ffn_norm_g", "w_gate", "w_up", "w_down", "final_norm_g"]


def make_in_maps(inputs, ncores=8):
    maps = []
    shared = {}
    for k in _INPUT_NAMES:
        if k in ("x", "mem"):
            continue
        a = np.ascontiguousarray(np.asarray(inputs[k], dtype=np.float32))
        if k in ("s5_w_in", "s5_lambda_re", "s5_lambda_im", "s5_log_dt", "s5_b_re", "s5_b_im", "s5_c_re", "s5_c_im",
                 "s5_d", "s5_w_glu", "s5_b_glu", "moba_w_in"):
            a = a[0]
        shared[k] = np.ascontiguousarray(a)
    xs = np.asarray(inputs["x"], dtype=np.float32)
    ms = np.asarray(inputs["mem"], dtype=np.float32)
    for c in range(ncores):
        m = dict(shared)
        m["x"] = np.ascontiguousarray(xs[c])
        m["mem"] = np.ascontiguousarray(ms[c])
        maps.append(m)
    return maps


def kernel(**inputs):
    b = Builder()
    nc = b.build()
    res = run_bass_kernel_spmd(nc, make_in_maps(inputs), core_ids=list(range(8)))
    return np.stack([np.asarray(r["out"]) for r in res.results], axis=0).astype(np.float32)
```

```python
import math
import numpy as np
import concourse.bass as bass
import concourse.mybir as mybir
from concourse.bass_utils import run_bass_kernel_spmd

F32 = mybir.dt.float32
BF16 = mybir.dt.bfloat16
I32 = mybir.dt.int32
U8 = mybir.dt.uint8
AF = mybir.ActivationFunctionType
ALU = mybir.AluOpType
AX = mybir.AxisListType

T = 4096
D = 1024
NEG = -30000.0
MAGIC = 12582912.0


class Dep:
    __slots__ = ("w", "r")

    def __init__(self):
        self.w = None
        self.r = []


class Prog:
    ENGS = ("pe", "act", "dve", "pool", "sp")

    def __init__(self, nc, n_dma_sems=44):
        self.nc = nc
        self.streams = {e: [] for e in self.ENGS}
        self.sem = {e: nc.alloc_semaphore("s_" + e) for e in self.ENGS if e != "sp"}
        self.cnt = {e: 0 for e in self.ENGS}
        self.known = {e: {} for e in self.ENGS}
        self.dsems = [nc.alloc_semaphore("d%d" % i) for i in range(n_dma_sems)]
        self.dcnt = [0] * n_dma_sems
        n_sw = 12
        self.dq = {"pool": list(range(n_sw)), "sp": list(range(n_sw, n_dma_sems))}
        self.dqn = {"pool": 0, "sp": 0}
        self.tg = 0

    def _waits(self, eng, reads, writes):
        w = {}

        def add(t):
            if t is None:
                return
            s, v = t
            k = id(s)
            if k not in w or w[k][1] < v:
                w[k] = (s, v)

        for d in reads:
            add(d.w)
        for d in writes:
            add(d.w)
            for t in d.r:
                add(t)
        out = []
        kn = self.known[eng]
        own = id(self.sem["pe"]) if eng == "pe" else None
        for k, (s, v) in w.items():
            if k == own or kn.get(k, 0) >= v:
                continue
            kn[k] = v
            out.append((s, v))
        return out

    def _commit(self, tok, reads, writes):
        for d in reads:
            d.r.append(tok)
            if len(d.r) > 64:
                d.r = _compress(d.r)
        for d in writes:
            d.w = tok
            d.r = []

    def op(self, eng, fn, reads=(), writes=()):
        waits = self._waits(eng, reads, writes)
        self.cnt[eng] += 1
        tok = (self.sem[eng], self.cnt[eng])
        self.streams[eng].append((fn, waits, tok, 1))
        self._commit(tok, reads, writes)
        return tok

    def group(self, eng, fns, reads=(), writes=()):
        n = len(fns)
        if n == 1:
            return self.op(eng, fns[0], reads, writes)
        waits = self._waits(eng, reads, writes)
        self.streams[eng].append((fns[0], waits, None, 0))
        for fn in fns[1:-1]:
            self.streams[eng].append((fn, [], None, 0))
        self.cnt[eng] += 1
        tok = (self.sem[eng], self.cnt[eng])
        self.streams[eng].append((fns[-1], [], tok, 1))
        self._commit(tok, reads, writes)
        return tok

    def dma(self, q, fn, reads=(), writes=()):
        ql = self.dq[q]
        i = ql[self.dqn[q] % len(ql)]
        self.dqn[q] += 1
        s = self.dsems[i]
        waits = self._waits(q, reads, writes)
        if self.dcnt[i] > 0:
            kn = self.known[q]
            if kn.get(id(s), 0) < self.dcnt[i]:
                kn[id(s)] = self.dcnt[i]
                waits.append((s, self.dcnt[i]))
        self.dcnt[i] += 16
        tok = (s, self.dcnt[i])
        self.streams[q].append((fn, waits, tok, 16))
        self._commit(tok, reads, writes)
        return tok

    def barrier(self):
        toks = [(self.sem[e], self.cnt[e]) for e in self.sem if self.cnt[e] > 0]
        toks += [(s, c) for s, c in zip(self.dsems, self.dcnt) if c > 0]
        for e in self.ENGS:
            kn = self.known[e]
            ws = []
            for s, v in toks:
                if kn.get(id(s), 0) < v:
                    kn[id(s)] = v
                    ws.append((s, v))
            if ws:
                self.streams[e].append((None, ws, None, 0))

    def finish(self):
        self.barrier()
        nc = self.nc
        streams = self.streams

        def replay(name, e):
            for fn, waits, tok, inc in streams[name]:
                for s, v in waits:
                    e.wait_ge(s, v)
                if fn is None:
                    continue
                ins = fn(e)
                if tok is not None:
                    ins.then_inc(tok[0], inc)

        with nc.Block() as block:
            @block.tensor
            def _(e):
                replay("pe", e)

            @block.scalar
            def _(e):
                replay("act", e)

            @block.vector
            def _(e):
                replay("dve", e)

            @block.gpsimd
            def _(e):
                replay("pool", e)

            @block.sync
            def _(e):
                replay("sp", e)


def _compress(toks):
    best = {}
    for s, v in toks:
        k = id(s)
        if k not in best or best[k][1] < v:
            best[k] = (s, v)
    return list(best.values())


_ES = {F32: 4, BF16: 2, I32: 4}


class Arena:
    def __init__(self, nc, nbytes):
        self.base = nc.alloc_sbuf_tensor("arena", [128, nbytes], U8).ap()
        self.n = nbytes
        self.limit = nbytes
        self.off = 0

    def alloc(self, shape, dt):
        es = _ES[dt]
        free = 1
        for s in shape[1:]:
            free *= s
        nb = free * es
        off = (self.off + 63) // 64 * 64
        assert off + nb <= self.limit, ("arena overflow", off, nb, self.limit)
        self.off = off + nb
        ap = self.base[0:shape[0], off:off + nb].bitcast(dt)
        if len(shape) == 3:
            ap = ap.rearrange("p (a b) -> p a b", a=shape[1])
        elif len(shape) == 4:
            ap = ap.rearrange("p (a b c) -> p a b c", a=shape[1], b=shape[2])
        elif len(shape) == 5:
            ap = ap.rearrange("p (a b c d) -> p a b c d", a=shape[1], b=shape[2], c=shape[3])
        return ap

    def alloc_top(self, shape, dt):
        es = _ES[dt]
        free = 1
        for s in shape[1:]:
            free *= s
        nb = (free * es + 63) // 64 * 64
        self.limit -= nb
        assert self.limit >= self.off, ("arena top overflow", self.limit, self.off)
        off = self.limit
        ap = self.base[0:shape[0], off:off + free * es].bitcast(dt)
        if len(shape) == 3:
            ap = ap.rearrange("p (a b) -> p a b", a=shape[1])
        return ap

    def release_top(self):
        self.limit = self.n

    def mark(self):
        return self.off

    def release(self, m):
        self.off = m


class Builder:
    def __init__(self, dbg=(), stop=None):
        self.dbg = set(dbg)
        self.stop = stop
        nc = self.nc = bass.Bass("TRN2", target_bir_lowering=False)
        self.P = Prog(nc)
        self.A = Arena(nc, 207 * 1024)
        self.banks = [nc.alloc_psum_tensor("psb%d" % i, [128, 512], F32).ap() for i in range(8)]
        self.bdep = [Dep() for _ in range(8)]
        self.bnext = 0
        self.bank_set = list(range(8))
        self.etog = 0
        self.inp = {}
        self.outs = []

    def din(self, name, shape):
        ap = self.nc.dram_tensor(name, list(shape), F32, kind="ExternalInput").ap()
        self.inp[name] = ap
        return ap

    def scratch(self, name, shape, dt):
        if name in self.dbg:
            self.outs.append(name)
            return self.nc.dram_tensor(name, list(shape), dt, kind="ExternalOutput").ap()
        return self.nc.dram_tensor(name, list(shape), dt).ap()

    def bank(self):
        bs = self.bank_set
        i = bs[self.bnext % len(bs)]
        self.bnext += 1
        return self.banks[i], self.bdep[i]

    def evac(self, out, in_, reads, writes, scale=None, eng=None):
        P = self.P
        if eng is None:
            eng = "act" if self.etog == 0 else "dve"
            self.etog ^= 1
        if eng == "act":
            sc = 1.0 if scale is None else scale
            return P.op("act", lambda e: e.activation(out=out, in_=in_, func=AF.Copy, scale=sc), reads, writes)
        if scale is None:
            return P.op(eng, lambda e: e.tensor_copy(out=out, in_=in_), reads, writes)
        return P.op(eng, lambda e: e.tensor_scalar(out=out, in0=in_, scalar1=scale, scalar2=None, op0=ALU.mult), reads, writes)

    def load_w(self, dst, dd, src2d, kt, ncols, c0=0, r0=0):
        P = self.P
        for k in range(kt):
            for c in range(0, ncols, 2048):
                cw = min(2048, ncols - c)
                P.dma("pool", lambda e, k=k, c=c, cw=cw: e.dma_start(
                    out=dst[:, k, c:c + cw], in_=src2d[r0 + k * 128:r0 + (k + 1) * 128, c0 + c:c0 + c + cw]),
                    writes=[dd])

    def rmsnorm(self, xT, dx, gcol, out, dout, n, tmp):
        P = self.P
        sq, dsq, rt, drt, rs, drs = tmp
        P.op("act", lambda e: e.activation(out=sq[:, :, 0:n], in_=xT, func=AF.Square), [dx], [dsq])
        ps, dps = self.bank()
        fns = [lambda e, f=f: e.matmul(ps[:, 0:n], lhsT=self.ones_bf, rhs=sq[:, f, 0:n], start=(f == 0), stop=(f == 7))
               for f in range(8)]
        P.group("pe", fns, [dsq, self.dconst], [dps])
        P.op("act", lambda e: e.activation(out=rt[:, 0:n], in_=ps[:, 0:n], func=AF.Sqrt, bias=self.eps_col, scale=1.0 / D),
             [dps, self.dconst], [drt])
        P.op("dve", lambda e: e.reciprocal(out=rs[:, 0:n], in_=rt[:, 0:n]), [drt], [drs])
        for f in range(8):
            P.op("dve", lambda e, f=f: e.scalar_tensor_tensor(
                out=out[:, f, :], in0=xT[:, f, :], scalar=self.gains[:, gcol + f:gcol + f + 1], in1=rs[:, 0:n],
                op0=ALU.mult, op1=ALU.mult), [dx, drs, self.dconst], [dout])

    def rms_tmp(self, n):
        A = self.A
        return (A.alloc([128, 8, n], BF16), Dep(), A.alloc([128, n], F32), Dep(), A.alloc([128, n], F32), Dep())

    def build(self):
        nc, P, A = self.nc, self.P, self.A
        din = self.din
        x = din("x", [T, D])
        mem = din("mem", [256, D])
        mem_norm_g = din("mem_norm_g", [D])
        w_mem_kv = din("w_mem_kv", [D, 512])
        mix_norm_g = din("mix_norm_g", [2, D])
        s5_w_in = din("s5_w_in", [D, D])
        lam_re = din("s5_lambda_re", [48, 64])
        lam_im = din("s5_lambda_im", [48, 64])
        log_dt = din("s5_log_dt", [48])
        b_re = din("s5_b_re", [48, 64, 16])
        b_im = din("s5_b_im", [48, 64, 16])
        c_re = din("s5_c_re", [48, 16, 64])
        c_im = din("s5_c_im", [48, 16, 64])
        s5_d = din("s5_d", [768])
        w_glu = din("s5_w_glu", [768, 768])
        b_glu = din("s5_b_glu", [768])
        moba_w_in = din("moba_w_in", [D, 2560])
        w_out = din("w_out", [2, D, D])
        ffn_norm_g = din("ffn_norm_g", [2, D])
        w_gate = din("w_gate", [2, D, 2816])
        w_up = din("w_up", [2, D, 2816])
        w_down = din("w_down", [2, 2816, D])
        final_norm_g = din("final_norm_g", [D])
        out = nc.dram_tensor("out", [T, D], F32, kind="ExternalOutput").ap()
        self.outs.append("out")

        XT0 = self.scratch("XT0", [8, 128, T], F32)
        XT1 = self.scratch("XT1", [8, 128, T], F32)
        XT2 = self.scratch("XT2", [8, 128, T], F32)
        XT3 = self.scratch("XT3", [8, 128, T], F32)
        ATD = [self.scratch("ATD%d" % l, [8, 128, T], BF16) for l in range(2)]
        HTD = [self.scratch("HTD%d" % l, [8, 128, T], BF16) for l in range(2)]
        YTD = self.scratch("YTD", [6, 128, T], BF16)
        QMD = self.scratch("QMD", [2, 128, T], BF16)
        ALD = self.scratch("ALD", [12, 3, T], BF16)
        NALD = self.scratch("NALD", [12, 3, T], BF16)

        self.DEND = self.scratch("DEND", [4, 512], F32)
        self.dconst = dconst = Dep()
        ident = A.alloc([128, 128], F32)
        ident_bf = A.alloc([128, 128], BF16)
        self.ones_bf = ones_bf = A.alloc([128, 128], BF16)
        self.ones_f = ones_f = A.alloc([128, 64], F32)
        self.eps_col = eps_col = A.alloc([128, 1], F32)
        self.gains = gains = A.alloc([128, 64], F32)
        memKT = A.alloc([128, 2, 256], BF16)
        memVA = A.alloc([128, 2, 4, 65], BF16)
        dmemK, dmemV = Dep(), Dep()
        self.ident, self.ident_bf = ident, ident_bf

        P.op("pool", lambda e: e.memset(ident, 0.0), [], [dconst])
        P.op("pool", lambda e: e.affine_select(out=ident, in_=ident, pattern=[[-1, 128]], compare_op=ALU.not_equal,
                                                fill=1.0, base=0, channel_multiplier=1), [dconst], [dconst])
        P.op("pool", lambda e: e.tensor_copy(out=ident_bf, in_=ident), [dconst], [dconst])
        P.op("pool", lambda e: e.memset(ones_bf, 1.0), [], [dconst])
        P.op("pool", lambda e: e.memset(ones_f, 1.0), [], [dconst])
        P.op("pool", lambda e: e.memset(eps_col, 1e-6), [], [dconst])

        m0 = A.mark()
        grow = A.alloc([64, 128], F32)
        dgrow = Dep()
        P.op("pool", lambda e: e.memset(grow, 0.0), [], [dgrow])
        srcs = [mix_norm_g[0], mix_norm_g[1], ffn_norm_g[0], ffn_norm_g[1], final_norm_g, mem_norm_g]
        for i, s in enumerate(srcs):
            P.dma("sp", lambda e, i=i, s=s: e.dma_start(out=grow[8 * i:8 * i + 8, :], in_=s.rearrange("(f p) -> f p", p=128)),
                  [], [dgrow])
        P.dma("sp", lambda e: e.dma_start(out=grow[48:54, :], in_=s5_d.rearrange("(f p) -> f p", p=128)), [], [dgrow])
        P.dma("sp", lambda e: e.dma_start(out=grow[54:60, :], in_=b_glu.rearrange("(f p) -> f p", p=128)), [], [dgrow])
        ps, dps = self.bank()
        P.op("pe", lambda e: e.transpose(ps[:, 0:64], grow, ident[0:64, 0:64]), [dgrow, dconst], [dps])
        P.op("dve", lambda e: e.tensor_copy(out=gains, in_=ps[:, 0:64]), [dps], [dconst])

        memt = A.alloc([128, 2, D], F32)
        dmemt = Dep()
        P.dma("sp", lambda e: e.dma_start(out=memt, in_=mem.rearrange("(m p) f -> p m f", p=128)), [], [dmemt])
        wkv = A.alloc([128, 8, 512], BF16)
        dwkv = Dep()
        self.load_w(wkv, dwkv, w_mem_kv, 8, 512)
        memT = A.alloc([128, 8, 256], F32)
        dmemT = Dep()
        for fp in range(4):
            ps, dps = self.bank()
            fns = []
            for fl in range(2):
                for m in range(2):
                    f = 2 * fp + fl
                    fns.append(lambda e, f=f, m=m, fl=fl, ps=ps: e.transpose(
                        ps[:, fl * 256 + m * 128: fl * 256 + (m + 1) * 128], memt[:, m, f * 128:(f + 1) * 128], ident))
            P.group("pe", fns, [dmemt, dconst], [dps])
            self.evac(memT[:, 2 * fp:2 * fp + 2, :].rearrange("p a b -> p (a b)"), ps, [dps], [dmemT])
        tmpn = self.rms_tmp(256)
        hmT = A.alloc([128, 8, 256], BF16)
        dhm = Dep()
        self.rmsnorm(memT, dmemT, 40, hmT, dhm, 256, tmpn)
        for hp in range(2):
            ps, dps = self.bank()
            fns = [lambda e, k=k, hp=hp, ps=ps: e.matmul(ps[:, 0:256], lhsT=wkv[:, k, hp * 128:(hp + 1) * 128], rhs=hmT[:, k, :],
                                                          start=(k == 0), stop=(k == 7)) for k in range(8)]
            P.group("pe", fns, [dwkv, dhm], [dps])
            self.evac(memKT[:, hp, :], ps[:, 0:256], [dps], [dmemK])
        P.op("pool", lambda e: e.memset(memVA[:, :, :, 64:65], 1.0), [], [dmemV])
        for m in range(2):
            ps, dps = self.bank()
            fns = [lambda e, k=k, m=m, ps=ps: e.matmul(ps[:, 0:256], lhsT=hmT[:, k, m * 128:(m + 1) * 128], rhs=wkv[:, k, 256:512],
                                                        start=(k == 0), stop=(k == 7)) for k in range(8)]
            P.group("pe", fns, [dwkv, dhm], [dps])
            self.evac(memVA[:, m, :, 0:64], ps[:, 0:256].rearrange("p (h d) -> p h d", h=4), [dps], [dmemV])
        P.barrier()
        A.release(m0)
        if self.stop == "M":
            return self._finish_dbg({"memKT": (memKT, dmemK, [128, 2, 256], BF16), "memVA": (memVA, dmemV, [128, 2, 4, 65], BF16),
                                     "gains": (gains, dconst, [128, 64], F32)})

        g_mix0, g_mix1, g_ffn0, g_ffn1, g_fin, g_mem = 0, 8, 16, 24, 32, 40
        mU = A.mark()
        UT = A.alloc([128, 6, T], BF16)
        dUT = [[Dep() for _ in range(8)] for _ in range(6)]
        mA = A.mark()
        QM = A.alloc([128, 2, T], BF16)
        dQM = [Dep() for _ in range(8)]
        dQMD = Dep()
        win = A.alloc([128, 8, D], BF16)
        dwin = Dep()
        self.load_w(win, dwin, s5_w_in, 8, D)
        xrow = [A.alloc([128, 4, D], F32) for _ in range(2)]
        dxrow = [Dep(), Dep()]
        xT = [A.alloc([128, 8, 512], F32) for _ in range(2)]
        dxT = [Dep(), Dep()]
        hT = [A.alloc([128, 8, 512], BF16) for _ in range(2)]
        dhT = [Dep(), Dep()]
        tmpn = self.rms_tmp(512)
        xv = x.rearrange("(c j p) f -> c p j f", p=128, j=4)
        UTv = [UT[:, ot, :].rearrange("p (s k) -> p s k", s=8) for ot in range(6)]
        for sl in range(8):
            b2 = sl % 2
            P.dma("sp", lambda e, sl=sl, b2=b2: e.dma_start(out=xrow[b2], in_=xv[sl]), [], [dxrow[b2]])
            for f in range(8):
                ps, dps = self.bank()
                fns = [lambda e, j=j, f=f, ps=ps, b2=b2: e.transpose(ps[:, j * 128:(j + 1) * 128], xrow[b2][:, j, f * 128:(f + 1) * 128], ident)
                       for j in range(4)]
                P.group("pe", fns, [dxrow[b2], dconst], [dps])
                self.evac(xT[b2][:, f, :], ps, [dps], [dxT[b2]])
            P.dma("sp", lambda e, sl=sl, b2=b2: e.dma_start(
                out=XT0[:, :, sl * 512:(sl + 1) * 512].rearrange("f p n -> p f n"), in_=xT[b2]), [dxT[b2]], [Dep()])
            self.rmsnorm(xT[b2], dxT[b2], g_mix0, hT[b2], dhT[b2], 512, tmpn)
            for ot in range(8):
                ps, dps = self.bank()
                fns = [lambda e, k=k, ot=ot, ps=ps, b2=b2: e.matmul(ps, lhsT=win[:, k, ot * 128:(ot + 1) * 128], rhs=hT[b2][:, k, :],
                                                                   start=(k == 0), stop=(k == 7)) for k in range(8)]
                P.group("pe", fns, [dwin, dhT[b2]], [dps])
                if ot < 6:
                    self.evac(UTv[ot][:, :, 64 * sl:64 * sl + 64], ps.rearrange("p (k s) -> p s k", s=8), [dps], dUT[ot])
                else:
                    self.evac(QM[:, ot - 6, sl * 512:(sl + 1) * 512], ps, [dps], [dQM[sl]], scale=0.125)
            P.dma("sp", lambda e, sl=sl: e.dma_start(out=QMD[:, :, sl * 512:(sl + 1) * 512].rearrange("f p n -> p f n"),
                                                     in_=QM[:, :, sl * 512:(sl + 1) * 512]), [dQM[sl]], [dQMD])
        P.barrier()
        A.release(mA)
        if self.stop == "A":
            dall = Dep()
            return self._finish_dbg({"UT": (UT, dall, [128, 6, T], BF16)})

        WS_sb = A.alloc([128, 6, 8, 2, 128], BF16)
        WY_sb = A.alloc([128, 48, 8, 32], BF16)
        KB_sb = A.alloc([128, 6, 8, 128], BF16)
        V12 = A.alloc([128, 48, 9, 2], F32)
        II = A.alloc([128, 64], BF16)
        dS5 = Dep()
        mS = A.mark()
        self._s5_setup(lam_re, lam_im, log_dt, b_re, b_im, c_re, c_im, WS_sb, WY_sb, KB_sb, V12, II, dS5)
        P.barrier()
        A.release(mS)
        if self.stop == "S":
            return self._finish_dbg({"WS": (WS_sb, dS5, [128, 6, 8, 2, 128], BF16), "WY": (WY_sb, dS5, [128, 48, 8, 32], BF16),
                                     "KB": (KB_sb, dS5, [128, 6, 8, 128], BF16), "V12": (V12, dS5, [128, 48, 9, 2], F32)})

        Xab = A.alloc([128, 8, 2, 512], BF16)
        dX = [[Dep(), Dep()] for _ in range(8)]
        XP = A.alloc([128, 2, 8, 512], BF16)
        dXP = [[Dep() for _ in range(8)] for _ in range(2)]
        DG = A.alloc([128, 8, 2, 128], BF16)
        dDG = [[Dep(), Dep()] for _ in range(8)]
        P.op("pool", lambda e: e.memset(XP[:, :, :, 0:1], 0.0), [], [d for r in dXP for d in r])
        def y_group(o, so):
            ob = o % 2
            ps, dps = self.bank()
            fns = []
            for si in range(so + 1):
                fns.append(lambda e, si=si: e.matmul(
                    ps, lhsT=KB_sb[:, o, so - si, :], rhs=UT[:, o, si * 512:(si + 1) * 512], start=(si == 0), stop=False))
            for g8 in range(8):
                j = g8 // 2
                fns.append(lambda e, g8=g8, j=j: e.matmul(
                    ps[32 * j:32 * j + 32, :], lhsT=WY_sb[:, 8 * o + g8, so, :], rhs=XP[:, ob, g8, :],
                    start=False, stop=(g8 == 7), tile_position=(0, 32 * j)))
            P.group("pe", fns, [dS5] + dUT[o][:so + 1] + dXP[ob], [dps])
            P.op("act", lambda e: e.activation(out=UT[:, o, so * 512:(so + 1) * 512], in_=ps, func=AF.Gelu), [dps], [dUT[o][so]])

        for o in range(6):
            ob = o % 2
            ylist = [(o - 1, so) for so in range(7, -1, -1)] if o > 0 else []
            for g8 in range(8):
                j, par = g8 // 2, g8 % 2
                ps, dps = self.bank()
                fns = [lambda e, sl=sl, j=j, par=par, o=o, ps=ps: e.matmul(
                    ps, lhsT=WS_sb[32 * j:32 * j + 32, o, sl, par, :], rhs=UT[32 * j:32 * j + 32, o, sl * 512:(sl + 1) * 512],
                    start=(sl == 0), stop=(sl == 7), tile_position=(32 * j, 0)) for sl in range(8)]
                P.group("pe", fns, [dS5] + dUT[o], [dps])
                self.evac(Xab[:, g8, 0, :], ps, [dps], [dX[g8][0]])
            cur = 0
            for st in range(9):
                dsh = 1 << st
                for g8 in range(8):
                    g = 8 * o + g8
                    db = st % 2
                    P.op("dve", lambda e, g=g, g8=g8, st=st, db=db: e.tensor_scalar(
                        out=DG[:, g8, db, 0:64], in0=II, scalar1=V12[:, g, st, 0:1], scalar2=None, op0=ALU.mult),
                        [dS5], [dDG[g8][db]])
                    P.op("dve", lambda e, g=g, g8=g8, st=st, db=db: e.tensor_scalar(
                        out=DG[:, g8, db, 64:128], in0=II, scalar1=V12[:, g, st, 1:2], scalar2=None, op0=ALU.mult),
                        [dS5], [dDG[g8][db]])
                    ps, dps = self.bank()
                    fns = [lambda e, g8=g8, cur=cur, ps=ps: e.matmul(ps, lhsT=ident_bf, rhs=Xab[:, g8, cur, :], start=True, stop=False),
                           lambda e, g8=g8, cur=cur, ps=ps, dsh=dsh, db=db: e.matmul(
                               ps[:, dsh:512], lhsT=DG[:, g8, db, :], rhs=Xab[:, g8, cur, 0:512 - dsh], start=False, stop=True)]
                    P.group("pe", fns, [dX[g8][cur], dDG[g8][db], dconst], [dps])
                    ev = "dve" if g8 in (3, 7) else "act"
                    if st < 8:
                        self.evac(Xab[:, g8, 1 - cur, :], ps, [dps], [dX[g8][1 - cur]], eng=ev)
                    else:
                        self.evac(XP[:, ob, g8, 1:512], ps[:, 0:511], [dps], [dXP[ob][g8]], eng=ev)
                if ylist:
                    y_group(*ylist.pop(0))
                cur = 1 - cur
            while ylist:
                y_group(*ylist.pop(0))
        for so in range(7, -1, -1):
            y_group(5, so)
        P.barrier()
        A.release(mA)
        if self.stop == "B":
            return self._finish_dbg({"UT": (UT, Dep(), [128, 6, T], BF16)})

        wglu = A.alloc([128, 6, 768], BF16)
        dwglu = Dep()
        self.load_w(wglu, dwglu, w_glu, 6, 768)
        wts = self.ffn_prefetch(w_gate[0], w_up[0])
        self.attn_init(depth=4, obanks=(0, 1, 3), dbbank=2)
        QMc = [A.alloc([128, 2, 512], BF16) for _ in range(2)]
        dQMc = [Dep(), Dep()]
        SG = [A.alloc([128, 512], BF16) for _ in range(2)]
        dSG = [Dep(), Dep()]
        TOKc = [A.alloc([128, 6, 512], BF16) for _ in range(2)]
        dTOK = [Dep(), Dep()]
        dATD = [Dep(), Dep()]
        for sl in range(8):
            b2 = sl % 2
            cs = slice(sl * 512, (sl + 1) * 512)
            P.dma("sp", lambda e, cs=cs, b2=b2: e.dma_start(out=QMc[b2], in_=QMD[:, :, cs].rearrange("f p n -> p f n")),
                  [dQMD], [dQMc[b2]])
            self.bank_set = [4, 5, 6, 7]
            ks = slice(64 * sl, 64 * sl + 64)
            dUall = [d for r in dUT for d in r]
            for ot in range(6):
                ps, dps = self.bank()
                fns = [lambda e, k=k, ot=ot, ps=ps, ks=ks: e.matmul(ps, lhsT=wglu[:, k, ot * 128:(ot + 1) * 128], rhs=UTv[k][:, :, ks],
                                                                   start=(k == 0), stop=(k == 5)) for k in range(6)]
                P.group("pe", fns, [dwglu] + dUall, [dps])
                sb = ot % 2
                P.op("act", lambda e, ot=ot, ps=ps, sb=sb: e.activation(out=SG[sb], in_=ps, func=AF.Sigmoid,
                                                                     bias=gains[:, 54 + ot:55 + ot]), [dps, dconst], [dSG[sb]])
                P.op("dve", lambda e, ot=ot, ks=ks, sb=sb, b2=b2: e.tensor_tensor(
                    out=TOKc[b2][:, ot, :].rearrange("p (k s) -> p s k", s=8), in0=UTv[ot][:, :, ks],
                    in1=SG[sb].rearrange("p (s k) -> p s k", s=8), op=ALU.mult), [dSG[sb]] + dUall, [dTOK[b2]])
            P.dma("sp", lambda e, cs=cs, b2=b2: e.dma_start(out=ATD[0][0:6, :, cs].rearrange("f p n -> p f n"), in_=TOKc[b2]),
                  [dTOK[b2]], [dATD[0]])
            for h in range(4):
                hb, hp = 64 * (h % 2), h // 2
                self.mem_unit(QMc[b2][hb:hb + 64, hp, :], dQMc[b2],
                              [memKT[hb:hb + 64, hp, j * 128:(j + 1) * 128] for j in range(2)],
                              [memVA[:, j, h, 0:64] for j in range(2)], [dmemK, dmemV],
                              ATD[0][6 + hp, hb:hb + 64, cs], dATD[0])
        self.bank_set = list(range(8))
        P.barrier()
        A.release(mU)
        if self.stop == "C":
            return self._finish_dbg({})

        H2D = self.scratch("H2D", [8, 128, T], BF16)
        self.gf_phase(wts, w_out[0], w_down[0], ATD[0], XT0, XT2, g_ffn0, None, None, nxt=(g_mix1, H2D))
        if self.stop == "D0":
            return self._finish_dbg({})
        if self.stop == "D0":
            return self._finish_dbg({})

        mL = A.mark()
        POS = A.alloc([128, 32], F32)
        VAL = A.alloc([128, 12, 32], F32)
        R1 = A.alloc([128, 12, 32], F32)
        PC = A.alloc([128, 3, 12, 32], BF16)
        NPC = A.alloc([128, 3, 12, 32], BF16)
        dal = Dep()
        dALD = Dep()
        P.op("pool", lambda e: e.iota(POS, pattern=[[1, 32]], base=0, channel_multiplier=32, allow_small_or_imprecise_dtypes=True), [], [dal])
        for h in range(12):
            sl_h = float(np.float32(2.0 ** (-8.0 * (h + 1) / 12)))
            P.op("dve", lambda e, h=h, sl_h=sl_h: e.tensor_scalar(out=VAL[:, h, :], in0=POS, scalar1=sl_h, scalar2=None, op0=ALU.mult),
                 [dal], [dal])
        P.op("dve", lambda e: e.tensor_copy(out=PC[:, 0, :, :], in_=VAL), [dal], [dal])
        P.op("dve", lambda e: e.tensor_tensor(out=R1, in0=VAL, in1=PC[:, 0, :, :], op=ALU.subtract), [dal], [dal])
        P.op("dve", lambda e: e.tensor_copy(out=PC[:, 1, :, :], in_=R1), [dal], [dal])
        P.op("dve", lambda e: e.tensor_tensor(out=VAL, in0=R1, in1=PC[:, 1, :, :], op=ALU.subtract), [dal], [dal])
        P.op("dve", lambda e: e.tensor_copy(out=PC[:, 2, :, :], in_=VAL), [dal], [dal])
        P.op("dve", lambda e: e.tensor_scalar(out=NPC, in0=PC, scalar1=-1.0, scalar2=None, op0=ALU.mult), [dal], [dal])
        for c3 in range(3):
            P.dma("sp", lambda e, c3=c3: e.dma_start(out=ALD[:, c3, :].rearrange("h (p j) -> p h j", j=32), in_=PC[:, c3, :, :]), [dal], [dALD])
            P.dma("sp", lambda e, c3=c3: e.dma_start(out=NALD[:, c3, :].rearrange("h (p j) -> p h j", j=32), in_=NPC[:, c3, :, :]), [dal], [dALD])
        P.barrier()
        A.release(mL)

        H2T = A.alloc([128, 8, T], BF16)
        dH2 = [Dep() for _ in range(8)]
        for f in range(8):
            P.dma("sp", lambda e, f=f: e.dma_start(out=H2T[:, f, :], in_=H2D[f]), [], dH2)
        if self.stop == "E":
            return self._finish_dbg({"H2T": (H2T, Dep(), [128, 8, T], BF16)})

        CM = A.alloc([128, 4, 512], BF16)
        dCM = Dep()
        mAI = A.mark()
        self.attn_init(depth=3, obanks=(0, 1, 2), dbbank=2)
        P.op("pool", lambda e: e.memset(CM.rearrange("p a b -> p (a b)"), 0.0), [], [dCM])
        for jj in range(4):
            P.op("pool", lambda e, jj=jj: e.affine_select(out=CM[:, jj, :], in_=CM[:, jj, :], pattern=[[1, 512]], compare_op=ALU.is_ge,
                                                          fill=NEG, base=-128 * jj, channel_multiplier=-1), [dCM], [dCM])
        dATD1 = Dep()
        mF = A.mark()
        for hg in range(3):
            QA = A.alloc([128, 4, T], BF16)
            KA = A.alloc([128, 4, T], BF16)
            VA = A.alloc([128, 32, 4, 65], BF16)
            wq = A.alloc([128, 8, 256], BF16)
            wk = A.alloc([128, 8, 256], BF16)
            wv = A.alloc([128, 8, 256], BF16)
            KS = A.alloc([64, 4, 16], F32)
            KSb = A.alloc([64, 4, 16], BF16)
            GM = A.alloc([128, 4, 16], F32)
            M8 = A.alloc([128, 4, 8], F32)
            SEL = A.alloc([128, 4, 16], F32)
            BTok = A.alloc([128, 4, 4, 16], BF16)
            BTS = [A.alloc([64, 512], BF16) for _ in range(2)]
            dwq, dwk, dwv, dKS, dKSb, dGM, dM8, dSEL, dBTok = [Dep() for _ in range(9)]
            dBTS = [Dep(), Dep()]
            dQ = [Dep() for _ in range(8)]
            dK = [Dep() for _ in range(8)]
            dV = [Dep() for _ in range(8)]
            dBias = [Dep() for _ in range(8)]
            dQaug, dKaug, dVone = Dep(), Dep(), Dep()
            self.load_w(wq, dwq, moba_w_in, 8, 256, c0=256 * hg)
            self.load_w(wk, dwk, moba_w_in, 8, 256, c0=768 + 256 * hg)
            self.load_w(wv, dwv, moba_w_in, 8, 256, c0=1536 + 256 * hg)
            P.op("pool", lambda e, QA=QA: e.memset(QA[64:86, :, :].rearrange("p a b -> p (a b)"), 1.0), [], [dQaug])
            P.op("pool", lambda e, KA=KA: e.memset(KA[64:86, :, :].rearrange("p a b -> p (a b)"), 1.0), [], [dKaug])
            P.op("pool", lambda e, KA=KA: e.affine_select(out=KA[64:80, :, :], in_=KA[64:80, :, :], pattern=[[0, 4], [1, T]],
                                                          compare_op=ALU.is_ge, fill=0.0, base=0, channel_multiplier=-256), [dKaug], [dKaug])
            P.op("pool", lambda e, KA=KA: e.affine_select(out=KA[64:80, :, :], in_=KA[64:80, :, :], pattern=[[0, 4], [-1, T]],
                                                          compare_op=ALU.is_ge, fill=0.0, base=255, channel_multiplier=256), [dKaug], [dKaug])
            for hh in range(4):
                hd = 4 * hg + hh
                P.dma("sp", lambda e, hh=hh, hd=hd, QA=QA: e.dma_start(out=QA[80:83, hh, :], in_=NALD[hd]), [dALD, dQaug], [dQaug])
                P.dma("sp", lambda e, hh=hh, hd=hd, KA=KA: e.dma_start(out=KA[83:86, hh, :], in_=ALD[hd]), [dALD, dKaug], [dKaug])
            P.op("pool", lambda e, VA=VA: e.memset(VA[:, :, :, 64:65], 1.0), [], [dVone])
            self.bank_set = list(range(8))
            for c in range(8):
                cs = slice(c * 512, (c + 1) * 512)
                for hh in range(4):
                    ps, dps = self.bank()
                    fns = [lambda e, k=k, hh=hh, ps=ps, cs=cs, wq=wq: e.matmul(ps[0:64, :], lhsT=wq[:, k, 64 * hh:64 * hh + 64], rhs=H2T[:, k, cs],
                                                                            start=(k == 0), stop=(k == 7)) for k in range(8)]
                    P.group("pe", fns, [dwq, dH2[c]], [dps])
                    P.op("act", lambda e, hh=hh, ps=ps, cs=cs, QA=QA: e.activation(out=QA[0:64, hh, cs], in_=ps[0:64, :], func=AF.Copy, scale=0.125),
                         [dps], [dQ[c]])
                    ps, dps = self.bank()
                    fns = [lambda e, k=k, hh=hh, ps=ps, cs=cs, wk=wk: e.matmul(ps[0:64, :], lhsT=wk[:, k, 64 * hh:64 * hh + 64], rhs=H2T[:, k, cs],
                                                                            start=(k == 0), stop=(k == 7)) for k in range(8)]
                    P.group("pe", fns, [dwk, dH2[c]], [dps])
                    P.op("dve", lambda e, hh=hh, ps=ps, cs=cs, KA=KA: e.tensor_copy(out=KA[0:64, hh, cs], in_=ps[0:64, :]), [dps], [dK[c]])
                    P.op("dve", lambda e, hh=hh, ps=ps, c=c, KS=KS: e.tensor_reduce(
                        out=KS[:, hh, 2 * c:2 * c + 2], in_=ps[0:64, :].rearrange("p (b n) -> p b n", b=2), axis=AX.X, op=ALU.add), [dps], [dKS])
                for s4 in range(4):
                    ps, dps = self.bank()
                    t0 = c * 512 + s4 * 128
                    fns = [lambda e, k=k, ps=ps, t0=t0, wv=wv: e.matmul(ps[:, 0:256], lhsT=H2T[:, k, t0:t0 + 128], rhs=wv[:, k, :],
                                                                     start=(k == 0), stop=(k == 7)) for k in range(8)]
                    P.group("pe", fns, [dwv, dH2[c]], [dps])
                    self.evac(VA[:, 4 * c + s4, :, 0:64], ps[:, 0:256].rearrange("p (h d) -> p h d", h=4), [dps], [dV[c]])
            P.op("dve", lambda e, KS=KS, KSb=KSb: e.tensor_copy(out=KSb, in_=KS), [dKS], [dKSb])
            for c in range(8):
                cs = slice(c * 512, (c + 1) * 512)
                for i in range(4):
                    tt_ = 4 * c + i
                    qb = tt_ // 2
                    bt = BTok[:, i, :, :]
                    if qb == 0:
                        P.op("pool", lambda e, bt=bt: e.memset(bt[:, :, 0:1], 0.0), [], [dBTok])
                        P.op("pool", lambda e, bt=bt: e.memset(bt[:, :, 1:16], NEG), [], [dBTok])
                        continue
                    ps, dps = self.bank()
                    fns = [lambda e, hh=hh, ps=ps, tt_=tt_, QA=QA, KSb=KSb: e.matmul(
                        ps[:, 16 * hh:16 * hh + 16], lhsT=QA[0:64, hh, tt_ * 128:(tt_ + 1) * 128], rhs=KSb[:, hh, :], start=True, stop=True)
                        for hh in range(4)]
                    P.group("pe", fns, [dQ[c], dKSb], [dps])
                    if qb < 3:
                        P.op("dve", lambda e, GM=GM: e.memset(GM.rearrange("p a b -> p (a b)"), -1e30), [], [dGM])
                    P.op("dve", lambda e, ps=ps, qb=qb, GM=GM: e.tensor_copy(out=GM[:, :, 0:qb], in_=ps[:, 0:64].rearrange("p (h n) -> p h n", h=4)[:, :, 0:qb]),
                         [dps], [dGM])
                    if qb >= 3:
                        P.op("dve", lambda e, qb=qb, GM=GM: e.memset(GM[:, :, qb:qb + 1], 1e30), [dGM], [dGM])
                    for hh in range(4):
                        P.op("dve", lambda e, hh=hh, GM=GM, M8=M8: e.max(out=M8[:, hh, :], in_=GM[:, hh, :]), [dGM], [dM8])
                    ti = 3 if qb >= 3 else 2
                    P.op("dve", lambda e, GM=GM, M8=M8, SEL=SEL, ti=ti: e.tensor_tensor(
                        out=SEL, in0=GM, in1=M8[:, :, ti:ti + 1].to_broadcast([128, 4, 16]), op=ALU.is_ge), [dGM, dM8], [dSEL])
                    P.op("dve", lambda e, bt=bt, SEL=SEL: e.tensor_scalar(out=bt, in0=SEL, scalar1=-NEG, scalar2=NEG, op0=ALU.mult, op1=ALU.add),
                         [dSEL], [dBTok])
                    if qb < 3:
                        P.op("dve", lambda e, bt=bt, qb=qb: e.memset(bt[:, :, qb:qb + 1], 0.0), [dBTok], [dBTok])
                        P.op("dve", lambda e, bt=bt, qb=qb: e.memset(bt[:, :, qb + 1:16], NEG), [dBTok], [dBTok])
                ps, dps = self.bank()
                psb = ps.bitcast(BF16)
                fns = [lambda e, i=i, psb=psb, BTok=BTok: e.transpose(psb[0:64, 128 * i:128 * i + 128],
                                                                    BTok[:, i, :, :].rearrange("p h n -> p (h n)"), ident_bf) for i in range(4)]
                P.group("pe", fns, [dBTok, dconst], [dps])
                b2 = c % 2
                P.op("act", lambda e, psb=psb, b2=b2, BTS=BTS: e.activation(out=BTS[b2], in_=psb[0:64, 0:512], func=AF.Copy), [dps], [dBTS[b2]])
                for hh in range(4):
                    P.dma("sp", lambda e, hh=hh, b2=b2, cs=cs, QA=QA, BTS=BTS: e.dma_start(out=QA[64:80, hh, cs], in_=BTS[b2][16 * hh:16 * hh + 16, :]),
                          [dBTS[b2], dQaug], [dBias[c]])
            if self.stop == "F2" and hg == 0:
                return self._finish_dbg({"QA": (QA, Dep(), [128, 4, T], BF16), "KA": (KA, Dep(), [128, 4, T], BF16)})
            self.bank_set = [3, 4, 5, 6, 7]
            for c in range(8):
                cs = slice(c * 512, (c + 1) * 512)
                nk = 4 * c + 4
                for hh in range(4):
                    hd = 4 * hg + hh
                    self.attn_unit(QA[0:86, hh, cs], [dQ[c], dBias[c], dQaug],
                                   [KA[0:86, hh, j * 128:(j + 1) * 128] for j in range(nk)],
                                   [VA[:, j, hh, :] for j in range(nk)], dK[:c + 1] + dV[:c + 1] + [dKaug, dVone],
                                   [None] * (nk - 4) + [CM[:, jj, :] for jj in range(4)],
                                   ATD[1][hd // 2, 64 * (hd % 2):64 * (hd % 2) + 64, cs], dATD1, cm_dep=dCM)
            self.bank_set = list(range(8))
            P.barrier()
            A.release(mF)
        A.release(mAI)
        self.attn_init(depth=4, obanks=(0, 1, 3), dbbank=2)
        wqm = A.alloc([128, 8, 256], BF16)
        dwqm = Dep()
        self.load_w(wqm, dwqm, moba_w_in, 8, 256, c0=2304)
        wts = self.ffn_prefetch(w_gate[1], w_up[1])
        QMn = A.alloc([128, 2, T], BF16)
        dQn = [Dep() for _ in range(8)]
        for c in range(8):
            cs = slice(c * 512, (c + 1) * 512)
            self.bank_set = [4, 5, 6, 7]
            for hp in range(2):
                ps, dps = self.bank()
                fns = [lambda e, k=k, hp=hp, ps=ps, cs=cs: e.matmul(ps, lhsT=wqm[:, k, 128 * hp:128 * hp + 128], rhs=H2T[:, k, cs],
                                                                   start=(k == 0), stop=(k == 7)) for k in range(8)]
                P.group("pe", fns, [dwqm, dH2[c]], [dps])
                self.evac(QMn[:, hp, cs], ps, [dps], [dQn[c]], scale=0.125)
            for h in range(4):
                hb, hp = 64 * (h % 2), h // 2
                self.mem_unit(QMn[hb:hb + 64, hp, cs], dQn[c],
                              [memKT[hb:hb + 64, hp, j * 128:(j + 1) * 128] for j in range(2)],
                              [memVA[:, j, h, 0:64] for j in range(2)], [dmemK, dmemV],
                              ATD[1][6 + hp, hb:hb + 64, cs], dATD1)
        self.bank_set = list(range(8))
        P.barrier()
        A.release(mL)
        if self.stop == "F":
            return self._finish_dbg({})
        self.gf_phase(wts, w_out[1], w_down[1], ATD[1], XT2, None, g_ffn1, out, g_fin)
        return self._finish_dbg({})

    def attn_init(self, depth=2, obanks=(0, 1), dbbank=2):
        A = self.A
        self.E = [A.alloc([128, 512], BF16) for _ in range(4)]
        self.dE = [Dep() for _ in range(4)]
        self.enext = 0
        self.adepth = depth
        self.obanks = list(obanks)
        self.dbbank = dbbank
        self.DEN = [A.alloc([128, 512], F32) for _ in range(depth)]
        self.dDEN = [Dep() for _ in range(depth)]
        self.RB = [A.alloc([64, 512], F32) for _ in range(depth)]
        self.dRB = [Dep() for _ in range(depth)]
        self.ON = [A.alloc([64, 512], BF16) for _ in range(depth)]
        self.dON = [Dep() for _ in range(depth)]
        self.unext = 0

    def mem_unit(self, q_ap, dq, k_tiles, v_tiles, dkv, dest, ddest):
        P = self.P
        u = self.unext
        self.unext += 1
        E, dE = self.E, self.dE
        di = u % self.adepth
        LN, dLN, RB, dRB, ON, dON = self.DEN[di], self.dDEN[di], self.RB[di], self.dRB[di], self.ON[di], self.dON[di]
        ones_bf, dconst = self.ones_bf, self.dconst
        O, dO = self.banks[u % 2], self.bdep[u % 2]
        DB, dDB = self.banks[2 + u % 2], self.bdep[2 + u % 2]
        dql = list(dq) if isinstance(dq, (list, tuple)) else [dq]
        eis = []
        for j in range(2):
            ps, dps = self.bank()
            P.op("pe", lambda e, j=j, ps=ps: e.matmul(ps, lhsT=k_tiles[j], rhs=q_ap, start=True, stop=True), dql + dkv, [dps])
            ei = self.enext % 4
            self.enext += 1
            P.op("act", lambda e, ei=ei, ps=ps: e.activation(out=E[ei], in_=ps, func=AF.Exp), [dps], [dE[ei]])
            eis.append(ei)
        fns = [lambda e, j=j: e.matmul(O[0:64, :], lhsT=v_tiles[j], rhs=E[eis[j]], start=(j == 0), stop=(j == 1)) for j in range(2)]
        P.group("pe", fns, [dE[eis[0]], dE[eis[1]]] + dkv, [dO])
        fns = [lambda e, j=j: e.matmul(DB[0:64, :], lhsT=ones_bf[:, 0:64], rhs=E[eis[j]], start=(j == 0), stop=(j == 1)) for j in range(2)]
        P.group("pe", fns, [dE[eis[0]], dE[eis[1]], dconst], [dDB])
        P.op("act", lambda e: e.activation(out=LN[0:64, :], in_=DB[0:64, :], func=AF.Ln), [dDB], [dLN])
        P.op("act", lambda e: e.activation(out=RB, in_=LN[0:64, :], func=AF.Exp, scale=-1.0), [dLN], [dRB])
        P.op("dve", lambda e: e.tensor_tensor(out=ON, in0=O[0:64, :], in1=RB, op=ALU.mult), [dO, dRB], [dON])
        P.dma("sp", lambda e: e.dma_start(out=dest, in_=ON), [dON], [ddest])

    def attn_unit(self, q_ap, dq, k_tiles, v_tiles, dkv, cms, dest, ddest, cm_dep=None):
        P = self.P
        u = self.unext
        self.unext += 1
        E, dE = self.E, self.dE
        di = u % self.adepth
        DEN, dDEN, RB, dRB = self.DEN[di], self.dDEN[di], self.RB[di], self.dRB[di]
        ON, dON = self.ON, self.dON
        ones_f, ident_bf = self.ones_f, self.ident_bf
        ob = self.obanks[u % len(self.obanks)]
        O, dO = self.banks[ob], self.bdep[ob]
        DB, dDB = self.banks[self.dbbank], self.bdep[self.dbbank]
        n = len(k_tiles)
        K = q_ap.shape[0]
        Sj = [None] * n

        dql = list(dq) if isinstance(dq, (list, tuple)) else [dq]

        def qk(j):
            ps, dps = self.bank()
            c0 = 0
            if cms[j] is None:
                P.op("pe", lambda e: e.matmul(ps, lhsT=k_tiles[j], rhs=q_ap, start=True, stop=True), dql + dkv, [dps])
            else:
                c0 = 128 * (j - (n - 4))
                fns = [lambda e: e.matmul(ps[:, c0:512], lhsT=k_tiles[j], rhs=q_ap[:, c0:512], start=True, stop=False),
                       lambda e: e.matmul(ps[:, c0:512], lhsT=ident_bf, rhs=cms[j][:, c0:512], start=False, stop=True)]
                P.group("pe", fns, dql + [cm_dep, self.dconst] + dkv, [dps])
            ei = self.enext % 4
            self.enext += 1
            P.op("act", lambda e: e.activation(out=E[ei][:, c0:512], in_=ps[:, c0:512], func=AF.Exp), [dps], [dE[ei]])
            Sj[j] = (ei, c0)

        def pv(j):
            ei, c0 = Sj[j]
            P.op("pe", lambda e: e.matmul(O[0:65, c0:512], lhsT=v_tiles[j], rhs=E[ei][:, c0:512], start=(j == 0), stop=(j == n - 1)),
                 [dE[ei]] + dkv, [dO])

        LOOK = 2
        for j in range(min(LOOK, n)):
            qk(j)
        for j in range(n):
            if j + LOOK < n:
                qk(j + LOOK)
            pv(j)
        P.op("act", lambda e: e.activation(out=DEN[64:65, :], in_=O[64:65, :], func=AF.Copy), [dO], [dDEN])
        dend = self.DEND[di:di + 1, :]
        P.dma("sp", lambda e: e.dma_start(out=dend, in_=DEN[64:65, :]), [dDEN], [dDEN])
        P.dma("sp", lambda e: e.dma_start(out=DEN[0:64, :], in_=dend.partition_broadcast(64)), [dDEN], [dDEN])
        P.op("dve", lambda e: e.reciprocal(out=RB, in_=DEN[0:64, :]), [dDEN], [dRB])
        oi = di
        P.op("dve", lambda e: e.tensor_tensor(out=ON[oi], in0=O[0:64, :], in1=RB, op=ALU.mult), [dO, dRB], [dON[oi]])
        P.dma("sp", lambda e: e.dma_start(out=dest, in_=ON[oi]), [dON[oi]], [ddest])

    def ffn_prefetch(self, wg_d, wu_d):
        P, A = self.P, self.A
        wg = A.alloc_top([128, 8, 2816], BF16)
        wu = A.alloc_top([128, 8, 2816], BF16)
        dwg, dwu = Dep(), Dep()
        self._ffn_w = (None, wg, wu, dwg, dwu, wg_d, wu_d)
        self._issue_ffn_loads()
        return self._ffn_w

    def gf_phase(self, wts, wo_d, wd_d, ATDl, XTin, XTout, gcol, out_final, gfin, nxt=None):
        P, A = self.P, self.A
        _, wg, wu, dwg, dwu, _, _ = wts
        m = A.mark()
        NC = 256
        NCH = T // NC
        wout = A.alloc([128, 8, D], BF16)
        dwo = Dep()
        self.load_w(wout, dwo, wo_d, 8, D)
        wd = A.alloc([128, 22, D], BF16)
        dwd = Dep()
        self.load_w(wd, dwd, wd_d, 22, D)
        ATc = A.alloc([128, 8, NC], BF16)
        dAT = Dep()
        xc = [A.alloc([128, 8, NC], F32) for _ in range(2)]
        dxc = [Dep(), Dep()]
        hc = [A.alloc([128, 8, NC], BF16) for _ in range(2)]
        dhc = [Dep(), Dep()]
        Ac = A.alloc([128, 22, NC], BF16)
        dAc = [Dep() for _ in range(22)]
        sg = [A.alloc([128, NC], F32) for _ in range(2)]
        dsg = [Dep(), Dep()]
        tmpn = self.rms_tmp(NC)
        if nxt is not None:
            hn = A.alloc([128, 8, NC], BF16)
            dhn = Dep()
        if out_final is not None:
            OT = A.alloc([128, 2, D], F32)
            dOT = Dep()
            dfin = [Dep(), Dep()]
            outv = out_final.rearrange("(c t p) f -> c p t f", p=128, t=2)

        def op_part(c):
            b2 = c % 2
            cs = slice(c * NC, (c + 1) * NC)
            P.dma("sp", lambda e: e.dma_start(out=ATc, in_=ATDl[:, :, cs].rearrange("f p n -> p f n")), [], [dAT])
            P.dma("sp", lambda e: e.dma_start(out=xc[b2], in_=XTin[:, :, cs].rearrange("f p n -> p f n")), [], [dxc[b2]])
            for ft in range(8):
                ps, dps = self.bank()
                fns = [lambda e, k=k, ft=ft, ps=ps: e.matmul(ps[:, 0:NC], lhsT=wout[:, k, ft * 128:(ft + 1) * 128], rhs=ATc[:, k, :],
                                                            start=(k == 0), stop=(k == 7)) for k in range(8)]
                P.group("pe", fns, [dwo, dAT], [dps])
                P.op("dve", lambda e, ft=ft, ps=ps: e.tensor_tensor(out=xc[b2][:, ft, :], in0=ps[:, 0:NC], in1=xc[b2][:, ft, :], op=ALU.add),
                     [dps], [dxc[b2]])
            self.rmsnorm(xc[b2], dxc[b2], gcol, hc[b2], dhc[b2], NC, tmpn)

        def ffn_gu(c, mid=None):
            b2 = c % 2
            for ht in range(22):
                if ht == 6 and mid is not None:
                    mid()
                psg, dpsg = self.bank()
                fns = [lambda e, k=k, ht=ht, ps=psg: e.matmul(ps[:, 0:NC], lhsT=wg[:, k, ht * 128:(ht + 1) * 128], rhs=hc[b2][:, k, :],
                                                             start=(k == 0), stop=(k == 7)) for k in range(8)]
                P.group("pe", fns, [dwg, dhc[b2]], [dpsg])
                psu, dpsu = self.bank()
                fns = [lambda e, k=k, ht=ht, ps=psu: e.matmul(ps[:, 0:NC], lhsT=wu[:, k, ht * 128:(ht + 1) * 128], rhs=hc[b2][:, k, :],
                                                             start=(k == 0), stop=(k == 7)) for k in range(8)]
                P.group("pe", fns, [dwu, dhc[b2]], [dpsu])
                sb = ht % 2
                P.op("act", lambda e, ps=psg, sb=sb: e.activation(out=sg[sb], in_=ps[:, 0:NC], func=AF.Silu), [dpsg], [dsg[sb]])
                P.op("dve", lambda e, ps=psu, sb=sb, ht=ht: e.tensor_tensor(out=Ac[:, ht, :], in0=ps[:, 0:NC], in1=sg[sb], op=ALU.mult),
                     [dpsu, dsg[sb]], [dAc[ht]])

        def ffn_down(c):
            b2 = c % 2
            cs = slice(c * NC, (c + 1) * NC)
            for ft in range(8):
                ps, dps = self.bank()
                fns = [lambda e, ht=ht, ft=ft, ps=ps: e.matmul(ps[:, 0:NC], lhsT=wd[:, ht, ft * 128:(ft + 1) * 128], rhs=Ac[:, ht, :],
                                                              start=(ht == 0), stop=(ht == 21)) for ht in range(22)]
                P.group("pe", fns, [dwd] + dAc, [dps])
                P.op("dve", lambda e, ft=ft, ps=ps: e.tensor_tensor(out=xc[b2][:, ft, :], in0=ps[:, 0:NC], in1=xc[b2][:, ft, :], op=ALU.add),
                     [dps], [dxc[b2]])
            if out_final is None:
                P.dma("sp", lambda e: e.dma_start(out=XTout[:, :, cs].rearrange("f p n -> p f n"), in_=xc[b2]), [dxc[b2]], [Dep()])
                if nxt is not None:
                    self.rmsnorm(xc[b2], dxc[b2], nxt[0], hn, dhn, NC, tmpn)
                    P.dma("sp", lambda e: e.dma_start(out=nxt[1][:, :, cs].rearrange("f p n -> p f n"), in_=hn), [dhn], [dhn])
            else:
                self.rmsnorm(xc[b2], dxc[b2], gfin, xc[b2], dxc[b2], NC, tmpn)

        def final_part(c):
            b2 = c % 2
            for t2 in range(2):
                for fq in range(2):
                    ps, dps = self.bank()
                    fns = [lambda e, i=i, t2=t2, fq=fq, ps=ps: e.transpose(
                        ps[:, 128 * i:128 * i + 128], xc[b2][:, 4 * fq + i, t2 * 128:(t2 + 1) * 128], self.ident) for i in range(4)]
                    P.group("pe", fns, [dxc[b2], self.dconst], [dps])
                    self.evac(OT[:, t2, fq * 512:(fq + 1) * 512], ps, [dps], [dOT])
            P.dma("sp", lambda e: e.dma_start(out=outv[c], in_=OT), [dOT, dxc[b2]], [dOT, dxc[b2]])

        op_part(0)
        for c in range(NCH):
            if out_final is not None and c > 0:
                ffn_gu(c, mid=lambda c=c: final_part(c - 1))
            else:
                ffn_gu(c)
            if c + 1 < NCH:
                op_part(c + 1)
            ffn_down(c)
        if out_final is not None:
            final_part(NCH - 1)
        P.barrier()
        A.release(m)
        A.release_top()

    def _issue_ffn_loads(self):
        P = self.P
        m, wg, wu, dwg, dwu, wg_d, wu_d = self._ffn_w
        for k in range(8):
            for (dst, dd, src) in ((wg, dwg, wg_d), (wu, dwu, wu_d)):
                for c in (0, 2048):
                    cw = min(2048, 2816 - c)
                    P.dma("pool", lambda e, dst=dst, src=src, k=k, c=c, cw=cw: e.dma_start(
                        out=dst[:, k, c:c + cw], in_=src[k * 128:(k + 1) * 128, c:c + cw]), [], [dd])

    def outproj_phase(self, wo, ATDl, XTin, XTout, HTDl, gcol):
        P, A = self.P, self.A
        m = A.mark()
        wout = A.alloc([128, 8, D], BF16)
        dwo = Dep()
        self.load_w(wout, dwo, wo, 8, D)
        self._issue_ffn_loads()
        ATc = [A.alloc([128, 8, 512], BF16) for _ in range(2)]
        dAT = [Dep(), Dep()]
        xc = [A.alloc([128, 8, 512], F32) for _ in range(2)]
        dxc = [Dep(), Dep()]
        hc = [A.alloc([128, 8, 512], BF16) for _ in range(2)]
        dhc = [Dep(), Dep()]
        tmpn = self.rms_tmp(512)
        for c in range(8):
            b2 = c % 2
            cs = slice(c * 512, (c + 1) * 512)
            P.dma("sp", lambda e, cs=cs, b2=b2: e.dma_start(out=ATc[b2], in_=ATDl[:, :, cs].rearrange("f p n -> p f n")), [], [dAT[b2]])
            P.dma("sp", lambda e, cs=cs, b2=b2: e.dma_start(out=xc[b2], in_=XTin[:, :, cs].rearrange("f p n -> p f n")), [], [dxc[b2]])
            for ft in range(8):
                ps, dps = self.bank()
                fns = [lambda e, k=k, ft=ft, ps=ps, b2=b2: e.matmul(ps, lhsT=wout[:, k, ft * 128:(ft + 1) * 128], rhs=ATc[b2][:, k, :],
                                                                   start=(k == 0), stop=(k == 7)) for k in range(8)]
                P.group("pe", fns, [dwo, dAT[b2]], [dps])
                P.op("dve", lambda e, ft=ft, ps=ps, b2=b2: e.tensor_tensor(out=xc[b2][:, ft, :], in0=ps, in1=xc[b2][:, ft, :], op=ALU.add),
                     [dps], [dxc[b2]])
            P.dma("sp", lambda e, cs=cs, b2=b2: e.dma_start(out=XTout[:, :, cs].rearrange("f p n -> p f n"), in_=xc[b2]), [dxc[b2]], [Dep()])
            self.rmsnorm(xc[b2], dxc[b2], gcol, hc[b2], dhc[b2], 512, tmpn)
            P.dma("sp", lambda e, cs=cs, b2=b2: e.dma_start(out=HTDl[:, :, cs].rearrange("f p n -> p f n"), in_=hc[b2]), [dhc[b2]], [Dep()])
        P.barrier()
        A.release(m)

    def ffn_phase(self, wts, wd_d, HTDl, XTin, XTout, out_final, gfin):
        P, A = self.P, self.A
        m, wg, wu, dwg, dwu, _, _ = wts
        NC = 256
        wd = A.alloc([128, 22, D], BF16)
        dwd = Dep()
        self.load_w(wd, dwd, wd_d, 22, D)
        hc = [A.alloc([128, 8, NC], BF16) for _ in range(2)]
        dhc = [Dep(), Dep()]
        xc = [A.alloc([128, 8, NC], F32) for _ in range(2)]
        dxc = [Dep(), Dep()]
        Ac = A.alloc([128, 22, NC], BF16)
        dAc = [Dep() for _ in range(22)]
        sg = [A.alloc([128, NC], F32) for _ in range(2)]
        dsg = [Dep(), Dep()]
        if out_final is not None:
            yT = A.alloc([128, 8, NC], F32)
            dyT = Dep()
            OT = A.alloc([128, 2, D], F32)
            dOT = Dep()
            tmpn = self.rms_tmp(NC)
            outv = out_final.rearrange("(c t p) f -> c p t f", p=128, t=2)
        for c in range(T // NC):
            b2 = c % 2
            cs = slice(c * NC, (c + 1) * NC)
            P.dma("sp", lambda e, cs=cs, b2=b2: e.dma_start(out=hc[b2], in_=HTDl[:, :, cs].rearrange("f p n -> p f n")), [], [dhc[b2]])
            P.dma("sp", lambda e, cs=cs, b2=b2: e.dma_start(out=xc[b2], in_=XTin[:, :, cs].rearrange("f p n -> p f n")), [], [dxc[b2]])
            for ht in range(22):
                psg, dpsg = self.bank()
                fns = [lambda e, k=k, ht=ht, ps=psg, b2=b2: e.matmul(ps[:, 0:NC], lhsT=wg[:, k, ht * 128:(ht + 1) * 128], rhs=hc[b2][:, k, :],
                                                                    start=(k == 0), stop=(k == 7)) for k in range(8)]
                P.group("pe", fns, [dwg, dhc[b2]], [dpsg])
                psu, dpsu = self.bank()
                fns = [lambda e, k=k, ht=ht, ps=psu, b2=b2: e.matmul(ps[:, 0:NC], lhsT=wu[:, k, ht * 128:(ht + 1) * 128], rhs=hc[b2][:, k, :],
                                                                    start=(k == 0), stop=(k == 7)) for k in range(8)]
                P.group("pe", fns, [dwu, dhc[b2]], [dpsu])
                sb = ht % 2
                P.op("act", lambda e, ps=psg, sb=sb: e.activation(out=sg[sb], in_=ps[:, 0:NC], func=AF.Silu), [dpsg], [dsg[sb]])
                P.op("dve", lambda e, ps=psu, sb=sb, ht=ht: e.tensor_tensor(out=Ac[:, ht, :], in0=ps[:, 0:NC], in1=sg[sb], op=ALU.mult),
                     [dpsu, dsg[sb]], [dAc[ht]])
            for ft in range(8):
                ps, dps = self.bank()
                fns = [lambda e, ht=ht, ft=ft, ps=ps: e.matmul(ps[:, 0:NC], lhsT=wd[:, ht, ft * 128:(ft + 1) * 128], rhs=Ac[:, ht, :],
                                                              start=(ht == 0), stop=(ht == 21)) for ht in range(22)]
                P.group("pe", fns, [dwd] + dAc, [dps])
                P.op("dve", lambda e, ft=ft, ps=ps, b2=b2: e.tensor_tensor(out=xc[b2][:, ft, :], in0=ps[:, 0:NC], in1=xc[b2][:, ft, :], op=ALU.add),
                     [dps], [dxc[b2]])
            if out_final is None:
                P.dma("sp", lambda e, cs=cs, b2=b2: e.dma_start(out=XTout[:, :, cs].rearrange("f p n -> p f n"), in_=xc[b2]), [dxc[b2]], [Dep()])
            else:
                self.rmsnorm(xc[b2], dxc[b2], gfin, yT, dyT, NC, tmpn)
                for t2 in range(2):
                    for fq in range(2):
                        ps, dps = self.bank()
                        fns = [lambda e, i=i, t2=t2, fq=fq, ps=ps: e.transpose(
                            ps[:, 128 * i:128 * i + 128], yT[:, 4 * fq + i, t2 * 128:(t2 + 1) * 128], self.ident) for i in range(4)]
                        P.group("pe", fns, [dyT, self.dconst], [dps])
                        self.evac(OT[:, t2, fq * 512:(fq + 1) * 512], ps, [dps], [dOT])
                P.dma("sp", lambda e, c=c: e.dma_start(out=outv[c], in_=OT), [dOT], [Dep()])
        P.barrier()
        A.release(m)

    def _s5_setup(self, lam_re, lam_im, log_dt, b_re, b_im, c_re, c_im, WS_sb, WY_sb, KB_sb, V12, II, dS5):
        nc, P, A = self.nc, self.P, self.A
        ident, ident_bf, dconst = self.ident, self.ident_bf, self.dconst

        def tl(shape, dt=F32):
            return (A.alloc(shape, dt), Dep())

        def tt(o, a, b, op, eng="dve"):
            P.op(eng, lambda e: e.tensor_tensor(out=o[0], in0=a[0], in1=b[0], op=op), [a[1], b[1]], [o[1]])

        def ts(o, a, s1, op0, s2=None, op1=None, extra=()):
            if op1 is None:
                P.op("dve", lambda e: e.tensor_scalar(out=o[0], in0=a[0], scalar1=s1, scalar2=None, op0=op0), [a[1]] + list(extra), [o[1]])
            else:
                P.op("dve", lambda e: e.tensor_scalar(out=o[0], in0=a[0], scalar1=s1, scalar2=s2, op0=op0, op1=op1),
                     [a[1]] + list(extra), [o[1]])

        def stt(o, a, sc, b, op0, op1, extra=()):
            P.op("dve", lambda e: e.scalar_tensor_tensor(out=o[0], in0=a[0], scalar=sc, in1=b[0], op0=op0, op1=op1),
                 [a[1], b[1]] + list(extra), [o[1]])

        def act(o, a, func, scale=1.0):
            P.op("act", lambda e: e.activation(out=o[0], in_=a[0], func=func, scale=scale), [a[1]], [o[1]])

        def sub(t, ap):
            return (ap, t[1])

        LT = tl([48, 3, 128])
        LD = tl([48, 1])
        for h in range(2):
            P.dma("sp", lambda e, h=h: e.dma_start(out=LT[0][:, 0, 64 * h:64 * h + 64], in_=lam_re), [], [LT[1]])
            P.dma("sp", lambda e, h=h: e.dma_start(out=LT[0][:, 1, 64 * h:64 * h + 64], in_=lam_im), [], [LT[1]])
        P.dma("sp", lambda e: e.dma_start(out=LD[0], in_=log_dt.rearrange("(g o) -> g o", o=1)), [], [LD[1]])
        P.op("dve", lambda e: e.tensor_copy(out=LT[0][:, 2, :], in_=LD[0].to_broadcast([48, 128])), [LD[1]], [LT[1]])
        BR, BI, CR, CI = tl([128, 48, 16]), tl([128, 48, 16]), tl([128, 48, 16]), tl([128, 48, 16])
        for (dst, src) in ((BR, b_re), (BI, b_im)):
            for h in range(2):
                for gq in range(4):
                    P.dma("sp", lambda e, dst=dst, src=src, h=h, gq=gq: e.dma_start(
                        out=dst[0][64 * h:64 * h + 64, 12 * gq:12 * gq + 12, :],
                        in_=src[12 * gq:12 * gq + 12].rearrange("g p c -> p g c")), [], [dst[1]])
        CT = [tl([128, 6, 2, 64]), tl([128, 6, 2, 64])]
        for i, src in enumerate((c_re, c_im)):
            for h in range(2):
                P.dma("sp", lambda e, i=i, src=src, h=h: e.dma_start(
                    out=CT[i][0][:, :, h, :], in_=src.rearrange("(o g) c p -> (g c) o p", g=8)), [], [CT[i][1]])
        lr, li, ldt = tl([128, 48]), tl([128, 48]), tl([128, 48])
        ps, dps = self.bank()
        fns = [lambda e, i=i, ps=ps: e.transpose(ps[:, 64 * i:64 * i + 48], LT[0][:, i, :], ident[0:48, 0:48]) for i in range(3)]
        P.group("pe", fns, [LT[1], dconst], [dps])
        for i, t in enumerate((lr, li, ldt)):
            P.op("dve", lambda e, i=i, t=t, ps=ps: e.tensor_copy(out=t[0], in_=ps[:, 64 * i:64 * i + 48]), [dps], [t[1]])
        for i, dst in enumerate((CR, CI)):
            for half in range(2):
                ps, dps = self.bank()
                os_ = list(range(4)) if half == 0 else [4, 5]
                fns = [lambda e, o=o, i=i, ps=ps: e.transpose(ps[:, 128 * (o % 4):128 * (o % 4) + 128],
                                                              CT[i][0][:, o, :, :].rearrange("p a b -> p (a b)"), ident) for o in os_]
                P.group("pe", fns, [CT[i][1], dconst], [dps])
                n = len(os_) * 128
                o0 = os_[0]
                P.op("dve", lambda e, dst=dst, ps=ps, n=n, o0=o0: e.tensor_copy(
                    out=dst[0][:, 8 * o0:8 * o0 + n // 16, :].rearrange("p g c -> p (g c)"), in_=ps[:, 0:n]), [dps], [dst[1]])
        mtop, mbot, nmbot, nmtop = tl([128, 1]), tl([128, 1]), tl([128, 1]), tl([128, 1])
        for t, (v0, v1) in ((mtop, (1.0, 0.0)), (mbot, (0.0, 1.0)), (nmbot, (0.0, -1.0)), (nmtop, (-1.0, 0.0))):
            P.op("pool", lambda e, t=t, v0=v0: e.memset(t[0][0:64, :], v0), [], [t[1]])
            P.op("pool", lambda e, t=t, v1=v1: e.memset(t[0][64:128, :], v1), [], [t[1]])
        G = tl([8, 128])
        G2 = tl([4, 128])
        P.op("pool", lambda e: e.memset(G[0], 1.0), [], [G[1]])
        P.op("pool", lambda e: e.affine_select(out=G[0], in_=G[0], pattern=[[1, 128]], compare_op=ALU.is_ge, fill=0.0,
                                                base=0, channel_multiplier=-16), [G[1]], [G[1]])
        P.op("pool", lambda e: e.affine_select(out=G[0], in_=G[0], pattern=[[-1, 128]], compare_op=ALU.is_ge, fill=0.0,
                                                base=15, channel_multiplier=16), [G[1]], [G[1]])
        P.op("pool", lambda e: e.memset(G2[0], 1.0), [], [G2[1]])
        P.op("pool", lambda e: e.affine_select(out=G2[0], in_=G2[0], pattern=[[1, 128]], compare_op=ALU.is_ge, fill=0.0,
                                                base=0, channel_multiplier=-32), [G2[1]], [G2[1]])
        P.op("pool", lambda e: e.affine_select(out=G2[0], in_=G2[0], pattern=[[-1, 128]], compare_op=ALU.is_ge, fill=0.0,
                                                base=15, channel_multiplier=32), [G2[1]], [G2[1]])
        onec = tl([4, 1])
        P.op("pool", lambda e: e.memset(onec[0], 1.0), [], [onec[1]])
        BD, me, mo = tl([128, 128]), tl([128, 1]), tl([128, 1])
        ps, dps = self.bank()
        P.op("pe", lambda e, ps=ps: e.matmul(ps[:, 0:128], lhsT=G[0], rhs=G[0], start=True, stop=True), [G[1]], [dps])
        P.op("dve", lambda e, ps=ps: e.tensor_copy(out=BD[0], in_=ps[:, 0:128]), [dps], [BD[1]])
        ps, dps = self.bank()
        P.op("pe", lambda e, ps=ps: e.matmul(ps[:, 0:1], lhsT=G2[0], rhs=onec[0], start=True, stop=True), [G2[1], onec[1]], [dps])
        P.op("dve", lambda e, ps=ps: e.tensor_copy(out=me[0], in_=ps[:, 0:1]), [dps], [me[1]])
        ts(mo, me, -1.0, ALU.mult, 1.0, ALU.add)
        P.op("dve", lambda e: e.tensor_tensor(out=II, in0=ident_bf[:, 0:64], in1=ident_bf[:, 64:128], op=ALU.add), [dconst], [dS5])
        dt, th, lrd, mag = tl([128, 48]), tl([128, 48]), tl([128, 48]), tl([128, 48])
        act(dt, ldt, AF.Exp)
        tt(th, li, dt, ALU.mult)
        tt(lrd, lr, dt, ALU.mult)
        act(mag, lrd, AF.Exp)
        sn, cs = tl([128, 48]), tl([128, 48])
        t1, t2, t3 = tl([128, 48]), tl([128, 48]), tl([128, 48])
        for (dst, shift) in ((sn, 0.0), (cs, math.pi / 2)):
            ts(t1, th, shift, ALU.add)
            ts(t2, t1, 1.0 / (2 * math.pi), ALU.mult)
            ts(t3, t2, MAGIC, ALU.add)
            ts(t2, t3, MAGIC, ALU.subtract)
            stt(t3, t2, -2.0 * math.pi, t1, ALU.mult, ALU.add)
            ts(t1, t3, 3.1415925, ALU.min, -3.1415925, ALU.max)
            act(dst, t1, AF.Sin)
        ar, ai = tl([128, 48]), tl([128, 48])
        tt(ar, mag, cs, ALU.mult)
        tt(ai, mag, sn, ALU.mult)
        den, rden, nr, fr, fi = tl([128, 48]), tl([128, 48]), tl([128, 48]), tl([128, 48]), tl([128, 48])
        tt(t1, lr, lr, ALU.mult)
        tt(t2, li, li, ALU.mult)
        tt(den, t1, t2, ALU.add)
        P.op("dve", lambda e: e.reciprocal(out=rden[0], in_=den[0]), [den[1]], [rden[1]])
        ts(nr, ar, -1.0, ALU.add)
        tt(t1, nr, lr, ALU.mult)
        tt(t2, ai, li, ALU.mult)
        tt(t3, t1, t2, ALU.add)
        tt(fr, t3, rden, ALU.mult)
        tt(t1, ai, lr, ALU.mult)
        tt(t2, nr, li, ALU.mult)
        tt(t3, t1, t2, ALU.subtract)
        tt(fi, t3, rden, ALU.mult)

        def bc(t):
            return (t[0].unsqueeze(2).to_broadcast([128, 48, 16]), t[1])

        bbr, bbi, u1, u2 = tl([128, 48, 16]), tl([128, 48, 16]), tl([128, 48, 16]), tl([128, 48, 16])
        tt(u1, BR, bc(fr), ALU.mult)
        tt(u2, BI, bc(fi), ALU.mult)
        tt(bbr, u1, u2, ALU.subtract)
        tt(u1, BI, bc(fr), ALU.mult)
        tt(u2, BR, bc(fi), ALU.mult)
        tt(bbi, u1, u2, ALU.add)
        PWr, PWi = tl([128, 48, 9]), tl([128, 48, 9])
        P.op("pool", lambda e: e.memset(PWr[0][:, :, 0:1], 1.0), [], [PWr[1]])
        P.op("pool", lambda e: e.memset(PWi[0][:, :, 0:1], 0.0), [], [PWi[1]])
        P.op("dve", lambda e: e.tensor_copy(out=PWr[0][:, :, 1], in_=ar[0]), [ar[1]], [PWr[1]])
        P.op("dve", lambda e: e.tensor_copy(out=PWi[0][:, :, 1], in_=ai[0]), [ai[1]], [PWi[1]])
        for n in range(1, 8):
            pr, pi = sub(PWr, PWr[0][:, :, n]), sub(PWi, PWi[0][:, :, n])
            tt(t1, pr, ar, ALU.mult)
            tt(t2, pi, ai, ALU.mult)
            tt(sub(PWr, PWr[0][:, :, n + 1]), t1, t2, ALU.subtract)
            tt(t1, pr, ai, ALU.mult)
            tt(t2, pi, ar, ALU.mult)
            tt(sub(PWi, PWi[0][:, :, n + 1]), t1, t2, ALU.add)
        SQr, SQi = tl([128, 48, 9]), tl([128, 48, 9])
        P.op("dve", lambda e: e.tensor_copy(out=SQr[0][:, :, 0], in_=PWr[0][:, :, 8]), [PWr[1]], [SQr[1]])
        P.op("dve", lambda e: e.tensor_copy(out=SQi[0][:, :, 0], in_=PWi[0][:, :, 8]), [PWi[1]], [SQi[1]])
        for j in range(8):
            r, i_ = sub(SQr, SQr[0][:, :, j]), sub(SQi, SQi[0][:, :, j])
            tt(t1, r, r, ALU.mult)
            tt(t2, i_, i_, ALU.mult)
            tt(sub(SQr, SQr[0][:, :, j + 1]), t1, t2, ALU.subtract)
            tt(t1, r, i_, ALU.mult)
            ts(sub(SQi, SQi[0][:, :, j + 1]), t1, 2.0, ALU.mult)
        w1 = tl([128, 48, 9])
        V12t = (V12, dS5)
        ts(w1, SQi, nmbot[0], ALU.mult, extra=[nmbot[1]])
        stt(sub(V12t, V12[:, :, :, 0]), SQr, mtop[0], w1, ALU.mult, ALU.add, extra=[mtop[1]])
        ts(w1, SQr, mbot[0], ALU.mult, extra=[mbot[1]])
        stt(sub(V12t, V12[:, :, :, 1]), SQi, mtop[0], w1, ALU.mult, ALU.add, extra=[mtop[1]])
        P1, P2, Q1, Q2 = tl([128, 48, 9]), tl([128, 48, 9]), tl([128, 48, 9]), tl([128, 48, 9])
        ts(w1, PWi, nmbot[0], ALU.mult, extra=[nmbot[1]])
        stt(P1, PWr, mtop[0], w1, ALU.mult, ALU.add, extra=[mtop[1]])
        ts(w1, PWr, nmbot[0], ALU.mult, extra=[nmbot[1]])
        stt(P2, PWi, nmtop[0], w1, ALU.mult, ALU.add, extra=[nmtop[1]])
        ts(w1, PWi, mbot[0], ALU.mult, extra=[mbot[1]])
        stt(Q1, PWr, mtop[0], w1, ALU.mult, ALU.add, extra=[mtop[1]])
        ts(w1, PWr, mbot[0], ALU.mult, extra=[mbot[1]])
        stt(Q2, PWi, nmtop[0], w1, ALU.mult, ALU.add, extra=[nmtop[1]])
        CA = tl([128, 9, 48, 16], BF16)
        WSb = tl([128, 8, 48, 16], BF16)
        for n in range(9):
            tt(u1, CR, (P1[0][:, :, n:n + 1].to_broadcast([128, 48, 16]), P1[1]), ALU.mult)
            tt(u2, CI, (P2[0][:, :, n:n + 1].to_broadcast([128, 48, 16]), P2[1]), ALU.mult)
            tt(sub(CA, CA[0][:, n, :, :]), u1, u2, ALU.add)
        for sl in range(8):
            n = 7 - sl
            tt(u1, bbr, (Q1[0][:, :, n:n + 1].to_broadcast([128, 48, 16]), Q1[1]), ALU.mult)
            tt(u2, bbi, (Q2[0][:, :, n:n + 1].to_broadcast([128, 48, 16]), Q2[1]), ALU.mult)
            tt(sub(WSb, WSb[0][:, sl, :, :]), u1, u2, ALU.add)
        BBs = tl([128, 48, 16], BF16)
        ts(u1, bbi, mbot[0], ALU.mult, extra=[mbot[1]])
        stt(BBs, bbr, mtop[0], u1, ALU.mult, ALU.add, extra=[mtop[1]])
        tmpk = tl([128, 128])
        for o in range(6):
            for half in range(2):
                ps, dps = self.bank()
                fns = [lambda e, o=o, lag=lag, ps=ps: e.matmul(
                    ps[:, 128 * (lag % 4):128 * (lag % 4) + 128], lhsT=BBs[0][:, 8 * o:8 * o + 8, :].rearrange("p g c -> p (g c)"),
                    rhs=CA[0][:, lag, 8 * o:8 * o + 8, :].rearrange("p g c -> p (g c)"), start=True, stop=True)
                    for lag in range(4 * half, 4 * half + 4)]
                P.group("pe", fns, [BBs[1], CA[1]], [dps])
                for lag in range(4 * half, 4 * half + 4):
                    src = ps[:, 128 * (lag % 4):128 * (lag % 4) + 128]
                    if lag == 0:
                        P.op("dve", lambda e, src=src: e.tensor_tensor(out=tmpk[0], in0=src, in1=BD[0], op=ALU.mult),
                             [dps, BD[1]], [tmpk[1]])
                        P.op("dve", lambda e, o=o: e.scalar_tensor_tensor(
                            out=KB_sb[:, o, 0, :], in0=ident, scalar=self.gains[:, 48 + o:49 + o], in1=tmpk[0],
                            op0=ALU.mult, op1=ALU.add), [tmpk[1], dconst], [dS5])
                    else:
                        P.op("dve", lambda e, src=src, o=o, lag=lag: e.tensor_tensor(out=KB_sb[:, o, lag, :], in0=src, in1=BD[0], op=ALU.mult),
                             [dps, BD[1]], [dS5])
        for o in range(6):
            ps, dps = self.bank()
            psb = ps.bitcast(BF16)
            fns = [lambda e, o=o, sl=sl, psb=psb: e.transpose(
                psb[:, 128 * sl:128 * sl + 128], WSb[0][:, sl, 8 * o:8 * o + 8, :].rearrange("p g c -> p (g c)"), ident_bf)
                for sl in range(8)]
            P.group("pe", fns, [WSb[1], dconst], [dps])
            for par, m in ((0, me), (1, mo)):
                P.op("dve", lambda e, o=o, par=par, m=m, psb=psb: e.tensor_scalar(
                    out=WS_sb[:, o, :, par, :], in0=psb.rearrange("p (s n) -> p s n", s=8), scalar1=m[0], scalar2=None, op0=ALU.mult),
                    [dps, m[1]], [dS5])
        P.op("pool", lambda e: e.memset(WY_sb.rearrange("p g s c -> p (g s c)"), 0.0), [], [dS5])
        CAv = CA[0].rearrange("p n (g two) c -> p n g two c", two=2)
        WYv = WY_sb.rearrange("p (g two) s c -> p g two s c", two=2)
        for sl in range(8):
            for par in range(2):
                P.op("dve", lambda e, sl=sl, par=par: e.tensor_copy(
                    out=WYv[:, :, par, sl, 16 * par:16 * par + 16], in_=CAv[:, sl + 1, :, par, :]), [CA[1]], [dS5])

    def _finish_dbg(self, sb):
        P, nc = self.P, self.nc
        for name, (ap, dep, shape, dt) in sb.items():
            o = nc.dram_tensor("dbg_" + name, list(shape), dt, kind="ExternalOutput").ap()
            self.outs.append("dbg_" + name)
            P.dma("sp", lambda e, o=o, ap=ap: e.dma_start(out=o, in_=ap), [dep], [Dep()])
        P.finish()
        return nc


_INPUT_NAMES = ["x", "mem", "mem_norm_g", "w_mem_kv", "mix_norm_g", "s5_w_in", "s5_lambda_re", "s5_lambda_im", "s5_log_dt",
                "s5_b_re", "s5_b_im", "s5_c_re", "s5_c_im", "s5_d", "s5_w_glu", "s5_b_glu", "moba_w_in", "w_out",
                "ffn_norm_g", "w_gate", "w_up", "w_down", "final_norm_g"]


def make_in_maps(inputs, ncores=8):
    maps = []
    shared = {}
    for k in _INPUT_NAMES:
        if k in ("x", "mem"):
            continue
        a = np.ascontiguousarray(np.asarray(inputs[k], dtype=np.float32))
        if k in ("s5_w_in", "s5_lambda_re", "s5_lambda_im", "s5_log_dt", "s5_b_re", "s5_b_im", "s5_c_re", "s5_c_im",
                 "s5_d", "s5_w_glu", "s5_b_glu", "moba_w_in"):
            a = a[0]
        shared[k] = np.ascontiguousarray(a)
    xs = np.asarray(inputs["x"], dtype=np.float32)
    ms = np.asarray(inputs["mem"], dtype=np.float32)
    for c in range(ncores):
        m = dict(shared)
        m["x"] = np.ascontiguousarray(xs[c])
        m["mem"] = np.ascontiguousarray(ms[c])
        maps.append(m)
    return maps


def kernel(**inputs):
    b = Builder()
    nc = b.build()
    res = run_bass_kernel_spmd(nc, make_in_maps(inputs), core_ids=list(range(8)))
    return np.stack([np.asarray(r["out"]) for r in res.results], axis=0).astype(np.float32)
```

```python
import math
import numpy as np
import concourse.bass as bass
import concourse.mybir as mybir
from concourse.bass_utils import run_bass_kernel_spmd

F32 = mybir.dt.float32
BF16 = mybir.dt.bfloat16
I32 = mybir.dt.int32
U8 = mybir.dt.uint8
AF = mybir.ActivationFunctionType
ALU = mybir.AluOpType
AX = mybir.AxisListType

T = 4096
D = 1024
NEG = -30000.0
MAGIC = 12582912.0


class Dep:
    __slots__ = ("w", "r")

    def __init__(self):
        self.w = None
        self.r = []


class Prog:
    ENGS = ("pe", "act", "dve", "pool", "sp")

    def __init__(self, nc, n_dma_sems=44):
        self.nc = nc
        self.streams = {e: [] for e in self.ENGS}
        self.sem = {e: nc.alloc_semaphore("s_" + e) for e in self.ENGS if e != "sp"}
        self.cnt = {e: 0 for e in self.ENGS}
        self.known = {e: {} for e in self.ENGS}
        self.dsems = [nc.alloc_semaphore("d%d" % i) for i in range(n_dma_sems)]
        self.dcnt = [0] * n_dma_sems
        n_sw = 12
        self.dq = {"pool": list(range(n_sw)), "sp": list(range(n_sw, n_dma_sems))}
        self.dqn = {"pool": 0, "sp": 0}
        self.tg = 0

    def _waits(self, eng, reads, writes):
        w = {}

        def add(t):
            if t is None:
                return
            s, v = t
            k = id(s)
            if k not in w or w[k][1] < v:
                w[k] = (s, v)

        def addw(t):
            if isinstance(t, list):
                for x in t:
                    add(x)
            else:
                add(t)

        for d in reads:
            addw(d.w)
        for d in writes:
            addw(d.w)
            for t in d.r:
                add(t)
        out = []
        kn = self.known[eng]
        own = id(self.sem["pe"]) if eng == "pe" else None
        for k, (s, v) in w.items():
            if k == own or kn.get(k, 0) >= v:
                continue
            kn[k] = v
            out.append((s, v))
        return out

    def _commit(self, tok, reads, writes):
        for d in reads:
            d.r.append(tok)
            if len(d.r) > 64:
                d.r = _compress(d.r)
        for d in writes:
            d.w = tok
            d.r = []

    def op(self, eng, fn, reads=(), writes=()):
        waits = self._waits(eng, reads, writes)
        self.cnt[eng] += 1
        tok = (self.sem[eng], self.cnt[eng])
        self.streams[eng].append((fn, waits, tok, 1))
        self._commit(tok, reads, writes)
        return tok

    def group(self, eng, fns, reads=(), writes=()):
        n = len(fns)
        if n == 1:
            return self.op(eng, fns[0], reads, writes)
        waits = self._waits(eng, reads, writes)
        self.streams[eng].append((fns[0], waits, None, 0))
        for fn in fns[1:-1]:
            self.streams[eng].append((fn, [], None, 0))
        self.cnt[eng] += 1
        tok = (self.sem[eng], self.cnt[eng])
        self.streams[eng].append((fns[-1], [], tok, 1))
        self._commit(tok, reads, writes)
        return tok

    def dma(self, q, fn, reads=(), writes=()):
        ql = self.dq[q]
        i = ql[self.dqn[q] % len(ql)]
        self.dqn[q] += 1
        s = self.dsems[i]
        waits = self._waits(q, reads, writes)
        if self.dcnt[i] > 0:
            kn = self.known[q]
            if kn.get(id(s), 0) < self.dcnt[i]:
                kn[id(s)] = self.dcnt[i]
                waits.append((s, self.dcnt[i]))
        self.dcnt[i] += 16
        tok = (s, self.dcnt[i])
        self.streams[q].append((fn, waits, tok, 16))
        self._commit(tok, reads, writes)
        return tok

    def dma_multi(self, q, fns, reads=(), writes=()):
        first_waits = self._waits(q, reads, writes)
        toks = []
        for n, fn in enumerate(fns):
            ql = self.dq[q]
            i = ql[self.dqn[q] % len(ql)]
            self.dqn[q] += 1
            s = self.dsems[i]
            waits = list(first_waits) if n == 0 else []
            if self.dcnt[i] > 0:
                kn = self.known[q]
                if kn.get(id(s), 0) < self.dcnt[i]:
                    kn[id(s)] = self.dcnt[i]
                    waits.append((s, self.dcnt[i]))
            self.dcnt[i] += 16
            tok = (s, self.dcnt[i])
            self.streams[q].append((fn, waits, tok, 16))
            toks.append(tok)
        for d in reads:
            d.r.extend(toks)
        for d in writes:
            d.w = list(toks)
            d.r = []
        return toks

    def barrier(self):
        toks = [(self.sem[e], self.cnt[e]) for e in self.sem if self.cnt[e] > 0]
        toks += [(s, c) for s, c in zip(self.dsems, self.dcnt) if c > 0]
        for e in self.ENGS:
            kn = self.known[e]
            ws = []
            for s, v in toks:
                if kn.get(id(s), 0) < v:
                    kn[id(s)] = v
                    ws.append((s, v))
            if ws:
                self.streams[e].append((None, ws, None, 0))

    def finish(self):
        self.barrier()
        nc = self.nc
        streams = self.streams
        eng_sems = {id(s) for s in self.sem.values()}
        needed = {}
        for name in self.ENGS:
            for fn, waits, tok, inc in streams[name]:
                for s, v in waits:
                    if id(s) in eng_sems:
                        needed.setdefault(id(s), set()).add(v)
        rank = {k: {v: i + 1 for i, v in enumerate(sorted(vs))} for k, vs in needed.items()}
        self.n_inc = sum(len(v) for v in needed.values())

        def replay(name, e):
            for fn, waits, tok, inc in streams[name]:
                for s, v in waits:
                    if id(s) in eng_sems:
                        e.wait_ge(s, rank[id(s)][v])
                    else:
                        e.wait_ge(s, v)
                if fn is None:
                    continue
                ins = fn(e)
                if tok is not None:
                    if id(tok[0]) in eng_sems:
                        if tok[1] in needed.get(id(tok[0]), ()):
                            ins.then_inc(tok[0], 1)
                    else:
                        ins.then_inc(tok[0], inc)

        with nc.Block() as block:
            @block.tensor
            def _(e):
                replay("pe", e)

            @block.scalar
            def _(e):
                replay("act", e)

            @block.vector
            def _(e):
                replay("dve", e)

            @block.gpsimd
            def _(e):
                replay("pool", e)

            @block.sync
            def _(e):
                replay("sp", e)


def _compress(toks):
    best = {}
    for s, v in toks:
        k = id(s)
        if k not in best or best[k][1] < v:
            best[k] = (s, v)
    return list(best.values())


_ES = {F32: 4, BF16: 2, I32: 4}


class Arena:
    def __init__(self, nc, nbytes):
        self.base = nc.alloc_sbuf_tensor("arena", [128, nbytes], U8).ap()
        self.n = nbytes
        self.limit = nbytes
        self.off = 0

    def alloc(self, shape, dt):
        es = _ES[dt]
        free = 1
        for s in shape[1:]:
            free *= s
        nb = free * es
        off = (self.off + 63) // 64 * 64
        assert off + nb <= self.limit, ("arena overflow", off, nb, self.limit)
        self.off = off + nb
        ap = self.base[0:shape[0], off:off + nb].bitcast(dt)
        if len(shape) == 3:
            ap = ap.rearrange("p (a b) -> p a b", a=shape[1])
        elif len(shape) == 4:
            ap = ap.rearrange("p (a b c) -> p a b c", a=shape[1], b=shape[2])
        elif len(shape) == 5:
            ap = ap.rearrange("p (a b c d) -> p a b c d", a=shape[1], b=shape[2], c=shape[3])
        return ap

    def alloc_top(self, shape, dt):
        es = _ES[dt]
        free = 1
        for s in shape[1:]:
            free *= s
        nb = (free * es + 63) // 64 * 64
        self.limit -= nb
        assert self.limit >= self.off, ("arena top overflow", self.limit, self.off)
        off = self.limit
        ap = self.base[0:shape[0], off:off + free * es].bitcast(dt)
        if len(shape) == 3:
            ap = ap.rearrange("p (a b) -> p a b", a=shape[1])
        return ap

    def release_top(self):
        self.limit = self.n

    def mark(self):
        return self.off

    def release(self, m):
        self.off = m


class Builder:
    def __init__(self, dbg=(), stop=None):
        self.dbg = set(dbg)
        self.stop = stop
        nc = self.nc = bass.Bass("TRN2", target_bir_lowering=False)
        self.P = Prog(nc)
        self.A = Arena(nc, 207 * 1024)
        self.banks = [nc.alloc_psum_tensor("psb%d" % i, [128, 512], F32).ap() for i in range(8)]
        self.bdep = [Dep() for _ in range(8)]
        self.bnext = 0
        self.bank_set = list(range(8))
        self.etog = 0
        self.inp = {}
        self.outs = []

    def din(self, name, shape):
        ap = self.nc.dram_tensor(name, list(shape), F32, kind="ExternalInput").ap()
        self.inp[name] = ap
        return ap

    def scratch(self, name, shape, dt):
        if name in self.dbg:
            self.outs.append(name)
            return self.nc.dram_tensor(name, list(shape), dt, kind="ExternalOutput").ap()
        return self.nc.dram_tensor(name, list(shape), dt).ap()

    def bank(self):
        bs = self.bank_set
        i = bs[self.bnext % len(bs)]
        self.bnext += 1
        return self.banks[i], self.bdep[i]

    def evac(self, out, in_, reads, writes, scale=None, eng=None):
        P = self.P
        if eng is None:
            eng = "act" if self.etog == 0 else "dve"
            self.etog ^= 1
        if eng == "act":
            sc = 1.0 if scale is None else scale
            return P.op("act", lambda e: e.activation(out=out, in_=in_, func=AF.Copy, scale=sc), reads, writes)
        if scale is None:
            return P.op(eng, lambda e: e.tensor_copy(out=out, in_=in_), reads, writes)
        return P.op(eng, lambda e: e.tensor_scalar(out=out, in0=in_, scalar1=scale, scalar2=None, op0=ALU.mult), reads, writes)

    def load_w(self, dst, dd, src2d, kt, ncols, c0=0, r0=0):
        P = self.P
        fns = []
        for k in range(kt):
            for c in range(0, ncols, 2048):
                cw = min(2048, ncols - c)
                fns.append(lambda e, k=k, c=c, cw=cw: e.dma_start(
                    out=dst[:, k, c:c + cw], in_=src2d[r0 + k * 128:r0 + (k + 1) * 128, c0 + c:c0 + c + cw]))
        P.dma_multi("pool", fns, writes=[dd])

    def rmsnorm(self, xT, dx, gcol, out, dout, n, tmp):
        P = self.P
        sq, dsq, rt, drt, rs, drs = tmp
        P.op("act", lambda e: e.activation(out=sq[:, :, 0:n], in_=xT, func=AF.Square), [dx], [dsq])
        ps, dps = self.bank()
        fns = [lambda e, f=f: e.matmul(ps[:, 0:n], lhsT=self.ones_bf, rhs=sq[:, f, 0:n], start=(f == 0), stop=(f == 7))
               for f in range(8)]
        P.group("pe", fns, [dsq, self.dconst], [dps])
        P.op("act", lambda e: e.activation(out=rt[:, 0:n], in_=ps[:, 0:n], func=AF.Sqrt, bias=self.eps_col, scale=1.0 / D),
             [dps, self.dconst], [drt])
        P.op("dve", lambda e: e.reciprocal(out=rs[:, 0:n], in_=rt[:, 0:n]), [drt], [drs])
        for f in range(8):
            P.op("dve", lambda e, f=f: e.scalar_tensor_tensor(
                out=out[:, f, :], in0=xT[:, f, :], scalar=self.gains[:, gcol + f:gcol + f + 1], in1=rs[:, 0:n],
                op0=ALU.mult, op1=ALU.mult), [dx, drs, self.dconst], [dout])

    def rms_tmp(self, n):
        A = self.A
        return (A.alloc([128, 8, n], BF16), Dep(), A.alloc([128, n], F32), Dep(), A.alloc([128, n], F32), Dep())

    def build(self):
        nc, P, A = self.nc, self.P, self.A
        din = self.din
        x = din("x", [T, D])
        mem = din("mem", [256, D])
        mem_norm_g = din("mem_norm_g", [D])
        w_mem_kv = din("w_mem_kv", [D, 512])
        mix_norm_g = din("mix_norm_g", [2, D])
        s5_w_in = din("s5_w_in", [D, D])
        lam_re = din("s5_lambda_re", [48, 64])
        lam_im = din("s5_lambda_im", [48, 64])
        log_dt = din("s5_log_dt", [48])
        b_re = din("s5_b_re", [48, 64, 16])
        b_im = din("s5_b_im", [48, 64, 16])
        c_re = din("s5_c_re", [48, 16, 64])
        c_im = din("s5_c_im", [48, 16, 64])
        s5_d = din("s5_d", [768])
        w_glu = din("s5_w_glu", [768, 768])
        b_glu = din("s5_b_glu", [768])
        moba_w_in = din("moba_w_in", [D, 2560])
        w_out = din("w_out", [2, D, D])
        ffn_norm_g = din("ffn_norm_g", [2, D])
        w_gate = din("w_gate", [2, D, 2816])
        w_up = din("w_up", [2, D, 2816])
        w_down = din("w_down", [2, 2816, D])
        final_norm_g = din("final_norm_g", [D])
        out = nc.dram_tensor("out", [T, D], F32, kind="ExternalOutput").ap()
        self.outs.append("out")

        XT0 = self.scratch("XT0", [8, 128, T], F32)
        XT1 = self.scratch("XT1", [8, 128, T], F32)
        XT2 = self.scratch("XT2", [8, 128, T], F32)
        XT3 = self.scratch("XT3", [8, 128, T], F32)
        ATD = [self.scratch("ATD%d" % l, [8, 128, T], BF16) for l in range(2)]
        HTD = [self.scratch("HTD%d" % l, [8, 128, T], BF16) for l in range(2)]
        YTD = self.scratch("YTD", [6, 128, T], BF16)
        QMD = self.scratch("QMD", [2, 128, T], BF16)
        ALD = self.scratch("ALD", [12, 3, T], BF16)
        NALD = self.scratch("NALD", [12, 3, T], BF16)

        self.DEND = self.scratch("DEND", [4, 512], F32)
        self.dconst = dconst = Dep()
        ident = A.alloc([128, 128], F32)
        ident_bf = A.alloc([128, 128], BF16)
        self.ones_bf = ones_bf = A.alloc([128, 128], BF16)
        self.ones_f = ones_f = A.alloc([128, 64], F32)
        self.eps_col = eps_col = A.alloc([128, 1], F32)
        self.gains = gains = A.alloc([128, 64], F32)
        memKT = A.alloc([128, 2, 256], BF16)
        memVA = A.alloc([128, 2, 4, 65], BF16)
        dmemK, dmemV = Dep(), Dep()
        self.ident, self.ident_bf = ident, ident_bf

        P.op("pool", lambda e: e.memset(ident, 0.0), [], [dconst])
        P.op("pool", lambda e: e.affine_select(out=ident, in_=ident, pattern=[[-1, 128]], compare_op=ALU.not_equal,
                                                fill=1.0, base=0, channel_multiplier=1), [dconst], [dconst])
        P.op("pool", lambda e: e.tensor_copy(out=ident_bf, in_=ident), [dconst], [dconst])
        P.op("pool", lambda e: e.memset(ones_bf, 1.0), [], [dconst])
        P.op("pool", lambda e: e.memset(ones_f, 1.0), [], [dconst])
        P.op("pool", lambda e: e.memset(eps_col, 1e-6), [], [dconst])

        m0 = A.mark()
        grow = A.alloc([64, 128], F32)
        dgrow = Dep()
        P.op("pool", lambda e: e.memset(grow, 0.0), [], [dgrow])
        srcs = [mix_norm_g[0], mix_norm_g[1], ffn_norm_g[0], ffn_norm_g[1], final_norm_g, mem_norm_g]
        for i, s in enumerate(srcs):
            P.dma("sp", lambda e, i=i, s=s: e.dma_start(out=grow[8 * i:8 * i + 8, :], in_=s.rearrange("(f p) -> f p", p=128)),
                  [], [dgrow])
        P.dma("sp", lambda e: e.dma_start(out=grow[48:54, :], in_=s5_d.rearrange("(f p) -> f p", p=128)), [], [dgrow])
        P.dma("sp", lambda e: e.dma_start(out=grow[54:60, :], in_=b_glu.rearrange("(f p) -> f p", p=128)), [], [dgrow])
        ps, dps = self.bank()
        P.op("pe", lambda e: e.transpose(ps[:, 0:64], grow, ident[0:64, 0:64]), [dgrow, dconst], [dps])
        P.op("dve", lambda e: e.tensor_copy(out=gains, in_=ps[:, 0:64]), [dps], [dconst])

        memt = A.alloc([128, 2, D], F32)
        dmemt = Dep()
        P.dma("sp", lambda e: e.dma_start(out=memt, in_=mem.rearrange("(m p) f -> p m f", p=128)), [], [dmemt])
        wkv = A.alloc([128, 8, 512], BF16)
        dwkv = Dep()
        self.load_w(wkv, dwkv, w_mem_kv, 8, 512)
        memT = A.alloc([128, 8, 256], F32)
        dmemT = Dep()
        for fp in range(4):
            ps, dps = self.bank()
            fns = []
            for fl in range(2):
                for m in range(2):
                    f = 2 * fp + fl
                    fns.append(lambda e, f=f, m=m, fl=fl, ps=ps: e.transpose(
                        ps[:, fl * 256 + m * 128: fl * 256 + (m + 1) * 128], memt[:, m, f * 128:(f + 1) * 128], ident))
            P.group("pe", fns, [dmemt, dconst], [dps])
            self.evac(memT[:, 2 * fp:2 * fp + 2, :].rearrange("p a b -> p (a b)"), ps, [dps], [dmemT])
        tmpn = self.rms_tmp(256)
        hmT = A.alloc([128, 8, 256], BF16)
        dhm = Dep()
        self.rmsnorm(memT, dmemT, 40, hmT, dhm, 256, tmpn)
        for hp in range(2):
            ps, dps = self.bank()
            fns = [lambda e, k=k, hp=hp, ps=ps: e.matmul(ps[:, 0:256], lhsT=wkv[:, k, hp * 128:(hp + 1) * 128], rhs=hmT[:, k, :],
                                                          start=(k == 0), stop=(k == 7)) for k in range(8)]
            P.group("pe", fns, [dwkv, dhm], [dps])
            self.evac(memKT[:, hp, :], ps[:, 0:256], [dps], [dmemK])
        P.op("pool", lambda e: e.memset(memVA[:, :, :, 64:65], 1.0), [], [dmemV])
        for m in range(2):
            ps, dps = self.bank()
            fns = [lambda e, k=k, m=m, ps=ps: e.matmul(ps[:, 0:256], lhsT=hmT[:, k, m * 128:(m + 1) * 128], rhs=wkv[:, k, 256:512],
                                                        start=(k == 0), stop=(k == 7)) for k in range(8)]
            P.group("pe", fns, [dwkv, dhm], [dps])
            self.evac(memVA[:, m, :, 0:64], ps[:, 0:256].rearrange("p (h d) -> p h d", h=4), [dps], [dmemV])
        P.barrier()
        A.release(m0)
        if self.stop == "M":
            return self._finish_dbg({"memKT": (memKT, dmemK, [128, 2, 256], BF16), "memVA": (memVA, dmemV, [128, 2, 4, 65], BF16),
                                     "gains": (gains, dconst, [128, 64], F32)})

        g_mix0, g_mix1, g_ffn0, g_ffn1, g_fin, g_mem = 0, 8, 16, 24, 32, 40
        mU = A.mark()
        UT = A.alloc([128, 6, T], BF16)
        dUT = [[Dep() for _ in range(8)] for _ in range(6)]
        mA = A.mark()
        QM = A.alloc([128, 2, T], BF16)
        dQM = [Dep() for _ in range(8)]
        dQMD = Dep()
        win = A.alloc([128, 8, D], BF16)
        dwin = Dep()
        self.load_w(win, dwin, s5_w_in, 8, D)
        xrow = [A.alloc([128, 4, D], F32) for _ in range(2)]
        dxrow = [Dep(), Dep()]
        xT = [A.alloc([128, 8, 512], F32) for _ in range(2)]
        dxT = [Dep(), Dep()]
        hT = [A.alloc([128, 8, 512], BF16) for _ in range(2)]
        dhT = [Dep(), Dep()]
        tmpn = self.rms_tmp(512)
        xv = x.rearrange("(c j p) f -> c p j f", p=128, j=4)
        UTv = [UT[:, ot, :].rearrange("p (s k) -> p s k", s=8) for ot in range(6)]
        def stage1(sl):
            b2 = sl % 2
            P.dma("sp", lambda e, sl=sl, b2=b2: e.dma_start(out=xrow[b2], in_=xv[sl]), [], [dxrow[b2]])
            for f in range(8):
                ps, dps = self.bank()
                fns = [lambda e, j=j, f=f, ps=ps, b2=b2: e.transpose(ps[:, j * 128:(j + 1) * 128], xrow[b2][:, j, f * 128:(f + 1) * 128], ident)
                       for j in range(4)]
                P.group("pe", fns, [dxrow[b2], dconst], [dps])
                self.evac(xT[b2][:, f, :], ps, [dps], [dxT[b2]])
            P.dma("sp", lambda e, sl=sl, b2=b2: e.dma_start(
                out=XT0[:, :, sl * 512:(sl + 1) * 512].rearrange("f p n -> p f n"), in_=xT[b2]), [dxT[b2]], [Dep()])
            self.rmsnorm(xT[b2], dxT[b2], g_mix0, hT[b2], dhT[b2], 512, tmpn)
        def stage2(sl):
            b2 = sl % 2
            for ot in range(8):
                ps, dps = self.bank()
                fns = [lambda e, k=k, ot=ot, ps=ps, b2=b2: e.matmul(ps, lhsT=win[:, k, ot * 128:(ot + 1) * 128], rhs=hT[b2][:, k, :],
                                                                   start=(k == 0), stop=(k == 7)) for k in range(8)]
                P.group("pe", fns, [dwin, dhT[b2]], [dps])
                if ot < 6:
                    self.evac(UTv[ot][:, :, 64 * sl:64 * sl + 64], ps.rearrange("p (k s) -> p s k", s=8), [dps], dUT[ot])
                else:
                    self.evac(QM[:, ot - 6, sl * 512:(sl + 1) * 512], ps, [dps], [dQM[sl]], scale=0.125)
            P.dma("sp", lambda e, sl=sl: e.dma_start(out=QMD[:, :, sl * 512:(sl + 1) * 512].rearrange("f p n -> p f n"),
                                                     in_=QM[:, :, sl * 512:(sl + 1) * 512]), [dQM[sl]], [dQMD])
        stage1(0)
        for sl in range(8):
            if sl + 1 < 8:
                stage1(sl + 1)
            stage2(sl)
        P.barrier()
        A.release(mA)
        if self.stop == "A":
            dall = Dep()
            return self._finish_dbg({"UT": (UT, dall, [128, 6, T], BF16)})

        WS_sb = A.alloc([128, 6, 8, 2, 128], BF16)
        WY_sb = A.alloc([128, 48, 8, 32], BF16)
        KB_sb = A.alloc([128, 6, 8, 128], BF16)
        V12 = A.alloc([128, 48, 9, 2], F32)
        II = A.alloc([128, 64], BF16)
        dS5 = Dep()
        mS = A.mark()
        self._s5_setup(lam_re, lam_im, log_dt, b_re, b_im, c_re, c_im, WS_sb, WY_sb, KB_sb, V12, II, dS5)
        P.barrier()
        A.release(mS)
        if self.stop == "S":
            return self._finish_dbg({"WS": (WS_sb, dS5, [128, 6, 8, 2, 128], BF16), "WY": (WY_sb, dS5, [128, 48, 8, 32], BF16),
                                     "KB": (KB_sb, dS5, [128, 6, 8, 128], BF16), "V12": (V12, dS5, [128, 48, 9, 2], F32)})

        Xab = A.alloc([128, 8, 2, 512], BF16)
        dX = [[Dep(), Dep()] for _ in range(8)]
        XP = A.alloc([128, 2, 8, 512], BF16)
        dXP = [[Dep() for _ in range(8)] for _ in range(2)]
        DG = A.alloc([128, 8, 2, 128], BF16)
        dDG = [[Dep(), Dep()] for _ in range(8)]
        P.op("pool", lambda e: e.memset(XP[:, :, :, 0:1], 0.0), [], [d for r in dXP for d in r])
        def y_group(o, so):
            ob = o % 2
            ps, dps = self.bank()
            fns = []
            for si in range(so + 1):
                fns.append(lambda e, si=si: e.matmul(
                    ps, lhsT=KB_sb[:, o, so - si, :], rhs=UT[:, o, si * 512:(si + 1) * 512], start=(si == 0), stop=False))
            for g8 in range(8):
                j = g8 // 2
                fns.append(lambda e, g8=g8, j=j: e.matmul(
                    ps[32 * j:32 * j + 32, :], lhsT=WY_sb[:, 8 * o + g8, so, :], rhs=XP[:, ob, g8, :],
                    start=False, stop=(g8 == 7), tile_position=(0, 32 * j)))
            P.group("pe", fns, [dS5] + dUT[o][:so + 1] + dXP[ob], [dps])
            P.op("act", lambda e: e.activation(out=UT[:, o, so * 512:(so + 1) * 512], in_=ps, func=AF.Gelu), [dps], [dUT[o][so]])

        for o in range(6):
            ob = o % 2
            ylist = [(o - 1, so) for so in range(7, -1, -1)] if o > 0 else []
            for g8 in range(8):
                j, par = g8 // 2, g8 % 2
                ps, dps = self.bank()
                fns = [lambda e, sl=sl, j=j, par=par, o=o, ps=ps: e.matmul(
                    ps, lhsT=WS_sb[32 * j:32 * j + 32, o, sl, par, :], rhs=UT[32 * j:32 * j + 32, o, sl * 512:(sl + 1) * 512],
                    start=(sl == 0), stop=(sl == 7), tile_position=(32 * j, 0)) for sl in range(8)]
                P.group("pe", fns, [dS5] + dUT[o], [dps])
                self.evac(Xab[:, g8, 0, :], ps, [dps], [dX[g8][0]])
            cur = 0
            for st in range(9):
                dsh = 1 << st
                for g8 in range(8):
                    g = 8 * o + g8
                    db = st % 2
                    P.op("dve", lambda e, g=g, g8=g8, st=st, db=db: e.tensor_scalar(
                        out=DG[:, g8, db, 0:64], in0=II, scalar1=V12[:, g, st, 0:1], scalar2=None, op0=ALU.mult),
                        [dS5], [dDG[g8][db]])
                    P.op("dve", lambda e, g=g, g8=g8, st=st, db=db: e.tensor_scalar(
                        out=DG[:, g8, db, 64:128], in0=II, scalar1=V12[:, g, st, 1:2], scalar2=None, op0=ALU.mult),
                        [dS5], [dDG[g8][db]])
                    ps, dps = self.bank()
                    fns = [lambda e, g8=g8, cur=cur, ps=ps: e.matmul(ps, lhsT=ident_bf, rhs=Xab[:, g8, cur, :], start=True, stop=False),
                           lambda e, g8=g8, cur=cur, ps=ps, dsh=dsh, db=db: e.matmul(
                               ps[:, dsh:512], lhsT=DG[:, g8, db, :], rhs=Xab[:, g8, cur, 0:512 - dsh], start=False, stop=True)]
                    P.group("pe", fns, [dX[g8][cur], dDG[g8][db], dconst], [dps])
                    ev = "dve" if g8 in (3, 7) else "act"
                    if st < 8:
                        self.evac(Xab[:, g8, 1 - cur, :], ps, [dps], [dX[g8][1 - cur]], eng=ev)
                    else:
                        self.evac(XP[:, ob, g8, 1:512], ps[:, 0:511], [dps], [dXP[ob][g8]], eng=ev)
                if ylist:
                    y_group(*ylist.pop(0))
                cur = 1 - cur
            while ylist:
                y_group(*ylist.pop(0))
        for so in range(7, -1, -1):
            y_group(5, so)
        P.barrier()
        A.release(mA)
        if self.stop == "B":
            return self._finish_dbg({"UT": (UT, Dep(), [128, 6, T], BF16)})

        wglu = A.alloc([128, 6, 768], BF16)
        dwglu = Dep()
        self.load_w(wglu, dwglu, w_glu, 6, 768)
        wts = self.ffn_prefetch(w_gate[0], w_up[0])
        self.attn_init(depth=4, obanks=(0, 1, 3), dbbank=2)
        QMc = [A.alloc([128, 2, 512], BF16) for _ in range(2)]
        dQMc = [Dep(), Dep()]
        SG = [A.alloc([128, 512], BF16) for _ in range(2)]
        dSG = [Dep(), Dep()]
        TOKc = [A.alloc([128, 6, 512], BF16) for _ in range(2)]
        dTOK = [Dep(), Dep()]
        dATD = [Dep(), Dep()]
        for sl in range(8):
            b2 = sl % 2
            cs = slice(sl * 512, (sl + 1) * 512)
            P.dma("sp", lambda e, cs=cs, b2=b2: e.dma_start(out=QMc[b2], in_=QMD[:, :, cs].rearrange("f p n -> p f n")),
                  [dQMD], [dQMc[b2]])
            self.bank_set = [4, 5, 6, 7]
            ks = slice(64 * sl, 64 * sl + 64)
            dUall = [d for r in dUT for d in r]
            for ot in range(6):
                ps, dps = self.bank()
                fns = [lambda e, k=k, ot=ot, ps=ps, ks=ks: e.matmul(ps, lhsT=wglu[:, k, ot * 128:(ot + 1) * 128], rhs=UTv[k][:, :, ks],
                                                                   start=(k == 0), stop=(k == 5)) for k in range(6)]
                P.group("pe", fns, [dwglu] + dUall, [dps])
                sb = ot % 2
                P.op("act", lambda e, ot=ot, ps=ps, sb=sb: e.activation(out=SG[sb], in_=ps, func=AF.Sigmoid,
                                                                     bias=gains[:, 54 + ot:55 + ot]), [dps, dconst], [dSG[sb]])
                P.op("dve", lambda e, ot=ot, ks=ks, sb=sb, b2=b2: e.tensor_tensor(
                    out=TOKc[b2][:, ot, :].rearrange("p (k s) -> p s k", s=8), in0=UTv[ot][:, :, ks],
                    in1=SG[sb].rearrange("p (s k) -> p s k", s=8), op=ALU.mult), [dSG[sb]] + dUall, [dTOK[b2]])
            P.dma("sp", lambda e, cs=cs, b2=b2: e.dma_start(out=ATD[0][0:6, :, cs].rearrange("f p n -> p f n"), in_=TOKc[b2]),
                  [dTOK[b2]], [Dep()])
            for h in range(4):
                hb, hp = 64 * (h % 2), h // 2
                self.mem_unit(QMc[b2][hb:hb + 64, hp, :], dQMc[b2],
                              [memKT[hb:hb + 64, hp, j * 128:(j + 1) * 128] for j in range(2)],
                              [memVA[:, j, h, 0:64] for j in range(2)], [dmemK, dmemV],
                              ATD[0][6 + hp, hb:hb + 64, cs], dATD[0])
        self.bank_set = list(range(8))
        P.barrier()
        A.release(mU)
        if self.stop == "C":
            return self._finish_dbg({})

        H2D = self.scratch("H2D", [8, 128, T], BF16)
        self.gf_phase(wts, w_out[0], w_down[0], ATD[0], XT0, XT2, g_ffn0, None, None, nxt=(g_mix1, H2D))
        if self.stop == "D0":
            return self._finish_dbg({})
        if self.stop == "D0":
            return self._finish_dbg({})

        mL = A.mark()
        POS = A.alloc([128, 32], F32)
        VAL = A.alloc([128, 12, 32], F32)
        R1 = A.alloc([128, 12, 32], F32)
        PC = A.alloc([128, 3, 12, 32], BF16)
        NPC = A.alloc([128, 3, 12, 32], BF16)
        dal = Dep()
        dALD = Dep()
        P.op("pool", lambda e: e.iota(POS, pattern=[[1, 32]], base=0, channel_multiplier=32, allow_small_or_imprecise_dtypes=True), [], [dal])
        for h in range(12):
            sl_h = float(np.float32(2.0 ** (-8.0 * (h + 1) / 12)))
            P.op("dve", lambda e, h=h, sl_h=sl_h: e.tensor_scalar(out=VAL[:, h, :], in0=POS, scalar1=sl_h, scalar2=None, op0=ALU.mult),
                 [dal], [dal])
        P.op("dve", lambda e: e.tensor_copy(out=PC[:, 0, :, :], in_=VAL), [dal], [dal])
        P.op("dve", lambda e: e.tensor_tensor(out=R1, in0=VAL, in1=PC[:, 0, :, :], op=ALU.subtract), [dal], [dal])
        P.op("dve", lambda e: e.tensor_copy(out=PC[:, 1, :, :], in_=R1), [dal], [dal])
        P.op("dve", lambda e: e.tensor_tensor(out=VAL, in0=R1, in1=PC[:, 1, :, :], op=ALU.subtract), [dal], [dal])
        P.op("dve", lambda e: e.tensor_copy(out=PC[:, 2, :, :], in_=VAL), [dal], [dal])
        P.op("dve", lambda e: e.tensor_scalar(out=NPC, in0=PC, scalar1=-1.0, scalar2=None, op0=ALU.mult), [dal], [dal])
        for c3 in range(3):
            P.dma("sp", lambda e, c3=c3: e.dma_start(out=ALD[:, c3, :].rearrange("h (p j) -> p h j", j=32), in_=PC[:, c3, :, :]), [dal], [dALD])
            P.dma("sp", lambda e, c3=c3: e.dma_start(out=NALD[:, c3, :].rearrange("h (p j) -> p h j", j=32), in_=NPC[:, c3, :, :]), [dal], [dALD])
        P.barrier()
        A.release(mL)

        H2T = A.alloc([128, 8, T], BF16)
        dH2 = [Dep() for _ in range(8)]
        P.dma_multi("sp", [lambda e, f=f: e.dma_start(out=H2T[:, f, :], in_=H2D[f]) for f in range(8)], writes=dH2)
        if self.stop == "E":
            return self._finish_dbg({"H2T": (H2T, Dep(), [128, 8, T], BF16)})

        CM = A.alloc([128, 4, 512], BF16)
        dCM = Dep()
        mAI = A.mark()
        self.attn_init(depth=3, obanks=(0, 1, 2), dbbank=2)
        P.op("pool", lambda e: e.memset(CM.rearrange("p a b -> p (a b)"), 0.0), [], [dCM])
        for jj in range(4):
            P.op("pool", lambda e, jj=jj: e.affine_select(out=CM[:, jj, :], in_=CM[:, jj, :], pattern=[[1, 512]], compare_op=ALU.is_ge,
                                                          fill=NEG, base=-128 * jj, channel_multiplier=-1), [dCM], [dCM])
        dATD1 = Dep()
        mF = A.mark()
        for hg in range(3):
            QA = A.alloc([128, 4, T], BF16)
            KA = A.alloc([128, 4, T], BF16)
            VA = A.alloc([128, 32, 4, 65], BF16)
            wq = A.alloc([128, 8, 256], BF16)
            wk = A.alloc([128, 8, 256], BF16)
            wv = A.alloc([128, 8, 256], BF16)
            KS = A.alloc([64, 4, 16], F32)
            KSb = A.alloc([64, 4, 16], BF16)
            GM = A.alloc([128, 4, 16], F32)
            M8 = A.alloc([128, 4, 8], F32)
            SEL = A.alloc([128, 4, 16], F32)
            BTok = A.alloc([128, 4, 4, 16], BF16)
            BTS = [A.alloc([64, 512], BF16) for _ in range(2)]
            dwq, dwk, dwv, dKS, dKSb, dGM, dM8, dSEL, dBTok = [Dep() for _ in range(9)]
            dBTS = [Dep(), Dep()]
            dQ = [Dep() for _ in range(8)]
            dK = [Dep() for _ in range(8)]
            dV = [Dep() for _ in range(8)]
            dBias = [Dep() for _ in range(8)]
            dQaug, dKaug, dVone = Dep(), Dep(), Dep()
            self.load_w(wq, dwq, moba_w_in, 8, 256, c0=256 * hg)
            self.load_w(wk, dwk, moba_w_in, 8, 256, c0=768 + 256 * hg)
            self.load_w(wv, dwv, moba_w_in, 8, 256, c0=1536 + 256 * hg)
            P.op("pool", lambda e, QA=QA: e.memset(QA[64:86, :, :].rearrange("p a b -> p (a b)"), 1.0), [], [dQaug])
            P.op("pool", lambda e, KA=KA: e.memset(KA[64:86, :, :].rearrange("p a b -> p (a b)"), 1.0), [], [dKaug])
            P.op("pool", lambda e, KA=KA: e.affine_select(out=KA[64:80, :, :], in_=KA[64:80, :, :], pattern=[[0, 4], [1, T]],
                                                          compare_op=ALU.is_ge, fill=0.0, base=0, channel_multiplier=-256), [dKaug], [dKaug])
            P.op("pool", lambda e, KA=KA: e.affine_select(out=KA[64:80, :, :], in_=KA[64:80, :, :], pattern=[[0, 4], [-1, T]],
                                                          compare_op=ALU.is_ge, fill=0.0, base=255, channel_multiplier=256), [dKaug], [dKaug])
            for hh in range(4):
                hd = 4 * hg + hh
                P.dma("sp", lambda e, hh=hh, hd=hd, QA=QA: e.dma_start(out=QA[80:83, hh, :], in_=NALD[hd]), [dALD, dQaug], [dQaug])
                P.dma("sp", lambda e, hh=hh, hd=hd, KA=KA: e.dma_start(out=KA[83:86, hh, :], in_=ALD[hd]), [dALD, dKaug], [dKaug])
            P.op("pool", lambda e, VA=VA: e.memset(VA[:, :, :, 64:65], 1.0), [], [dVone])
            self.bank_set = list(range(8))
            for c in range(8):
                cs = slice(c * 512, (c + 1) * 512)
                for hh in range(4):
                    ps, dps = self.bank()
                    fns = [lambda e, k=k, hh=hh, ps=ps, cs=cs, wq=wq: e.matmul(ps[0:64, :], lhsT=wq[:, k, 64 * hh:64 * hh + 64], rhs=H2T[:, k, cs],
                                                                            start=(k == 0), stop=(k == 7)) for k in range(8)]
                    P.group("pe", fns, [dwq, dH2[c]], [dps])
                    P.op("act", lambda e, hh=hh, ps=ps, cs=cs, QA=QA: e.activation(out=QA[0:64, hh, cs], in_=ps[0:64, :], func=AF.Copy, scale=0.125),
                         [dps], [dQ[c]])
                    ps, dps = self.bank()
                    fns = [lambda e, k=k, hh=hh, ps=ps, cs=cs, wk=wk: e.matmul(ps[0:64, :], lhsT=wk[:, k, 64 * hh:64 * hh + 64], rhs=H2T[:, k, cs],
                                                                            start=(k == 0), stop=(k == 7)) for k in range(8)]
                    P.group("pe", fns, [dwk, dH2[c]], [dps])
                    P.op("dve", lambda e, hh=hh, ps=ps, cs=cs, KA=KA: e.tensor_copy(out=KA[0:64, hh, cs], in_=ps[0:64, :]), [dps], [dK[c]])
                    P.op("dve", lambda e, hh=hh, ps=ps, c=c, KS=KS: e.tensor_reduce(
                        out=KS[:, hh, 2 * c:2 * c + 2], in_=ps[0:64, :].rearrange("p (b n) -> p b n", b=2), axis=AX.X, op=ALU.add), [dps], [dKS])
                for s4 in range(4):
                    ps, dps = self.bank()
                    t0 = c * 512 + s4 * 128
                    fns = [lambda e, k=k, ps=ps, t0=t0, wv=wv: e.matmul(ps[:, 0:256], lhsT=H2T[:, k, t0:t0 + 128], rhs=wv[:, k, :],
                                                                     start=(k == 0), stop=(k == 7)) for k in range(8)]
                    P.group("pe", fns, [dwv, dH2[c]], [dps])
                    self.evac(VA[:, 4 * c + s4, :, 0:64], ps[:, 0:256].rearrange("p (h d) -> p h d", h=4), [dps], [dV[c]])
            P.op("dve", lambda e, KS=KS, KSb=KSb: e.tensor_copy(out=KSb, in_=KS), [dKS], [dKSb])
            for c in range(8):
                cs = slice(c * 512, (c + 1) * 512)
                for i in range(4):
                    tt_ = 4 * c + i
                    qb = tt_ // 2
                    bt = BTok[:, i, :, :]
                    if qb == 0:
                        P.op("pool", lambda e, bt=bt: e.memset(bt[:, :, 0:1], 0.0), [], [dBTok])
                        P.op("pool", lambda e, bt=bt: e.memset(bt[:, :, 1:16], NEG), [], [dBTok])
                        continue
                    ps, dps = self.bank()
                    fns = [lambda e, hh=hh, ps=ps, tt_=tt_, QA=QA, KSb=KSb: e.matmul(
                        ps[:, 16 * hh:16 * hh + 16], lhsT=QA[0:64, hh, tt_ * 128:(tt_ + 1) * 128], rhs=KSb[:, hh, :], start=True, stop=True)
                        for hh in range(4)]
                    P.group("pe", fns, [dQ[c], dKSb], [dps])
                    if qb < 3:
                        P.op("dve", lambda e, GM=GM: e.memset(GM.rearrange("p a b -> p (a b)"), -1e30), [], [dGM])
                    P.op("dve", lambda e, ps=ps, qb=qb, GM=GM: e.tensor_copy(out=GM[:, :, 0:qb], in_=ps[:, 0:64].rearrange("p (h n) -> p h n", h=4)[:, :, 0:qb]),
                         [dps], [dGM])
                    if qb >= 3:
                        P.op("dve", lambda e, qb=qb, GM=GM: e.memset(GM[:, :, qb:qb + 1], 1e30), [dGM], [dGM])
                    for hh in range(4):
                        P.op("dve", lambda e, hh=hh, GM=GM, M8=M8: e.max(out=M8[:, hh, :], in_=GM[:, hh, :]), [dGM], [dM8])
                    ti = 3 if qb >= 3 else 2
                    P.op("dve", lambda e, GM=GM, M8=M8, SEL=SEL, ti=ti: e.tensor_tensor(
                        out=SEL, in0=GM, in1=M8[:, :, ti:ti + 1].to_broadcast([128, 4, 16]), op=ALU.is_ge), [dGM, dM8], [dSEL])
                    P.op("dve", lambda e, bt=bt, SEL=SEL: e.tensor_scalar(out=bt, in0=SEL, scalar1=-NEG, scalar2=NEG, op0=ALU.mult, op1=ALU.add),
                         [dSEL], [dBTok])
                    if qb < 3:
                        P.op("dve", lambda e, bt=bt, qb=qb: e.memset(bt[:, :, qb:qb + 1], 0.0), [dBTok], [dBTok])
                        P.op("dve", lambda e, bt=bt, qb=qb: e.memset(bt[:, :, qb + 1:16], NEG), [dBTok], [dBTok])
                ps, dps = self.bank()
                psb = ps.bitcast(BF16)
                fns = [lambda e, i=i, psb=psb, BTok=BTok: e.transpose(psb[0:64, 128 * i:128 * i + 128],
                                                                    BTok[:, i, :, :].rearrange("p h n -> p (h n)"), ident_bf) for i in range(4)]
                P.group("pe", fns, [dBTok, dconst], [dps])
                b2 = c % 2
                P.op("act", lambda e, psb=psb, b2=b2, BTS=BTS: e.activation(out=BTS[b2], in_=psb[0:64, 0:512], func=AF.Copy), [dps], [dBTS[b2]])
                for hh in range(4):
                    P.dma("sp", lambda e, hh=hh, b2=b2, cs=cs, QA=QA, BTS=BTS: e.dma_start(out=QA[64:80, hh, cs], in_=BTS[b2][16 * hh:16 * hh + 16, :]),
                          [dBTS[b2], dQaug], [dBias[c]])
            if self.stop == "F2" and hg == 0:
                return self._finish_dbg({"QA": (QA, Dep(), [128, 4, T], BF16), "KA": (KA, Dep(), [128, 4, T], BF16)})
            self.bank_set = [3, 4, 5, 6, 7]
            for c in range(8):
                cs = slice(c * 512, (c + 1) * 512)
                nk = 4 * c + 4
                for hh in range(4):
                    hd = 4 * hg + hh
                    self.attn_unit(QA[0:86, hh, cs], [dQ[c], dBias[c], dQaug],
                                   [KA[0:86, hh, j * 128:(j + 1) * 128] for j in range(nk)],
                                   [VA[:, j, hh, :] for j in range(nk)], dK[:c + 1] + dV[:c + 1] + [dKaug, dVone],
                                   [None] * (nk - 4) + [CM[:, jj, :] for jj in range(4)],
                                   ATD[1][hd // 2, 64 * (hd % 2):64 * (hd % 2) + 64, cs], dATD1, cm_dep=dCM)
            self.bank_set = list(range(8))
            P.barrier()
            A.release(mF)
        A.release(mAI)
        self.attn_init(depth=4, obanks=(0, 1, 3), dbbank=2)
        wqm = A.alloc([128, 8, 256], BF16)
        dwqm = Dep()
        self.load_w(wqm, dwqm, moba_w_in, 8, 256, c0=2304)
        wts = self.ffn_prefetch(w_gate[1], w_up[1])
        QMn = A.alloc([128, 2, T], BF16)
        dQn = [Dep() for _ in range(8)]
        for c in range(8):
            cs = slice(c * 512, (c + 1) * 512)
            self.bank_set = [4, 5, 6, 7]
            for hp in range(2):
                ps, dps = self.bank()
                fns = [lambda e, k=k, hp=hp, ps=ps, cs=cs: e.matmul(ps, lhsT=wqm[:, k, 128 * hp:128 * hp + 128], rhs=H2T[:, k, cs],
                                                                   start=(k == 0), stop=(k == 7)) for k in range(8)]
                P.group("pe", fns, [dwqm, dH2[c]], [dps])
                self.evac(QMn[:, hp, cs], ps, [dps], [dQn[c]], scale=0.125)
            for h in range(4):
                hb, hp = 64 * (h % 2), h // 2
                self.mem_unit(QMn[hb:hb + 64, hp, cs], dQn[c],
                              [memKT[hb:hb + 64, hp, j * 128:(j + 1) * 128] for j in range(2)],
                              [memVA[:, j, h, 0:64] for j in range(2)], [dmemK, dmemV],
                              ATD[1][6 + hp, hb:hb + 64, cs], dATD1)
        self.bank_set = list(range(8))
        P.barrier()
        A.release(mL)
        if self.stop == "F":
            return self._finish_dbg({})
        self.gf_phase(wts, w_out[1], w_down[1], ATD[1], XT2, None, g_ffn1, out, g_fin)
        return self._finish_dbg({})

    def attn_init(self, depth=2, obanks=(0, 1), dbbank=2):
        A = self.A
        self.E = [A.alloc([128, 512], BF16) for _ in range(4)]
        self.dE = [Dep() for _ in range(4)]
        self.enext = 0
        self.adepth = depth
        self.obanks = list(obanks)
        self.dbbank = dbbank
        self.DEN = [A.alloc([128, 512], F32) for _ in range(depth)]
        self.dDEN = [Dep() for _ in range(depth)]
        self.RB = [A.alloc([64, 512], F32) for _ in range(depth)]
        self.dRB = [Dep() for _ in range(depth)]
        self.ON = [A.alloc([64, 512], BF16) for _ in range(depth)]
        self.dON = [Dep() for _ in range(depth)]
        self.unext = 0

    def mem_unit(self, q_ap, dq, k_tiles, v_tiles, dkv, dest, ddest):
        P = self.P
        u = self.unext
        self.unext += 1
        E, dE = self.E, self.dE
        di = u % self.adepth
        LN, dLN, RB, dRB, ON, dON = self.DEN[di], self.dDEN[di], self.RB[di], self.dRB[di], self.ON[di], self.dON[di]
        ones_bf, dconst = self.ones_bf, self.dconst
        O, dO = self.banks[u % 2], self.bdep[u % 2]
        DB, dDB = self.banks[2 + u % 2], self.bdep[2 + u % 2]
        dql = list(dq) if isinstance(dq, (list, tuple)) else [dq]
        eis = []
        for j in range(2):
            ps, dps = self.bank()
            P.op("pe", lambda e, j=j, ps=ps: e.matmul(ps, lhsT=k_tiles[j], rhs=q_ap, start=True, stop=True), dql + dkv, [dps])
            ei = self.enext % 4
            self.enext += 1
            P.op("act", lambda e, ei=ei, ps=ps: e.activation(out=E[ei], in_=ps, func=AF.Exp), [dps], [dE[ei]])
            eis.append(ei)
        fns = [lambda e, j=j: e.matmul(O[0:64, :], lhsT=v_tiles[j], rhs=E[eis[j]], start=(j == 0), stop=(j == 1)) for j in range(2)]
        P.group("pe", fns, [dE[eis[0]], dE[eis[1]]] + dkv, [dO])
        fns = [lambda e, j=j: e.matmul(DB[0:64, :], lhsT=ones_bf[:, 0:64], rhs=E[eis[j]], start=(j == 0), stop=(j == 1)) for j in range(2)]
        P.group("pe", fns, [dE[eis[0]], dE[eis[1]], dconst], [dDB])
        P.op("act", lambda e: e.activation(out=LN[0:64, :], in_=DB[0:64, :], func=AF.Ln), [dDB], [dLN])
        P.op("act", lambda e: e.activation(out=RB, in_=LN[0:64, :], func=AF.Exp, scale=-1.0), [dLN], [dRB])
        P.op("dve", lambda e: e.tensor_tensor(out=ON, in0=O[0:64, :], in1=RB, op=ALU.mult), [dO, dRB], [dON])
        P.dma("sp", lambda e: e.dma_start(out=dest, in_=ON), [dON], [Dep()])

    def attn_unit(self, q_ap, dq, k_tiles, v_tiles, dkv, cms, dest, ddest, cm_dep=None):
        P = self.P
        u = self.unext
        self.unext += 1
        E, dE = self.E, self.dE
        di = u % self.adepth
        DEN, dDEN, RB, dRB = self.DEN[di], self.dDEN[di], self.RB[di], self.dRB[di]
        ON, dON = self.ON, self.dON
        ones_f, ident_bf = self.ones_f, self.ident_bf
        ob = self.obanks[u % len(self.obanks)]
        O, dO = self.banks[ob], self.bdep[ob]
        DB, dDB = self.banks[self.dbbank], self.bdep[self.dbbank]
        n = len(k_tiles)
        K = q_ap.shape[0]
        Sj = [None] * n

        dql = list(dq) if isinstance(dq, (list, tuple)) else [dq]

        def qk(j):
            ps, dps = self.bank()
            c0 = 0
            if cms[j] is None:
                P.op("pe", lambda e: e.matmul(ps, lhsT=k_tiles[j], rhs=q_ap, start=True, stop=True), dql + dkv, [dps])
            else:
                c0 = 128 * (j - (n - 4))
                fns = [lambda e: e.matmul(ps[:, c0:512], lhsT=k_tiles[j], rhs=q_ap[:, c0:512], start=True, stop=False),
                       lambda e: e.matmul(ps[:, c0:512], lhsT=ident_bf, rhs=cms[j][:, c0:512], start=False, stop=True)]
                P.group("pe", fns, dql + [cm_dep, self.dconst] + dkv, [dps])
            ei = self.enext % 4
            self.enext += 1
            P.op("act", lambda e: e.activation(out=E[ei][:, c0:512], in_=ps[:, c0:512], func=AF.Exp), [dps], [dE[ei]])
            Sj[j] = (ei, c0)

        def pv(j):
            ei, c0 = Sj[j]
            P.op("pe", lambda e: e.matmul(O[0:65, c0:512], lhsT=v_tiles[j], rhs=E[ei][:, c0:512], start=(j == 0), stop=(j == n - 1)),
                 [dE[ei]] + dkv, [dO])

        LOOK = 2
        for j in range(min(LOOK, n)):
            qk(j)
        for j in range(n):
            if j + LOOK < n:
                qk(j + LOOK)
            pv(j)
        P.op("act", lambda e: e.activation(out=DEN[64:65, :], in_=O[64:65, :], func=AF.Copy), [dO], [dDEN])
        dend = self.DEND[di:di + 1, :]
        P.dma("sp", lambda e: e.dma_start(out=dend, in_=DEN[64:65, :]), [dDEN], [dDEN])
        P.dma("sp", lambda e: e.dma_start(out=DEN[0:64, :], in_=dend.partition_broadcast(64)), [dDEN], [dDEN])
        P.op("dve", lambda e: e.reciprocal(out=RB, in_=DEN[0:64, :]), [dDEN], [dRB])
        oi = di
        P.op("dve", lambda e: e.tensor_tensor(out=ON[oi], in0=O[0:64, :], in1=RB, op=ALU.mult), [dO, dRB], [dON[oi]])
        P.dma("sp", lambda e: e.dma_start(out=dest, in_=ON[oi]), [dON[oi]], [Dep()])

    def ffn_prefetch(self, wg_d, wu_d):
        P, A = self.P, self.A
        wg = A.alloc_top([128, 8, 2816], BF16)
        wu = A.alloc_top([128, 8, 2816], BF16)
        dwg, dwu = Dep(), Dep()
        self._ffn_w = (None, wg, wu, dwg, dwu, wg_d, wu_d)
        self._issue_ffn_loads()
        return self._ffn_w

    def gf_phase(self, wts, wo_d, wd_d, ATDl, XTin, XTout, gcol, out_final, gfin, nxt=None):
        P, A = self.P, self.A
        _, wg, wu, dwg, dwu, _, _ = wts
        m = A.mark()
        NC = 256
        NCH = T // NC
        wout = A.alloc([128, 8, D], BF16)
        dwo = Dep()
        self.load_w(wout, dwo, wo_d, 8, D)
        wd = A.alloc([128, 22, D], BF16)
        dwd = Dep()
        self.load_w(wd, dwd, wd_d, 22, D)
        ATc = A.alloc([128, 8, NC], BF16)
        dAT = Dep()
        xc = [A.alloc([128, 8, NC], F32) for _ in range(2)]
        dxc = [Dep(), Dep()]
        hc = [A.alloc([128, 8, NC], BF16) for _ in range(2)]
        dhc = [Dep(), Dep()]
        Ac = A.alloc([128, 22, NC], BF16)
        dAc = [Dep() for _ in range(22)]
        sg = [A.alloc([128, NC], F32) for _ in range(2)]
        dsg = [Dep(), Dep()]
        tmpn = self.rms_tmp(NC)
        if nxt is not None:
            hn = A.alloc([128, 8, NC], BF16)
            dhn = Dep()
        if out_final is not None:
            OT = A.alloc([128, 2, D], F32)
            dOT = Dep()
            dfin = [Dep(), Dep()]
            outv = out_final.rearrange("(c t p) f -> c p t f", p=128, t=2)

        def op_part(c):
            b2 = c % 2
            cs = slice(c * NC, (c + 1) * NC)
            P.dma("sp", lambda e: e.dma_start(out=ATc, in_=ATDl[:, :, cs].rearrange("f p n -> p f n")), [], [dAT])
            P.dma("sp", lambda e: e.dma_start(out=xc[b2], in_=XTin[:, :, cs].rearrange("f p n -> p f n")), [], [dxc[b2]])
            for ft in range(8):
                ps, dps = self.bank()
                fns = [lambda e, k=k, ft=ft, ps=ps: e.matmul(ps[:, 0:NC], lhsT=wout[:, k, ft * 128:(ft + 1) * 128], rhs=ATc[:, k, :],
                                                            start=(k == 0), stop=(k == 7)) for k in range(8)]
                P.group("pe", fns, [dwo, dAT], [dps])
                P.op("dve", lambda e, ft=ft, ps=ps: e.tensor_tensor(out=xc[b2][:, ft, :], in0=ps[:, 0:NC], in1=xc[b2][:, ft, :], op=ALU.add),
                     [dps], [dxc[b2]])
            self.rmsnorm(xc[b2], dxc[b2], gcol, hc[b2], dhc[b2], NC, tmpn)

        def ffn_gu(c, mid=None):
            b2 = c % 2
            for ht in range(22):
                if ht == 6 and mid is not None:
                    mid()
                psg, dpsg = self.bank()
                fns = [lambda e, k=k, ht=ht, ps=psg: e.matmul(ps[:, 0:NC], lhsT=wg[:, k, ht * 128:(ht + 1) * 128], rhs=hc[b2][:, k, :],
                                                             start=(k == 0), stop=(k == 7)) for k in range(8)]
                P.group("pe", fns, [dwg, dhc[b2]], [dpsg])
                psu, dpsu = self.bank()
                fns = [lambda e, k=k, ht=ht, ps=psu: e.matmul(ps[:, 0:NC], lhsT=wu[:, k, ht * 128:(ht + 1) * 128], rhs=hc[b2][:, k, :],
                                                             start=(k == 0), stop=(k == 7)) for k in range(8)]
                P.group("pe", fns, [dwu, dhc[b2]], [dpsu])
                sb = ht % 2
                P.op("act", lambda e, ps=psg, sb=sb: e.activation(out=sg[sb], in_=ps[:, 0:NC], func=AF.Silu), [dpsg], [dsg[sb]])
                P.op("dve", lambda e, ps=psu, sb=sb, ht=ht: e.tensor_tensor(out=Ac[:, ht, :], in0=ps[:, 0:NC], in1=sg[sb], op=ALU.mult),
                     [dpsu, dsg[sb]], [dAc[ht]])

        def ffn_down(c):
            b2 = c % 2
            cs = slice(c * NC, (c + 1) * NC)
            for ft in range(8):
                ps, dps = self.bank()
                fns = [lambda e, ht=ht, ft=ft, ps=ps: e.matmul(ps[:, 0:NC], lhsT=wd[:, ht, ft * 128:(ft + 1) * 128], rhs=Ac[:, ht, :],
                                                              start=(ht == 0), stop=(ht == 21)) for ht in range(22)]
                P.group("pe", fns, [dwd] + dAc, [dps])
                P.op("dve", lambda e, ft=ft, ps=ps: e.tensor_tensor(out=xc[b2][:, ft, :], in0=ps[:, 0:NC], in1=xc[b2][:, ft, :], op=ALU.add),
                     [dps], [dxc[b2]])
            if out_final is None:
                P.dma("sp", lambda e: e.dma_start(out=XTout[:, :, cs].rearrange("f p n -> p f n"), in_=xc[b2]), [dxc[b2]], [Dep()])
                if nxt is not None:
                    self.rmsnorm(xc[b2], dxc[b2], nxt[0], hn, dhn, NC, tmpn)
                    P.dma("sp", lambda e: e.dma_start(out=nxt[1][:, :, cs].rearrange("f p n -> p f n"), in_=hn), [dhn], [dhn])
            else:
                self.rmsnorm(xc[b2], dxc[b2], gfin, xc[b2], dxc[b2], NC, tmpn)

        def final_part(c):
            b2 = c % 2
            for t2 in range(2):
                for fq in range(2):
                    ps, dps = self.bank()
                    fns = [lambda e, i=i, t2=t2, fq=fq, ps=ps: e.transpose(
                        ps[:, 128 * i:128 * i + 128], xc[b2][:, 4 * fq + i, t2 * 128:(t2 + 1) * 128], self.ident) for i in range(4)]
                    P.group("pe", fns, [dxc[b2], self.dconst], [dps])
                    self.evac(OT[:, t2, fq * 512:(fq + 1) * 512], ps, [dps], [dOT])
            P.dma("sp", lambda e: e.dma_start(out=outv[c], in_=OT), [dOT, dxc[b2]], [dOT, dxc[b2]])

        op_part(0)
        for c in range(NCH):
            if out_final is not None and c > 0:
                ffn_gu(c, mid=lambda c=c: final_part(c - 1))
            else:
                ffn_gu(c)
            if c + 1 < NCH:
                op_part(c + 1)
            ffn_down(c)
        if out_final is not None:
            final_part(NCH - 1)
        P.barrier()
        A.release(m)
        A.release_top()

    def _issue_ffn_loads(self):
        P = self.P
        m, wg, wu, dwg, dwu, wg_d, wu_d = self._ffn_w
        for (dst, dd, src) in ((wg, dwg, wg_d), (wu, dwu, wu_d)):
            fns = []
            for k in range(8):
                for c in (0, 2048):
                    cw = min(2048, 2816 - c)
                    fns.append(lambda e, dst=dst, src=src, k=k, c=c, cw=cw: e.dma_start(
                        out=dst[:, k, c:c + cw], in_=src[k * 128:(k + 1) * 128, c:c + cw]))
            P.dma_multi("pool", fns, writes=[dd])

    def outproj_phase(self, wo, ATDl, XTin, XTout, HTDl, gcol):
        P, A = self.P, self.A
        m = A.mark()
        wout = A.alloc([128, 8, D], BF16)
        dwo = Dep()
        self.load_w(wout, dwo, wo, 8, D)
        self._issue_ffn_loads()
        ATc = [A.alloc([128, 8, 512], BF16) for _ in range(2)]
        dAT = [Dep(), Dep()]
        xc = [A.alloc([128, 8, 512], F32) for _ in range(2)]
        dxc = [Dep(), Dep()]
        hc = [A.alloc([128, 8, 512], BF16) for _ in range(2)]
        dhc = [Dep(), Dep()]
        tmpn = self.rms_tmp(512)
        for c in range(8):
            b2 = c % 2
            cs = slice(c * 512, (c + 1) * 512)
            P.dma("sp", lambda e, cs=cs, b2=b2: e.dma_start(out=ATc[b2], in_=ATDl[:, :, cs].rearrange("f p n -> p f n")), [], [dAT[b2]])
            P.dma("sp", lambda e, cs=cs, b2=b2: e.dma_start(out=xc[b2], in_=XTin[:, :, cs].rearrange("f p n -> p f n")), [], [dxc[b2]])
            for ft in range(8):
                ps, dps = self.bank()
                fns = [lambda e, k=k, ft=ft, ps=ps, b2=b2: e.matmul(ps, lhsT=wout[:, k, ft * 128:(ft + 1) * 128], rhs=ATc[b2][:, k, :],
                                                                   start=(k == 0), stop=(k == 7)) for k in range(8)]
                P.group("pe", fns, [dwo, dAT[b2]], [dps])
                P.op("dve", lambda e, ft=ft, ps=ps, b2=b2: e.tensor_tensor(out=xc[b2][:, ft, :], in0=ps, in1=xc[b2][:, ft, :], op=ALU.add),
                     [dps], [dxc[b2]])
            P.dma("sp", lambda e, cs=cs, b2=b2: e.dma_start(out=XTout[:, :, cs].rearrange("f p n -> p f n"), in_=xc[b2]), [dxc[b2]], [Dep()])
            self.rmsnorm(xc[b2], dxc[b2], gcol, hc[b2], dhc[b2], 512, tmpn)
            P.dma("sp", lambda e, cs=cs, b2=b2: e.dma_start(out=HTDl[:, :, cs].rearrange("f p n -> p f n"), in_=hc[b2]), [dhc[b2]], [Dep()])
        P.barrier()
        A.release(m)

    def ffn_phase(self, wts, wd_d, HTDl, XTin, XTout, out_final, gfin):
        P, A = self.P, self.A
        m, wg, wu, dwg, dwu, _, _ = wts
        NC = 256
        wd = A.alloc([128, 22, D], BF16)
        dwd = Dep()
        self.load_w(wd, dwd, wd_d, 22, D)
        hc = [A.alloc([128, 8, NC], BF16) for _ in range(2)]
        dhc = [Dep(), Dep()]
        xc = [A.alloc([128, 8, NC], F32) for _ in range(2)]
        dxc = [Dep(), Dep()]
        Ac = A.alloc([128, 22, NC], BF16)
        dAc = [Dep() for _ in range(22)]
        sg = [A.alloc([128, NC], F32) for _ in range(2)]
        dsg = [Dep(), Dep()]
        if out_final is not None:
            yT = A.alloc([128, 8, NC], F32)
            dyT = Dep()
            OT = A.alloc([128, 2, D], F32)
            dOT = Dep()
            tmpn = self.rms_tmp(NC)
            outv = out_final.rearrange("(c t p) f -> c p t f", p=128, t=2)
        for c in range(T // NC):
            b2 = c % 2
            cs = slice(c * NC, (c + 1) * NC)
            P.dma("sp", lambda e, cs=cs, b2=b2: e.dma_start(out=hc[b2], in_=HTDl[:, :, cs].rearrange("f p n -> p f n")), [], [dhc[b2]])
            P.dma("sp", lambda e, cs=cs, b2=b2: e.dma_start(out=xc[b2], in_=XTin[:, :, cs].rearrange("f p n -> p f n")), [], [dxc[b2]])
            for ht in range(22):
                psg, dpsg = self.bank()
                fns = [lambda e, k=k, ht=ht, ps=psg, b2=b2: e.matmul(ps[:, 0:NC], lhsT=wg[:, k, ht * 128:(ht + 1) * 128], rhs=hc[b2][:, k, :],
                                                                    start=(k == 0), stop=(k == 7)) for k in range(8)]
                P.group("pe", fns, [dwg, dhc[b2]], [dpsg])
                psu, dpsu = self.bank()
                fns = [lambda e, k=k, ht=ht, ps=psu, b2=b2: e.matmul(ps[:, 0:NC], lhsT=wu[:, k, ht * 128:(ht + 1) * 128], rhs=hc[b2][:, k, :],
                                                                    start=(k == 0), stop=(k == 7)) for k in range(8)]
                P.group("pe", fns, [dwu, dhc[b2]], [dpsu])
                sb = ht % 2
                P.op("act", lambda e, ps=psg, sb=sb: e.activation(out=sg[sb], in_=ps[:, 0:NC], func=AF.Silu), [dpsg], [dsg[sb]])
                P.op("dve", lambda e, ps=psu, sb=sb, ht=ht: e.tensor_tensor(out=Ac[:, ht, :], in0=ps[:, 0:NC], in1=sg[sb], op=ALU.mult),
                     [dpsu, dsg[sb]], [dAc[ht]])
            for ft in range(8):
                ps, dps = self.bank()
                fns = [lambda e, ht=ht, ft=ft, ps=ps: e.matmul(ps[:, 0:NC], lhsT=wd[:, ht, ft * 128:(ft + 1) * 128], rhs=Ac[:, ht, :],
                                                              start=(ht == 0), stop=(ht == 21)) for ht in range(22)]
                P.group("pe", fns, [dwd] + dAc, [dps])
                P.op("dve", lambda e, ft=ft, ps=ps, b2=b2: e.tensor_tensor(out=xc[b2][:, ft, :], in0=ps[:, 0:NC], in1=xc[b2][:, ft, :], op=ALU.add),
                     [dps], [dxc[b2]])
            if out_final is None:
                P.dma("sp", lambda e, cs=cs, b2=b2: e.dma_start(out=XTout[:, :, cs].rearrange("f p n -> p f n"), in_=xc[b2]), [dxc[b2]], [Dep()])
            else:
                self.rmsnorm(xc[b2], dxc[b2], gfin, yT, dyT, NC, tmpn)
                for t2 in range(2):
                    for fq in range(2):
                        ps, dps = self.bank()
                        fns = [lambda e, i=i, t2=t2, fq=fq, ps=ps: e.transpose(
                            ps[:, 128 * i:128 * i + 128], yT[:, 4 * fq + i, t2 * 128:(t2 + 1) * 128], self.ident) for i in range(4)]
                        P.group("pe", fns, [dyT, self.dconst], [dps])
                        self.evac(OT[:, t2, fq * 512:(fq + 1) * 512], ps, [dps], [dOT])
                P.dma("sp", lambda e, c=c: e.dma_start(out=outv[c], in_=OT), [dOT], [Dep()])
        P.barrier()
        A.release(m)

    def _s5_setup(self, lam_re, lam_im, log_dt, b_re, b_im, c_re, c_im, WS_sb, WY_sb, KB_sb, V12, II, dS5):
        nc, P, A = self.nc, self.P, self.A
        ident, ident_bf, dconst = self.ident, self.ident_bf, self.dconst

        def tl(shape, dt=F32):
            return (A.alloc(shape, dt), Dep())

        def tt(o, a, b, op, eng="dve"):
            P.op(eng, lambda e: e.tensor_tensor(out=o[0], in0=a[0], in1=b[0], op=op), [a[1], b[1]], [o[1]])

        def ts(o, a, s1, op0, s2=None, op1=None, extra=()):
            if op1 is None:
                P.op("dve", lambda e: e.tensor_scalar(out=o[0], in0=a[0], scalar1=s1, scalar2=None, op0=op0), [a[1]] + list(extra), [o[1]])
            else:
                P.op("dve", lambda e: e.tensor_scalar(out=o[0], in0=a[0], scalar1=s1, scalar2=s2, op0=op0, op1=op1),
                     [a[1]] + list(extra), [o[1]])

        def stt(o, a, sc, b, op0, op1, extra=()):
            P.op("dve", lambda e: e.scalar_tensor_tensor(out=o[0], in0=a[0], scalar=sc, in1=b[0], op0=op0, op1=op1),
                 [a[1], b[1]] + list(extra), [o[1]])

        def act(o, a, func, scale=1.0):
            P.op("act", lambda e: e.activation(out=o[0], in_=a[0], func=func, scale=scale), [a[1]], [o[1]])

        def sub(t, ap):
            return (ap, t[1])

        LT = tl([48, 3, 128])
        LD = tl([48, 1])
        for h in range(2):
            P.dma("sp", lambda e, h=h: e.dma_start(out=LT[0][:, 0, 64 * h:64 * h + 64], in_=lam_re), [], [LT[1]])
            P.dma("sp", lambda e, h=h: e.dma_start(out=LT[0][:, 1, 64 * h:64 * h + 64], in_=lam_im), [], [LT[1]])
        P.dma("sp", lambda e: e.dma_start(out=LD[0], in_=log_dt.rearrange("(g o) -> g o", o=1)), [], [LD[1]])
        P.op("dve", lambda e: e.tensor_copy(out=LT[0][:, 2, :], in_=LD[0].to_broadcast([48, 128])), [LD[1]], [LT[1]])
        BR, BI, CR, CI = tl([128, 48, 16]), tl([128, 48, 16]), tl([128, 48, 16]), tl([128, 48, 16])
        for (dst, src) in ((BR, b_re), (BI, b_im)):
            for h in range(2):
                for gq in range(4):
                    P.dma("sp", lambda e, dst=dst, src=src, h=h, gq=gq: e.dma_start(
                        out=dst[0][64 * h:64 * h + 64, 12 * gq:12 * gq + 12, :],
                        in_=src[12 * gq:12 * gq + 12].rearrange("g p c -> p g c")), [], [dst[1]])
        CT = [tl([128, 6, 2, 64]), tl([128, 6, 2, 64])]
        for i, src in enumerate((c_re, c_im)):
            for h in range(2):
                P.dma("sp", lambda e, i=i, src=src, h=h: e.dma_start(
                    out=CT[i][0][:, :, h, :], in_=src.rearrange("(o g) c p -> (g c) o p", g=8)), [], [CT[i][1]])
        lr, li, ldt = tl([128, 48]), tl([128, 48]), tl([128, 48])
        ps, dps = self.bank()
        fns = [lambda e, i=i, ps=ps: e.transpose(ps[:, 64 * i:64 * i + 48], LT[0][:, i, :], ident[0:48, 0:48]) for i in range(3)]
        P.group("pe", fns, [LT[1], dconst], [dps])
        for i, t in enumerate((lr, li, ldt)):
            P.op("dve", lambda e, i=i, t=t, ps=ps: e.tensor_copy(out=t[0], in_=ps[:, 64 * i:64 * i + 48]), [dps], [t[1]])
        for i, dst in enumerate((CR, CI)):
            for half in range(2):
                ps, dps = self.bank()
                os_ = list(range(4)) if half == 0 else [4, 5]
                fns = [lambda e, o=o, i=i, ps=ps: e.transpose(ps[:, 128 * (o % 4):128 * (o % 4) + 128],
                                                              CT[i][0][:, o, :, :].rearrange("p a b -> p (a b)"), ident) for o in os_]
                P.group("pe", fns, [CT[i][1], dconst], [dps])
                n = len(os_) * 128
                o0 = os_[0]
                P.op("dve", lambda e, dst=dst, ps=ps, n=n, o0=o0: e.tensor_copy(
                    out=dst[0][:, 8 * o0:8 * o0 + n // 16, :].rearrange("p g c -> p (g c)"), in_=ps[:, 0:n]), [dps], [dst[1]])
        mtop, mbot, nmbot, nmtop = tl([128, 1]), tl([128, 1]), tl([128, 1]), tl([128, 1])
        for t, (v0, v1) in ((mtop, (1.0, 0.0)), (mbot, (0.0, 1.0)), (nmbot, (0.0, -1.0)), (nmtop, (-1.0, 0.0))):
            P.op("pool", lambda e, t=t, v0=v0: e.memset(t[0][0:64, :], v0), [], [t[1]])
            P.op("pool", lambda e, t=t, v1=v1: e.memset(t[0][64:128, :], v1), [], [t[1]])
        G = tl([8, 128])
        G2 = tl([4, 128])
        P.op("pool", lambda e: e.memset(G[0], 1.0), [], [G[1]])
        P.op("pool", lambda e: e.affine_select(out=G[0], in_=G[0], pattern=[[1, 128]], compare_op=ALU.is_ge, fill=0.0,
                                                base=0, channel_multiplier=-16), [G[1]], [G[1]])
        P.op("pool", lambda e: e.affine_select(out=G[0], in_=G[0], pattern=[[-1, 128]], compare_op=ALU.is_ge, fill=0.0,
                                                base=15, channel_multiplier=16), [G[1]], [G[1]])
        P.op("pool", lambda e: e.memset(G2[0], 1.0), [], [G2[1]])
        P.op("pool", lambda e: e.affine_select(out=G2[0], in_=G2[0], pattern=[[1, 128]], compare_op=ALU.is_ge, fill=0.0,
                                                base=0, channel_multiplier=-32), [G2[1]], [G2[1]])
        P.op("pool", lambda e: e.affine_select(out=G2[0], in_=G2[0], pattern=[[-1, 128]], compare_op=ALU.is_ge, fill=0.0,
                                                base=15, channel_multiplier=32), [G2[1]], [G2[1]])
        onec = tl([4, 1])
        P.op("pool", lambda e: e.memset(onec[0], 1.0), [], [onec[1]])
        BD, me, mo = tl([128, 128]), tl([128, 1]), tl([128, 1])
        ps, dps = self.bank()
        P.op("pe", lambda e, ps=ps: e.matmul(ps[:, 0:128], lhsT=G[0], rhs=G[0], start=True, stop=True), [G[1]], [dps])
        P.op("dve", lambda e, ps=ps: e.tensor_copy(out=BD[0], in_=ps[:, 0:128]), [dps], [BD[1]])
        ps, dps = self.bank()
        P.op("pe", lambda e, ps=ps: e.matmul(ps[:, 0:1], lhsT=G2[0], rhs=onec[0], start=True, stop=True), [G2[1], onec[1]], [dps])
        P.op("dve", lambda e, ps=ps: e.tensor_copy(out=me[0], in_=ps[:, 0:1]), [dps], [me[1]])
        ts(mo, me, -1.0, ALU.mult, 1.0, ALU.add)
        P.op("dve", lambda e: e.tensor_tensor(out=II, in0=ident_bf[:, 0:64], in1=ident_bf[:, 64:128], op=ALU.add), [dconst], [dS5])
        dt, th, lrd, mag = tl([128, 48]), tl([128, 48]), tl([128, 48]), tl([128, 48])
        act(dt, ldt, AF.Exp)
        tt(th, li, dt, ALU.mult)
        tt(lrd, lr, dt, ALU.mult)
        act(mag, lrd, AF.Exp)
        sn, cs = tl([128, 48]), tl([128, 48])
        t1, t2, t3 = tl([128, 48]), tl([128, 48]), tl([128, 48])
        for (dst, shift) in ((sn, 0.0), (cs, math.pi / 2)):
            ts(t1, th, shift, ALU.add)
            ts(t2, t1, 1.0 / (2 * math.pi), ALU.mult)
            ts(t3, t2, MAGIC, ALU.add)
            ts(t2, t3, MAGIC, ALU.subtract)
            stt(t3, t2, -2.0 * math.pi, t1, ALU.mult, ALU.add)
            ts(t1, t3, 3.1415925, ALU.min, -3.1415925, ALU.max)
            act(dst, t1, AF.Sin)
        ar, ai = tl([128, 48]), tl([128, 48])
        tt(ar, mag, cs, ALU.mult)
        tt(ai, mag, sn, ALU.mult)
        den, rden, nr, fr, fi = tl([128, 48]), tl([128, 48]), tl([128, 48]), tl([128, 48]), tl([128, 48])
        tt(t1, lr, lr, ALU.mult)
        tt(t2, li, li, ALU.mult)
        tt(den, t1, t2, ALU.add)
        P.op("dve", lambda e: e.reciprocal(out=rden[0], in_=den[0]), [den[1]], [rden[1]])
        ts(nr, ar, -1.0, ALU.add)
        tt(t1, nr, lr, ALU.mult)
        tt(t2, ai, li, ALU.mult)
        tt(t3, t1, t2, ALU.add)
        tt(fr, t3, rden, ALU.mult)
        tt(t1, ai, lr, ALU.mult)
        tt(t2, nr, li, ALU.mult)
        tt(t3, t1, t2, ALU.subtract)
        tt(fi, t3, rden, ALU.mult)

        def bc(t):
            return (t[0].unsqueeze(2).to_broadcast([128, 48, 16]), t[1])

        bbr, bbi, u1, u2 = tl([128, 48, 16]), tl([128, 48, 16]), tl([128, 48, 16]), tl([128, 48, 16])
        tt(u1, BR, bc(fr), ALU.mult)
        tt(u2, BI, bc(fi), ALU.mult)
        tt(bbr, u1, u2, ALU.subtract)
        tt(u1, BI, bc(fr), ALU.mult)
        tt(u2, BR, bc(fi), ALU.mult)
        tt(bbi, u1, u2, ALU.add)
        PWr, PWi = tl([128, 48, 9]), tl([128, 48, 9])
        P.op("pool", lambda e: e.memset(PWr[0][:, :, 0:1], 1.0), [], [PWr[1]])
        P.op("pool", lambda e: e.memset(PWi[0][:, :, 0:1], 0.0), [], [PWi[1]])
        P.op("dve", lambda e: e.tensor_copy(out=PWr[0][:, :, 1], in_=ar[0]), [ar[1]], [PWr[1]])
        P.op("dve", lambda e: e.tensor_copy(out=PWi[0][:, :, 1], in_=ai[0]), [ai[1]], [PWi[1]])
        for n in range(1, 8):
            pr, pi = sub(PWr, PWr[0][:, :, n]), sub(PWi, PWi[0][:, :, n])
            tt(t1, pr, ar, ALU.mult)
            tt(t2, pi, ai, ALU.mult)
            tt(sub(PWr, PWr[0][:, :, n + 1]), t1, t2, ALU.subtract)
            tt(t1, pr, ai, ALU.mult)
            tt(t2, pi, ar, ALU.mult)
            tt(sub(PWi, PWi[0][:, :, n + 1]), t1, t2, ALU.add)
        SQr, SQi = tl([128, 48, 9]), tl([128, 48, 9])
        P.op("dve", lambda e: e.tensor_copy(out=SQr[0][:, :, 0], in_=PWr[0][:, :, 8]), [PWr[1]], [SQr[1]])
        P.op("dve", lambda e: e.tensor_copy(out=SQi[0][:, :, 0], in_=PWi[0][:, :, 8]), [PWi[1]], [SQi[1]])
        for j in range(8):
            r, i_ = sub(SQr, SQr[0][:, :, j]), sub(SQi, SQi[0][:, :, j])
            tt(t1, r, r, ALU.mult)
            tt(t2, i_, i_, ALU.mult)
            tt(sub(SQr, SQr[0][:, :, j + 1]), t1, t2, ALU.subtract)
            tt(t1, r, i_, ALU.mult)
            ts(sub(SQi, SQi[0][:, :, j + 1]), t1, 2.0, ALU.mult)
        w1 = tl([128, 48, 9])
        V12t = (V12, dS5)
        ts(w1, SQi, nmbot[0], ALU.mult, extra=[nmbot[1]])
        stt(sub(V12t, V12[:, :, :, 0]), SQr, mtop[0], w1, ALU.mult, ALU.add, extra=[mtop[1]])
        ts(w1, SQr, mbot[0], ALU.mult, extra=[mbot[1]])
        stt(sub(V12t, V12[:, :, :, 1]), SQi, mtop[0], w1, ALU.mult, ALU.add, extra=[mtop[1]])
        P1, P2, Q1, Q2 = tl([128, 48, 9]), tl([128, 48, 9]), tl([128, 48, 9]), tl([128, 48, 9])
        ts(w1, PWi, nmbot[0], ALU.mult, extra=[nmbot[1]])
        stt(P1, PWr, mtop[0], w1, ALU.mult, ALU.add, extra=[mtop[1]])
        ts(w1, PWr, nmbot[0], ALU.mult, extra=[nmbot[1]])
        stt(P2, PWi, nmtop[0], w1, ALU.mult, ALU.add, extra=[nmtop[1]])
        ts(w1, PWi, mbot[0], ALU.mult, extra=[mbot[1]])
        stt(Q1, PWr, mtop[0], w1, ALU.mult, ALU.add, extra=[mtop[1]])
        ts(w1, PWr, mbot[0], ALU.mult, extra=[mbot[1]])
        stt(Q2, PWi, nmtop[0], w1, ALU.mult, ALU.add, extra=[nmtop[1]])
        CA = tl([128, 9, 48, 16], BF16)
        WSb = tl([128, 8, 48, 16], BF16)
        for n in range(9):
            tt(u1, CR, (P1[0][:, :, n:n + 1].to_broadcast([128, 48, 16]), P1[1]), ALU.mult)
            tt(u2, CI, (P2[0][:, :, n:n + 1].to_broadcast([128, 48, 16]), P2[1]), ALU.mult)
            tt(sub(CA, CA[0][:, n, :, :]), u1, u2, ALU.add)
        for sl in range(8):
            n = 7 - sl
            tt(u1, bbr, (Q1[0][:, :, n:n + 1].to_broadcast([128, 48, 16]), Q1[1]), ALU.mult)
            tt(u2, bbi, (Q2[0][:, :, n:n + 1].to_broadcast([128, 48, 16]), Q2[1]), ALU.mult)
            tt(sub(WSb, WSb[0][:, sl, :, :]), u1, u2, ALU.add)
        BBs = tl([128, 48, 16], BF16)
        ts(u1, bbi, mbot[0], ALU.mult, extra=[mbot[1]])
        stt(BBs, bbr, mtop[0], u1, ALU.mult, ALU.add, extra=[mtop[1]])
        tmpk = tl([128, 128])
        for o in range(6):
            for half in range(2):
                ps, dps = self.bank()
                fns = [lambda e, o=o, lag=lag, ps=ps: e.matmul(
                    ps[:, 128 * (lag % 4):128 * (lag % 4) + 128], lhsT=BBs[0][:, 8 * o:8 * o + 8, :].rearrange("p g c -> p (g c)"),
                    rhs=CA[0][:, lag, 8 * o:8 * o + 8, :].rearrange("p g c -> p (g c)"), start=True, stop=True)
                    for lag in range(4 * half, 4 * half + 4)]
                P.group("pe", fns, [BBs[1], CA[1]], [dps])
                for lag in range(4 * half, 4 * half + 4):
                    src = ps[:, 128 * (lag % 4):128 * (lag % 4) + 128]
                    if lag == 0:
                        P.op("dve", lambda e, src=src: e.tensor_tensor(out=tmpk[0], in0=src, in1=BD[0], op=ALU.mult),
                             [dps, BD[1]], [tmpk[1]])
                        P.op("dve", lambda e, o=o: e.scalar_tensor_tensor(
                            out=KB_sb[:, o, 0, :], in0=ident, scalar=self.gains[:, 48 + o:49 + o], in1=tmpk[0],
                            op0=ALU.mult, op1=ALU.add), [tmpk[1], dconst], [dS5])
                    else:
                        P.op("dve", lambda e, src=src, o=o, lag=lag: e.tensor_tensor(out=KB_sb[:, o, lag, :], in0=src, in1=BD[0], op=ALU.mult),
                             [dps, BD[1]], [dS5])
        for o in range(6):
            ps, dps = self.bank()
            psb = ps.bitcast(BF16)
            fns = [lambda e, o=o, sl=sl, psb=psb: e.transpose(
                psb[:, 128 * sl:128 * sl + 128], WSb[0][:, sl, 8 * o:8 * o + 8, :].rearrange("p g c -> p (g c)"), ident_bf)
                for sl in range(8)]
            P.group("pe", fns, [WSb[1], dconst], [dps])
            for par, m in ((0, me), (1, mo)):
                P.op("dve", lambda e, o=o, par=par, m=m, psb=psb: e.tensor_scalar(
                    out=WS_sb[:, o, :, par, :], in0=psb.rearrange("p (s n) -> p s n", s=8), scalar1=m[0], scalar2=None, op0=ALU.mult),
                    [dps, m[1]], [dS5])
        P.op("pool", lambda e: e.memset(WY_sb.rearrange("p g s c -> p (g s c)"), 0.0), [], [dS5])
        CAv = CA[0].rearrange("p n (g two) c -> p n g two c", two=2)
        WYv = WY_sb.rearrange("p (g two) s c -> p g two s c", two=2)
        for sl in range(8):
            for par in range(2):
                P.op("dve", lambda e, sl=sl, par=par: e.tensor_copy(
                    out=WYv[:, :, par, sl, 16 * par:16 * par + 16], in_=CAv[:, sl + 1, :, par, :]), [CA[1]], [dS5])

    def _finish_dbg(self, sb):
        P, nc = self.P, self.nc
        for name, (ap, dep, shape, dt) in sb.items():
            o = nc.dram_tensor("dbg_" + name, list(shape), dt, kind="ExternalOutput").ap()
            self.outs.append("dbg_" + name)
            P.dma("sp", lambda e, o=o, ap=ap: e.dma_start(out=o, in_=ap), [dep], [Dep()])
        P.finish()
        return nc


_INPUT_NAMES = ["x", "mem", "mem_norm_g", "w_mem_kv", "mix_norm_g", "s5_w_in", "s5_lambda_re", "s5_lambda_im", "s5_log_dt",
                "s5_b_re", "s5_b_im", "s5_c_re", "s5_c_im", "s5_d", "s5_w_glu", "s5_b_glu", "moba_w_in", "w_out",
                "ffn_norm_g", "w_gate", "w_up", "w_down", "final_norm_g"]


def make_in_maps(inputs, ncores=8):
    maps = []
    shared = {}
    for k in _INPUT_NAMES:
        if k in ("x", "mem"):
            continue
        a = np.ascontiguousarray(np.asarray(inputs[k], dtype=np.float32))
        if k in ("s5_w_in", "s5_lambda_re", "s5_lambda_im", "s5_log_dt", "s5_b_re", "s5_b_im", "s5_c_re", "s5_c_im",
                 "s5_d", "s5_w_glu", "s5_b_glu", "moba_w_in"):
            a = a[0]
        shared[k] = np.ascontiguousarray(a)
    xs = np.asarray(inputs["x"], dtype=np.float32)
    ms = np.asarray(inputs["mem"], dtype=np.float32)
    for c in range(ncores):
        m = dict(shared)
        m["x"] = np.ascontiguousarray(xs[c])
        m["mem"] = np.ascontiguousarray(ms[c])
        maps.append(m)
    return maps


def kernel(**inputs):
    b = Builder()
    nc = b.build()
    res = run_bass_kernel_spmd(nc, make_in_maps(inputs), core_ids=list(range(8)))
    return np.stack([np.asarray(r["out"]) for r in res.results], axis=0).astype(np.float32)
```
